# Optimizing a Trainium2 kernel written in Bass

```python
import jax, jax.numpy as jnp
from jax import lax
import numpy as np

D_MODEL = 1024
BATCH = 8
SEQ = 4096
DEPTH = 1

D_MIX = D_MODEL
SB_HEADS = 8
HEAD_DIM = 64
SB_WIDTH = SB_HEADS * HEAD_DIM
LRU_WIDTH = D_MIX - SB_WIDTH
LRU_BLOCKS = 8
LRU_BLOCK_DIM = LRU_WIDTH // LRU_BLOCKS
CONV_WIDTH = 4
LRU_C = 8.0
D_IN = 3 * SB_WIDTH + 2 * LRU_WIDTH
Q_BLOCK = 128
N_EXPERTS = 32
TOP_K = 4
D_EXPERT = D_MODEL
SWIGLU_LIMIT = 7.0
SWIGLU_ALPHA = 1.702
EXPERT_BLOCK = 256
EPS = 1e-6
N_MOD = 6

kernel_name = "hymba_stickbreak_rglru_moe_adaln"


def rmsnorm(x, g):
    x32 = x.astype(jnp.float32)
    y = x32 * lax.rsqrt(jnp.mean(x32 * x32, axis=-1, keepdims=True) + EPS)
    return (y * g.astype(jnp.float32)).astype(x.dtype)


def modulate(h, shift, scale):
    return h * (1.0 + scale[:, None, :]) + shift[:, None, :]


def stick_breaking_attention(q, k, v):
    B, S, H, Dh = q.shape
    n_blocks = S // Q_BLOCK
    qh = jnp.transpose(q, (0, 2, 1, 3)).astype(jnp.float32) * (Dh ** -0.5)
    kh = jnp.transpose(k, (0, 2, 1, 3)).astype(jnp.float32)
    vh = jnp.transpose(v, (0, 2, 1, 3)).astype(jnp.float32)
    k_pos = jnp.arange(S)

    def one_block(i):
        q_blk = lax.dynamic_slice_in_dim(qh, i * Q_BLOCK, Q_BLOCK, axis=2)
        z = jnp.einsum('bhqd,bhkd->bhqk', q_blk, kh)
        q_pos = i * Q_BLOCK + jnp.arange(Q_BLOCK)
        causal = k_pos[None, :] < q_pos[:, None]
        log_beta = jax.nn.log_sigmoid(z)
        log_keep = jnp.where(causal, log_beta - z, 0.0)
        later = lax.cumsum(log_keep, axis=3, reverse=True) - log_keep
        weights = jnp.where(causal, jnp.exp(log_beta + later), 0.0)
        return jnp.einsum('bhqk,bhkd->bhqd', weights, vh)

    out = lax.map(one_block, jnp.arange(n_blocks))
    out = jnp.transpose(out, (1, 0, 3, 2, 4)).reshape(B, S, H * Dh)
    return out.astype(q.dtype)


def rg_lru_branch(xr, gr, conv_w, conv_b, rg_w, rg_b, ig_w, ig_b, lru_lambda):
    B, S, W = xr.shape
    x32 = xr.astype(jnp.float32)
    xp = jnp.pad(x32, ((0, 0), (CONV_WIDTH - 1, 0), (0, 0)))
    cw = conv_w.astype(jnp.float32)
    xc = conv_b.astype(jnp.float32) + sum(cw[j] * xp[:, j:j + S] for j in range(CONV_WIDTH))
    xh = xc.reshape(B, S, LRU_BLOCKS, LRU_BLOCK_DIM)
    r = jax.nn.sigmoid(jnp.einsum('bshi,hij->bshj', xh, rg_w.astype(jnp.float32)).reshape(B, S, W)
                       + rg_b.astype(jnp.float32))
    ig = jax.nn.sigmoid(jnp.einsum('bshi,hij->bshj', xh, ig_w.astype(jnp.float32)).reshape(B, S, W)
                        + ig_b.astype(jnp.float32))
    log_a = -LRU_C * r * jax.nn.softplus(-lru_lambda.astype(jnp.float32))
    a = jnp.exp(log_a)
    b = jnp.sqrt(-jnp.expm1(2.0 * log_a)) * (ig * xc)

    def combine(left, right):
        a1, b1 = left
        a2, b2 = right
        return a1 * a2, a2 * b1 + b2

    _, h = lax.associative_scan(combine, (a, b), axis=1)
    y = h * jax.nn.gelu(gr.astype(jnp.float32))
    return y.astype(xr.dtype)


def hybrid_mixer(h, w_in, conv_w, conv_b, rg_w, rg_b, ig_w, ig_b, lru_lambda,
                 attn_out_g, lru_out_g, w_out):
    B, S, _ = h.shape
    proj = jnp.einsum('bsd,de->bse', h, w_in)
    q, k, v, xr, gr = jnp.split(
        proj, [SB_WIDTH, 2 * SB_WIDTH, 3 * SB_WIDTH, 3 * SB_WIDTH + LRU_WIDTH], axis=-1)
    q = q.reshape(B, S, SB_HEADS, HEAD_DIM)
    k = k.reshape(B, S, SB_HEADS, HEAD_DIM)
    v = v.reshape(B, S, SB_HEADS, HEAD_DIM)
    y_attn = stick_breaking_attention(q, k, v)
    y_lru = rg_lru_branch(xr, gr, conv_w, conv_b, rg_w, rg_b, ig_w, ig_b, lru_lambda)
    y = jnp.concatenate([rmsnorm(y_attn, attn_out_g), rmsnorm(y_lru, lru_out_g)], axis=-1)
    return jnp.einsum('bse,ed->bsd', y, w_out)


def moe_ffn(h, router_w, router_b, w_gate, b_gate, w_up, b_up, w_down, b_down):
    B, S, D = h.shape
    N = B * S
    xf = h.reshape(N, D)
    logits = xf.astype(jnp.float32) @ router_w.astype(jnp.float32) + router_b.astype(jnp.float32)
    top_val, top_idx = lax.top_k(logits, TOP_K)
    probs = jax.nn.softmax(top_val, axis=-1)
    n_assign = N * TOP_K
    flat_e = top_idx.reshape(-1).astype(jnp.int32)
    order = jnp.argsort(flat_e)
    e_sorted = flat_e[order]
    tok_sorted = (order // TOP_K).astype(jnp.int32)
    p_sorted = probs.reshape(-1)[order]
    counts = jnp.bincount(flat_e, length=N_EXPERTS).astype(jnp.int32)
    starts = jnp.cumsum(counts) - counts
    padded = ((counts + EXPERT_BLOCK - 1) // EXPERT_BLOCK) * EXPERT_BLOCK
    pad_ends = jnp.cumsum(padded)
    pad_starts = pad_ends - padded
    dest = pad_starts[e_sorted] + (jnp.arange(n_assign, dtype=jnp.int32) - starts[e_sorted])
    n_blocks = -(-n_assign // EXPERT_BLOCK) + N_EXPERTS
    cap = n_blocks * EXPERT_BLOCK
    tok_buf = jnp.zeros((cap,), jnp.int32).at[dest].set(tok_sorted)
    p_buf = jnp.zeros((cap,), jnp.float32).at[dest].set(p_sorted)
    block_start = jnp.arange(n_blocks, dtype=jnp.int32) * EXPERT_BLOCK
    block_e = jnp.minimum(jnp.searchsorted(pad_ends, block_start, side='right'),
                          N_EXPERTS - 1).astype(jnp.int32)

    def expert_block(args):
        tok, e = args
        xb = xf[tok]
        g = jnp.minimum(xb @ w_gate[e] + b_gate[e], SWIGLU_LIMIT)
        u = jnp.clip(xb @ w_up[e] + b_up[e], -SWIGLU_LIMIT, SWIGLU_LIMIT)
        act = (u + 1.0) * (g * jax.nn.sigmoid(SWIGLU_ALPHA * g))
        return act @ w_down[e] + b_down[e]

    y_blocks = lax.map(expert_block, (tok_buf.reshape(n_blocks, EXPERT_BLOCK), block_e))
    y = y_blocks.reshape(cap, D).astype(jnp.float32) * p_buf[:, None]
    out = jax.ops.segment_sum(y, tok_buf, num_segments=N)
    return out.reshape(B, S, D).astype(h.dtype)


def setup_inputs(seed: int = 0) -> dict:
    key = jax.random.key(seed)
    ks = jax.random.split(key, 32)

    def nrm(k, shape, scale):
        return jax.random.normal(k, shape, jnp.float32) * scale

    a_pow = jax.random.uniform(ks[12], (DEPTH, LRU_WIDTH), jnp.float32, 0.9, 0.999)
    a_base = a_pow ** (1.0 / LRU_C)
    return {
        "x": nrm(ks[0], (BATCH, SEQ, D_MODEL), 1.0),
        "c": nrm(ks[1], (BATCH, D_MODEL), 1.0),
        "ada_w": nrm(ks[2], (DEPTH, D_MODEL, N_MOD * D_MODEL), D_MODEL ** -0.5),
        "ada_b": nrm(ks[3], (DEPTH, N_MOD * D_MODEL), 0.02),
        "mix_norm_g": 1.0 + nrm(ks[4], (DEPTH, D_MODEL), 0.02),
        "w_in": nrm(ks[5], (DEPTH, D_MODEL, D_IN), D_MODEL ** -0.5),
        "conv_w": nrm(ks[6], (DEPTH, CONV_WIDTH, LRU_WIDTH), CONV_WIDTH ** -0.5),
        "conv_b": nrm(ks[7], (DEPTH, LRU_WIDTH), 0.02),
        "rg_w": nrm(ks[8], (DEPTH, LRU_BLOCKS, LRU_BLOCK_DIM, LRU_BLOCK_DIM), LRU_BLOCK_DIM ** -0.5),
        "rg_b": nrm(ks[9], (DEPTH, LRU_WIDTH), 0.02),
        "ig_w": nrm(ks[10], (DEPTH, LRU_BLOCKS, LRU_BLOCK_DIM, LRU_BLOCK_DIM), LRU_BLOCK_DIM ** -0.5),
        "ig_b": nrm(ks[11], (DEPTH, LRU_WIDTH), 0.02),
        "lru_lambda": jnp.log(a_base) - jnp.log1p(-a_base),
        "attn_out_g": 1.0 + nrm(ks[13], (DEPTH, SB_WIDTH), 0.02),
        "lru_out_g": 1.0 + nrm(ks[14], (DEPTH, LRU_WIDTH), 0.02),
        "w_out": nrm(ks[15], (DEPTH, D_MIX, D_MODEL), D_MIX ** -0.5),
        "ffn_norm_g": 1.0 + nrm(ks[16], (DEPTH, D_MODEL), 0.02),
        "router_w": nrm(ks[17], (DEPTH, D_MODEL, N_EXPERTS), D_MODEL ** -0.5),
        "router_b": nrm(ks[18], (DEPTH, N_EXPERTS), 0.01),
        "exp_w_gate": nrm(ks[19], (DEPTH, N_EXPERTS, D_MODEL, D_EXPERT), D_MODEL ** -0.5),
        "exp_b_gate": nrm(ks[20], (DEPTH, N_EXPERTS, D_EXPERT), 0.02),
        "exp_w_up": nrm(ks[21], (DEPTH, N_EXPERTS, D_MODEL, D_EXPERT), D_MODEL ** -0.5),
        "exp_b_up": nrm(ks[22], (DEPTH, N_EXPERTS, D_EXPERT), 0.02),
        "exp_w_down": nrm(ks[23], (DEPTH, N_EXPERTS, D_EXPERT, D_MODEL), D_EXPERT ** -0.5),
        "exp_b_down": nrm(ks[24], (DEPTH, N_EXPERTS, D_MODEL), 0.02),
        "final_norm_g": 1.0 + nrm(ks[25], (D_MODEL,), 0.02),
    }


def reference(x, c, ada_w, ada_b, mix_norm_g, w_in, conv_w, conv_b, rg_w, rg_b, ig_w, ig_b,
              lru_lambda, attn_out_g, lru_out_g, w_out, ffn_norm_g, router_w, router_b,
              exp_w_gate, exp_b_gate, exp_w_up, exp_b_up, exp_w_down, exp_b_down, final_norm_g):
    c_act = jax.nn.silu(c)
    for l in range(DEPTH):
        mod = jnp.einsum('bd,de->be', c_act, ada_w[l]) + ada_b[l]
        sh_m, sc_m, g_m, sh_f, sc_f, g_f = jnp.split(mod, N_MOD, axis=-1)
        h = modulate(rmsnorm(x, mix_norm_g[l]), sh_m, sc_m)
        x = x + g_m[:, None, :] * hybrid_mixer(
            h, w_in[l], conv_w[l], conv_b[l], rg_w[l], rg_b[l], ig_w[l], ig_b[l],
            lru_lambda[l], attn_out_g[l], lru_out_g[l], w_out[l])
        h = modulate(rmsnorm(x, ffn_norm_g[l]), sh_f, sc_f)
        x = x + g_f[:, None, :] * moe_ffn(
            h, router_w[l], router_b[l], exp_w_gate[l], exp_b_gate[l], exp_w_up[l], exp_b_up[l],
            exp_w_down[l], exp_b_down[l])
    return rmsnorm(x, final_norm_g)
```

```python
import contextlib
import numpy as np
import concourse.bass as bass
import concourse.mybir as mybir
from concourse.bass_utils import run_bass_kernel_spmd

F32 = mybir.dt.float32
BF16 = mybir.dt.bfloat16
I32 = mybir.dt.int32
ALU = mybir.AluOpType
AF = mybir.ActivationFunctionType
AX = mybir.AxisListType

S = 4096
D = 1024
NT = S // 128
EPS = 1e-6
BLK = 512
QB = BLK // 128
NBLK = 32 + (S * 4 - 32) // BLK
NBP = 80
NSLOT = NBLK * BLK


class Sig:
    def __init__(self, nc, es, name, lanes=1):
        self.sems = [es.enter_context(nc.semaphore(f"{name}_l{i}")) for i in range(lanes)]
        self.cnt = [0] * lanes
        self.lanes = lanes

    @property
    def sem(self):
        return self.sems[0]

    @property
    def n(self):
        return self.cnt[0]

    def inc(self, ins, k=1, lane=0):
        ins.then_inc(self.sems[lane], k)
        self.cnt[lane] += k
        return self.cnt[lane] if self.lanes == 1 else (lane, self.cnt[lane])

    def dma(self, ins, lane=0):
        return self.inc(ins, 16, lane)


def W(eng, sig, val=None):
    if val is None:
        for l in range(sig.lanes):
            if sig.cnt[l] > 0:
                eng.wait_ge(sig.sems[l], sig.cnt[l])
        return
    if isinstance(val, tuple):
        lane, v = val
    else:
        lane, v = 0, val
    if v > 0:
        eng.wait_ge(sig.sems[lane], v)


def build_program(stop=99, dbg=None):
    nc = bass.Bass("TRN2", target_bir_lowering=False)
    dbg_out = {}

    def din(name, shape, dt=F32):
        return nc.dram_tensor(name, list(shape), dt, kind="ExternalInput").ap()

    x_d = din("x", [S, D])
    ccol_d = din("c_col", [128, 8])
    adaw_d = din("ada_w", [D, 6 * D])
    adab_row_d = din("ada_b_row", [1, 6 * D])
    adab_col_d = din("ada_b_col", [128, 48])
    mixg_d = din("mixg_col", [128, 8])
    win_d = din("w_in", [D, 2560])
    convw_d = din("convw_col", [128, 16])
    lruv_d = din("lru_cols", [128, 16])
    rgw_d = din("rg_w", [8, 64, 64])
    igw_d = din("ig_w", [8, 64, 64])
    outg_d = din("outg_col", [128, 8])
    wout_d = din("w_out", [D, D])
    ffng_d = din("ffn_g_row", [1, D])
    fing_d = din("final_g_row", [1, D])
    rw_d = din("router_w", [D, 32])
    rb_d = din("router_b_row", [1, 32])
    wg_d = din("wg", [4, 4096, 2048])
    wu_d = din("wu", [4, 4096, 2048])
    wd_d = din("wd", [4, 4096, 2048])
    bg_d = din("bg", [4096, 8])
    bu_d = din("bu", [4096, 8])
    bd_d = din("bd", [32, D])
    out_d = nc.dram_tensor("out", [S, D], F32, kind="ExternalOutput").ap()
    X1 = nc.dram_tensor("X1s", [S, D], F32, kind="Internal").ap()
    H2 = nc.dram_tensor("H2s", [S, D], BF16, kind="Internal").ap()
    Yd = nc.dram_tensor("Ys", [NSLOT, D], F32, kind="Internal").ap()
    SLOT = nc.dram_tensor("SLOTs", [NSLOT, 16], I32, kind="Internal").ap()

    def dbg_tensor(name, shape, dt=F32):
        t = nc.dram_tensor("dbg_" + name, list(shape), dt, kind="ExternalOutput").ap()
        dbg_out[name] = t
        return t

    V, A, G, T, SY = nc.vector, nc.scalar, nc.gpsimd, nc.tensor, nc.sync

    with contextlib.ExitStack() as top:
        def sb(es, name, shape, dt=F32):
            return es.enter_context(nc.sbuf_tensor(name, list(shape), dt))

        def ps(es, name, shape, dt=F32):
            return es.enter_context(nc.psum_tensor(name, list(shape), dt))

        sig_i = [0]

        def sig(es, name, lanes=1):
            sig_i[0] += 1
            return Sig(nc, es, f"{name}_{sig_i[0]}", lanes)

        s_out = sig(top, "out")

        ident_bf = sb(top, "ident_bf", [128, 128], BF16)
        ident_f = sb(top, "ident_f", [128, 128], F32)
        ones_f = sb(top, "ones_f", [128, 128], F32)
        ones_bf = sb(top, "ones_bf", [128, 128], BF16)
        negones_bf = sb(top, "negones_bf", [128, 128], BF16)
        negU_bf = sb(top, "negU_bf", [128, 128], BF16)
        tri_bf = sb(top, "tri_bf", [128, 128], BF16)
        zeros_bf = sb(top, "zeros_bf", [128, 512], BF16)
        s_c = sig(top, "const")
        s_cv = sig(top, "constv")
        s_cv.inc(V.memset(ones_f[:], 1.0))
        s_cv.inc(V.memset(ones_bf[:], 1.0))
        s_cv.inc(V.memset(negones_bf[:], -1.0))
        s_cv.inc(V.memset(zeros_bf[:], 0.0))
        W(G, s_cv)
        s_c.inc(G.affine_select(out=ident_f[:], in_=ones_f[:], pattern=[[-1, 128]], compare_op=ALU.is_equal,
                                fill=0.0, base=0, channel_multiplier=1))
        s_c.inc(G.affine_select(out=ident_bf[:], in_=ones_bf[:], pattern=[[-1, 128]], compare_op=ALU.is_equal,
                                fill=0.0, base=0, channel_multiplier=1))
        s_c.inc(G.affine_select(out=negU_bf[:], in_=negones_bf[:], pattern=[[-1, 128]], compare_op=ALU.is_ge,
                                fill=0.0, base=0, channel_multiplier=1))
        s_c.inc(G.affine_select(out=tri_bf[:], in_=ones_bf[:], pattern=[[1, 128]], compare_op=ALU.is_gt,
                                fill=0.0, base=0, channel_multiplier=-1))
        for e in (V, A, T, SY):
            W(e, s_c)
        W(G, s_c)

        modrow = sb(top, "modrow", [128, 4 * D])
        modcol = sb(top, "modcol", [128, 16])
        Am = sb(top, "Am", [128, 8])
        fing_row = sb(top, "fing_row", [128, D])
        gm_row = modrow[:, 0:D]
        shf_row = modrow[:, D:2 * D]
        Af_row = modrow[:, 2 * D:3 * D]
        gf_row = modrow[:, 3 * D:4 * D]
        Bm = modcol[:, 0:8]

        ssall = sb(top, "ssall", [128, 32])
        rstdall = sb(top, "rstdall", [128, 32])
        with contextlib.ExitStack() as es:
            xs = [sb(es, f"xs{i}", [128, D]) for i in range(4)]
            junk = sb(es, "junk0", [128, D], BF16)
            s_x = sig(es, "0x", 4)
            s_a = sig(es, "0a")
            s_v0 = sig(es, "0v")
            vxs = {}
            vas = {}
            for tt in range(NT):
                if tt >= 4:
                    W(SY, s_a, vas[tt - 4])
                vxs[tt] = s_x.dma(SY.dma_start(out=xs[tt % 4][:], in_=x_d[tt * 128:(tt + 1) * 128, :]), lane=tt % 4)
                W(A, s_x, vxs[tt])
                vas[tt] = s_a.inc(A.activation(out=junk[:], in_=xs[tt % 4][:], func=AF.Square, accum_out=ssall[:, tt:tt + 1]))
            W(A, s_a)
            va = s_a.inc(A.activation(out=ssall[:], in_=ssall[:], func=AF.Sqrt, scale=1.0 / D, bias=EPS))
            W(V, s_a, va)
            s_v0.inc(V.reciprocal(out=rstdall[:], in_=ssall[:]))
            barrier_early = [s_x, s_a, s_v0]
            for e in (V, A, T, SY, G):
                for s_ in barrier_early:
                    W(e, s_)

        with contextlib.ExitStack() as es:
            ccol = sb(es, "ccol", [128, 8])
            scol = sb(es, "scol", [128, 8])
            scb = sb(es, "scb", [128, 8, 128], BF16)
            scol_b = sb(es, "scol_b", [128, 8], BF16)
            adab_col = sb(es, "adab_col", [128, 48])
            mixg = sb(es, "mixg", [128, 8])
            ffng_row = sb(es, "ffng_row", [128, D])
            adab_row = sb(es, "adab_row", [128, 512])
            aw = [sb(es, f"aw{i}", [128, 8, 512], BF16) for i in range(2)]
            pr = [ps(es, f"p0r{i}", [128, 512]) for i in range(2)]
            pc = ps(es, "p0c", [128, 16])
            s_ld = sig(es, "p0ld")
            s_aw = sig(es, "p0aw", 2)
            s_awfree = sig(es, "p0awf")
            s_a = sig(es, "p0a")
            s_v = sig(es, "p0v")
            s_t = sig(es, "p0t")
            s_b = sig(es, "p0b")
            s_ld.dma(SY.dma_start(out=ccol[:], in_=ccol_d))
            s_ld.dma(SY.dma_start(out=adab_col[:], in_=adab_col_d))
            s_ld.dma(SY.dma_start(out=mixg[:], in_=mixg_d))
            s_ld.dma(SY.dma_start(out=ffng_row[:], in_=ffng_d.broadcast_to([128, D])))
            s_ld.dma(SY.dma_start(out=fing_row[:], in_=fing_d.broadcast_to([128, D])))
            W(A, s_ld)
            s_a.inc(A.activation(out=scol[:], in_=ccol[:], func=AF.Silu))
            W(V, s_a)
            W(V, s_ld)
            for k in range(8):
                s_v.inc(V.tensor_scalar(out=scb[:, k, :], in0=ones_f[:], scalar1=scol[:, k:k + 1], scalar2=None,
                                        op0=ALU.mult))
            s_v.inc(V.tensor_copy(out=scol_b[:], in_=scol[:]))
            aw_view = adaw_d.rearrange("(k p) n -> p k n", p=128)
            W(T, s_v)
            tdone = {}
            vev = {}
            for n in range(12):
                b = n % 2
                if n >= 2:
                    W(G, s_t, tdone[n - 2])
                v_aw = s_aw.dma(G.dma_start(out=aw[b][:], in_=aw_view[:, :, n * 512:(n + 1) * 512]), lane=b)
                W(T, s_aw, v_aw)
                if n < 4:
                    for fcl in range(4):
                        fc = n * 4 + fcl
                        for k in range(8):
                            ins = T.matmul(pc[:, fc:fc + 1], lhsT=aw[b][:, k, fcl * 128:(fcl + 1) * 128],
                                           rhs=scol_b[:, k:k + 1], start=(k == 0), stop=(k == 7))
                    tdone[n] = s_t.inc(ins)
                else:
                    if n - 2 >= 4:
                        W(T, s_v, vev[n - 2])
                    for k in range(8):
                        ins = T.matmul(pr[b][:], lhsT=scb[:, k, :], rhs=aw[b][:, k, :], start=(k == 0), stop=(k == 7))
                    tdone[n] = s_t.inc(ins)
                    if n - 1 >= 4:
                        W(SY, s_v, vev[n - 1])
                    v_b = s_b.dma(SY.dma_start(out=adab_row[:], in_=adab_row_d[:, n * 512:(n + 1) * 512]
                                               .broadcast_to([128, 512])))
                    W(V, s_b, v_b)
                    W(V, s_t, tdone[n])
                    vev[n] = s_v.inc(V.tensor_tensor(out=modrow[:, (n - 4) * 512:(n - 3) * 512], in0=pr[b][:],
                                                     in1=adab_row[:], op=ALU.add))
            W(V, s_t)
            s_v.inc(V.tensor_tensor(out=modcol[:], in0=pc[:], in1=adab_col[:, 0:16], op=ALU.add))
            W(V, s_v)
            s_v.inc(V.scalar_tensor_tensor(out=Am[:], in0=modcol[:, 8:16], scalar=1.0, in1=mixg[:], op0=ALU.add,
                                           op1=ALU.mult))
            s_v.inc(V.scalar_tensor_tensor(out=modrow[:, 2 * D:3 * D], in0=modrow[:, 2 * D:3 * D], scalar=1.0,
                                           in1=ffng_row[:], op0=ALU.add, op1=ALU.mult))
            for e in (A, T, SY, G):
                W(e, s_v)
            W(V, s_v)
            if dbg:
                d1 = dbg_tensor("modrow", [128, 4 * D])
                d2 = dbg_tensor("modcol", [128, 16])
                s_out.dma(SY.dma_start(out=d1, in_=modrow[:]))
                s_out.dma(SY.dma_start(out=d2, in_=modcol[:]))

        if stop <= 0:
            W(SY, s_out)
            return nc, dbg_out

        RA = sb(top, "RA", [128, 49152], BF16)
        RB = sb(top, "RB", [128, 16384], BF16)
        qT = RA[:, 0:16384].rearrange("p (c t) -> p c t", c=4)
        kT = RA[:, 16384:32768].rearrange("p (c t) -> p c t", c=4)
        vtok = RA[:, 32768:49152].rearrange("p (n f) -> p n f", n=32)
        yattnT = RB[:, :].rearrange("p (c t) -> p c t", c=4)
        cnt = {"tile": 0}

        def emit_norm_transpose(es, tiles, hT_of, xn_bufs, pT, xt, sg, hist):
            s_x, s_act, s_v, s_pe = sg
            st = hist
            base = len(st)

            def stage1(jl, tt):
                j = base + jl
                b = j % 2
                if j - 2 in st:
                    W(SY, s_v, st[j - 2]["xn"])
                vx = s_x.dma(SY.dma_start(out=xt[b][:], in_=x_d[tt * 128:(tt + 1) * 128, :]), lane=b)
                W(V, s_x, vx)
                if j - 2 in st:
                    W(V, s_pe, st[j - 2]["tr"])
                v3 = s_v.inc(V.tensor_scalar(out=xn_bufs[b], in0=xt[b][:], scalar1=rstdall[:, tt:tt + 1], scalar2=None,
                                             op0=ALU.mult))
                W(T, s_v, v3)
                if j - 2 in st:
                    W(T, s_v, st[j - 2]["ev"])
                for k in range(8):
                    ins = T.transpose(pT[b][:, k, :], xn_bufs[b][:, k * 128:(k + 1) * 128], ident_bf[:])
                st[j] = {"xn": v3, "tr": s_pe.inc(ins)}

            def stage2(jl):
                j = base + jl
                b = j % 2
                W(V, s_pe, st[j]["tr"])
                dst = hT_of(jl)
                for k in range(8):
                    ins = V.tensor_scalar(out=dst[:, k, :], in0=pT[b][:, k, :], scalar1=Am[:, k:k + 1],
                                          scalar2=Bm[:, k:k + 1], op0=ALU.mult, op1=ALU.add)
                st[j]["ev"] = s_v.inc(ins)

            for jl, tt in enumerate(tiles):
                stage1(jl, tt)
                if jl >= 1:
                    stage2(jl - 1)
            stage2(len(tiles) - 1)
            return st[base + len(tiles) - 1]["ev"]

        with contextlib.ExitStack() as es:
            win = RB[:, 0:12288].rearrange("p (k n) -> p k n", k=8)
            xn_bufs = [RB[:, 12288 + i * 1024:12288 + (i + 1) * 1024] for i in range(2)]
            hT = [sb(es, f"hT{i}", [128, 8, 1024], BF16) for i in range(2)]
            xt = [sb(es, f"xt{i}", [128, D]) for i in range(2)]
            pT = [ps(es, f"pT{i}", [128, 8, 128], BF16) for i in range(2)]
            pj = [ps(es, f"pj{i}", [128, 512]) for i in range(4)]
            s_w = sig(es, "p1w")
            sg = (sig(es, "p1x", 2), sig(es, "p1a"), sig(es, "p1v"), sig(es, "p1t"))
            s_x, s_act, s_v, s_pe = sg
            s_ea = sig(es, "p1ea")
            s_ev = sig(es, "p1ev")
            wv = win_d.rearrange("(k p) n -> p k n", p=128)
            for kh in range(2):
                s_w.dma(G.dma_start(out=win[:, kh * 4:(kh + 1) * 4, :], in_=wv[:, kh * 4:(kh + 1) * 4, 0:1536]))
            W(T, s_w)
            gi = 0
            ev_hist = {}
            hist1 = {}
            hT_last_pe = {}
            vh_q = {}

            def emit_nt(q):
                if q >= 2:
                    W(V, s_pe, hT_last_pe[q - 2])
                hq_ = hT[q % 2]
                vh_q[q] = emit_norm_transpose(es, list(range(q * 8, q * 8 + 8)),
                                              lambda j, hq_=hq_: hq_[:, :, j * 128:(j + 1) * 128], xn_bufs, pT, xt, sg, hist1)

            emit_nt(0)
            for q in range(4):
                hq = hT[q % 2]
                if q + 1 < 4:
                    emit_nt(q + 1)
                W(T, s_v, vh_q[q])
                groups = [("qk", ec, th) for ec in range(8) for th in range(2)] + [("v", tt, 0) for tt in range(8)]
                for (kind, a_, b_) in groups:
                    pb = pj[gi % 4]
                    if gi >= 4:
                        sg_, val_ = ev_hist[gi - 4]
                        W(T, sg_, val_)
                    for k in range(8):
                        if kind == "qk":
                            ins = T.matmul(pb[:], lhsT=win[:, k, a_ * 128:(a_ + 1) * 128],
                                           rhs=hq[:, k, b_ * 512:(b_ + 1) * 512], start=(k == 0), stop=(k == 7))
                        else:
                            ins = T.matmul(pb[:], lhsT=hq[:, k, a_ * 128:(a_ + 1) * 128], rhs=win[:, k, 1024:1536],
                                           start=(k == 0), stop=(k == 7))
                    vt = s_pe.inc(ins)
                    hT_last_pe[q] = vt
                    if kind == "qk":
                        tok0 = q * 1024 + b_ * 512
                        if a_ < 4:
                            dst = qT[:, a_, tok0:tok0 + 512]
                            scale = 0.125
                        else:
                            dst = kT[:, a_ - 4, tok0:tok0 + 512]
                            scale = 1.0
                    else:
                        dst = vtok[:, q * 8 + a_, :]
                        scale = 1.0
                    if gi % 2 == 0:
                        W(A, s_pe, vt)
                        ev_hist[gi] = (s_ea, s_ea.inc(A.activation(out=dst, in_=pb[:], func=AF.Copy, scale=scale)))
                    else:
                        W(V, s_pe, vt)
                        ev_hist[gi] = (s_ev, s_ev.inc(V.tensor_scalar(out=dst, in0=pb[:], scalar1=scale, scalar2=None,
                                                                      op0=ALU.mult)))
                    gi += 1
            for e in (A, V, T, G, SY):
                W(e, s_ea)
                W(e, s_ev)
                W(e, s_pe)
            if dbg and stop == 1:
                dq = dbg_tensor("qkv", [128, 49152], BF16)
                s_out.dma(SY.dma_start(out=dq, in_=RA[:, :]))
                d2 = dbg_tensor("rstd", [128, 32])
                s_out.dma(SY.dma_start(out=d2, in_=rstdall[:, :]))
                d3 = dbg_tensor("hT1", [128, 8192], BF16)
                s_out.dma(SY.dma_start(out=d3, in_=hT[1][:, :, :]))
                d4 = dbg_tensor("Am", [128, 8])
                s_out.dma(SY.dma_start(out=d4, in_=Am[:, :]))
        if stop <= 1:
            W(SY, s_out)
            return nc, dbg_out

        with contextlib.ExitStack() as es:
            Sbig = sb(es, "Sbig", [128, 32, 512], BF16)
            w_t = [sb(es, f"w_t{i}", [128, 512], BF16) for i in range(2)]
            R_t = [sb(es, f"R_t{i}", [128, 512], BF16) for i in range(2)]
            mask = [sb(es, f"mask{i}", [128, 512], BF16) for i in range(4)]
            pz = [ps(es, f"pz{i}", [128, 512]) for i in range(2)]
            pw = [ps(es, f"pw{i}", [128, 512]) for i in range(3)]
            po = [ps(es, f"po{i}", [128, 512]) for i in range(2)]
            s_pe = sig(es, "a_pe")
            s_act = sig(es, "a_act")
            s_pool = sig(es, "a_pool")
            s_v = sig(es, "a_v")
            for rel in range(4):
                s_pool.inc(G.affine_select(out=mask[rel][:], in_=zeros_bf[:], pattern=[[1, 512]], compare_op=ALU.is_gt,
                                           fill=-30000.0, base=-128 * rel, channel_multiplier=-1))
            W(T, s_pool)
            pairs = []
            groups = []
            for h in range(8):
                for c in range(8):
                    g = len(groups)
                    kbs = list(range(4 * c + 3, -1, -1))
                    groups.append((h, c, len(pairs), len(pairs) + len(kbs) - 1))
                    for kb in kbs:
                        pairs.append((h, c, kb, g))
            n = len(pairs)
            vA, vB, vC, vD, vE, vF, vG = {}, {}, {}, {}, {}, {}, {}
            gev = {}

            def ops(j):
                h, c, kb, g = pairs[j]
                hp = (h % 2) * 64
                kh = kT[hp:hp + 64, h // 2, kb * 128:(kb + 1) * 128]
                qh = qT[hp:hp + 64, h // 2, c * 512:(c + 1) * 512]
                rel = kb - 4 * c
                first = (j == groups[g][2])
                last = (j == groups[g][3])
                return h, c, kb, g, hp, kh, qh, rel, first, last

            def colsl(j):
                rel = pairs[j][2] - 4 * pairs[j][1]
                return slice(128 * rel, 512) if rel >= 1 else slice(0, 512)

            def zmm(out, j, stop_):
                h, c, kb, g, hp, kh, qh, rel, first, last = ops(j)
                cs = colsl(j)
                if rel >= 0:
                    T.matmul(out[:, cs], lhsT=kh, rhs=qh[:, cs], start=True, stop=False)
                    return T.matmul(out[:, cs], lhsT=ident_bf[:], rhs=mask[rel][:, cs], start=False, stop=stop_)
                return T.matmul(out[:, cs], lhsT=kh, rhs=qh[:, cs], start=True, stop=stop_)

            def st_A(j):
                if j - 2 >= 0:
                    W(T, s_act, vC[j - 2])
                vA[j] = s_pe.inc(zmm(pz[j % 2], j, True))

            def Sslot(j):
                g = pairs[j][3]
                return Sbig[:, j - groups[g][2], :]

            def st_C(j):
                g = pairs[j][3]
                jl = j - groups[g][2]
                W(A, s_pe, vA[j])
                if g >= 1:
                    jp = groups[g - 1][2] + jl
                    if jp <= groups[g - 1][3]:
                        W(A, s_pe, vD[jp])
                        if jp in vE:
                            W(A, s_pool, vE[jp])
                cs = colsl(j)
                vC[j] = s_act.inc(A.activation(out=Sslot(j)[:, cs], in_=pz[j % 2][:, cs], func=AF.Softplus))

            def st_E(j):
                h, c, kb, g, hp, kh, qh, rel, first, last = ops(j)
                if last:
                    return
                cs = colsl(j)
                W(G, s_act, vC[j])
                if j - 1 >= 0:
                    W(G, s_pe, vD[j - 1])
                Rn = R_t[(j + 1) % 2]
                if cs.start > 0:
                    s_pool.inc(G.memset(Rn[:, 0:cs.start], 0.0))
                if first:
                    vE[j] = s_pool.inc(G.tensor_copy(out=Rn[:, cs], in_=Sslot(j)[:, cs]))
                else:
                    W(G, s_pool, vE[j - 1])
                    vE[j] = s_pool.inc(G.tensor_tensor(out=Rn[:, cs], in0=R_t[j % 2][:, cs], in1=Sslot(j)[:, cs],
                                                       op=ALU.add))

            def st_D(j):
                h, c, kb, g, hp, kh, qh, rel, first, last = ops(j)
                W(T, s_act, vC[j])
                if j - 3 >= 0:
                    W(T, s_act, vF[j - 3])
                if not first:
                    W(T, s_pool, vE[j - 1])
                cs = colsl(j)
                zmm(pw[j % 3], j, False)
                ins = T.matmul(pw[j % 3][:, cs], lhsT=negU_bf[:], rhs=Sslot(j)[:, cs], start=False, stop=first)
                if not first:
                    ins = T.matmul(pw[j % 3][:, cs], lhsT=negones_bf[:], rhs=R_t[j % 2][:, cs], start=False, stop=True)
                vD[j] = s_pe.inc(ins)

            def st_F(j):
                W(A, s_pe, vD[j])
                if j - 2 >= 0:
                    W(A, s_pe, vG[j - 2])
                cs = colsl(j)
                vF[j] = s_act.inc(A.activation(out=w_t[j % 2][:, cs], in_=pw[j % 3][:, cs], func=AF.Exp))

            def st_G(j):
                h, c, kb, g, hp, kh, qh, rel, first, last = ops(j)
                W(T, s_act, vF[j])
                if first and g - 2 >= 0:
                    W(T, s_v, gev[g - 2])
                cs = colsl(j)
                if first:
                    T.matmul(po[g % 2][hp:hp + 64, :], lhsT=zeros_bf[:, 0:64], rhs=mask[0][:], start=True, stop=False)
                ins = T.matmul(po[g % 2][hp:hp + 64, cs], lhsT=vtok[:, kb, h * 64:(h + 1) * 64], rhs=w_t[j % 2][:, cs],
                               start=False, stop=last)
                vG[j] = s_pe.inc(ins)
                if last:
                    W(V, s_pe, vG[j])
                    gev[g] = s_v.inc(V.tensor_copy(out=yattnT[hp:hp + 64, h // 2, c * 512:(c + 1) * 512],
                                                   in_=po[g % 2][hp:hp + 64, :]))

            st_A(0)
            if n > 1:
                st_A(1)
            for (h_, c_, j0, j1) in groups:
                for j in range(j0, j1 + 1):
                    st_C(j)
                    if j + 2 < n:
                        st_A(j + 2)
                ng = j1 - j0 + 1
                for idx in range(ng + 2):
                    j = j0 + idx
                    if idx < ng:
                        st_D(j)
                        st_E(j)
                    if 0 <= idx - 1 < ng:
                        st_F(j - 1)
                    if 0 <= idx - 2 < ng:
                        st_G(j - 2)
            for e in (A, V, T, G, SY):
                W(e, s_v)
                W(e, s_pe)
                W(e, s_act)
                W(e, s_pool)
            if dbg and stop == 2:
                dq = dbg_tensor("yattn", [128, 16384], BF16)
                s_out.dma(SY.dma_start(out=dq, in_=RB[:, :]))
        if stop <= 2:
            W(SY, s_out)
            return nc, dbg_out

        ENG = (V, A, G, T, SY)

        def barrier(sigs, engines=ENG):
            for e in engines:
                for s_ in sigs:
                    W(e, s_)

        ylruT = RA[:, 0:16384].rearrange("p (c t) -> p c t", c=4)
        with contextlib.ExitStack() as es:
            win = RA[:, 16384:24576].rearrange("p (k n) -> p k n", k=8)
            hT = [RA[:, 24576 + i * 8192:24576 + (i + 1) * 8192].rearrange("p (k t) -> p k t", k=8) for i in range(2)]
            xn_bufs = [RA[:, 40960 + i * 1024:40960 + (i + 1) * 1024] for i in range(2)]
            xt = [sb(es, f"xtb{i}", [128, D]) for i in range(2)]
            HW_ = 512
            xpad = [sb(es, f"xpad{i}", [128, HW_ + 3]) for i in range(2)]
            xc = [sb(es, f"xc{i}", [128, HW_]) for i in range(2)]
            r_t = [sb(es, f"r_t{i}", [128, HW_]) for i in range(2)]
            ig_t = [sb(es, f"ig_t{i}", [128, HW_]) for i in range(2)]
            b_t = [sb(es, f"b_t{i}", [128, HW_]) for i in range(2)]
            h_t = [sb(es, f"h_t{i}", [128, HW_]) for i in range(2)]
            gg = [sb(es, f"gg{i}", [128, HW_]) for i in range(2)]
            g2 = [sb(es, f"g2{i}", [128, HW_]) for i in range(2)]
            xcb = [sb(es, f"xcb{i}", [128, HW_], BF16) for i in range(2)]
            rgbd_b = sb(es, "rgbd_b", [128, 4, 128], BF16)
            igbd_b = sb(es, "igbd_b", [128, 4, 128], BF16)
            rgbd = sb(es, "rgbd", [128, 4, 128])
            igbd = sb(es, "igbd", [128, 4, 128])
            convw = sb(es, "convw", [128, 16])
            lruv = sb(es, "lruv", [128, 16])
            cA = sb(es, "cA", [128, 4])
            tmp4 = sb(es, "tmp4", [128, 4])
            hlast = sb(es, "hlast", [128, 4])
            xtail = sb(es, "xtail", [128, 4, 3])
            pT = [ps(es, f"pTb{i}", [128, 8, 128], BF16) for i in range(2)]
            pxr = [ps(es, f"pxr{i}", [128, HW_]) for i in range(2)]
            pgr = [ps(es, f"pgr{i}", [128, HW_]) for i in range(2)]
            pgt = [ps(es, f"pgt{i}", [128, HW_]) for i in range(2)]
            s_w = sig(es, "lw")
            sg = (sig(es, "lx", 2), sig(es, "la"), sig(es, "lv"), sig(es, "lt"))
            s_x, s_act, s_v, s_pe = sg
            s_g = sig(es, "lg")
            allsig = [s_w, s_x, s_act, s_v, s_pe, s_g]

            def v(ins):
                val = s_v.inc(ins); W(V, s_v, val); return val

            def a(ins):
                val = s_act.inc(ins); W(A, s_act, val); return val

            def g(ins):
                val = s_g.inc(ins); W(G, s_g, val); return val

            wv = win_d.rearrange("(k p) n -> p k n", p=128)
            for kh in range(2):
                s_w.dma(G.dma_start(out=win[:, kh * 4:(kh + 1) * 4, :], in_=wv[:, kh * 4:(kh + 1) * 4, 1536:2560]))
            v(V.memset(rgbd[:], 0.0))
            v(V.memset(igbd[:], 0.0))
            v(V.memset(hlast[:], 0.0))
            v(V.memset(xtail[:], 0.0))
            W(SY, s_v)
            for cc in range(4):
                for hh in range(2):
                    s_w.dma(SY.dma_start(out=rgbd[hh * 64:(hh + 1) * 64, cc, hh * 64:(hh + 1) * 64], in_=rgw_d[2 * cc + hh]))
                    s_w.dma(SY.dma_start(out=igbd[hh * 64:(hh + 1) * 64, cc, hh * 64:(hh + 1) * 64], in_=igw_d[2 * cc + hh]))
            s_w.dma(SY.dma_start(out=convw[:], in_=convw_d))
            s_w.dma(SY.dma_start(out=lruv[:], in_=lruv_d))
            barrier([s_w])
            v(V.tensor_copy(out=rgbd_b[:], in_=rgbd[:]))
            v(V.tensor_copy(out=igbd_b[:], in_=igbd[:]))
            lam = lruv[:, :].rearrange("p (c f) -> p c f", f=4)[:, :, 3]
            a(A.activation(out=tmp4[:], in_=lam, func=AF.Exp, scale=-1.0))
            a(A.activation(out=tmp4[:], in_=tmp4[:], func=AF.Ln, bias=1.0, scale=1.0))
            W(V, s_act)
            v(V.tensor_scalar(out=cA[:], in0=tmp4[:], scalar1=-8.0, scalar2=None, op0=ALU.mult))
            barrier(allsig)
            hist2 = {}
            un = {}

            def S1(u, q, cc, hf, hq):
                p = u % 2
                U = un[u] = {}
                cw = lambda j: convw[:, cc * 4 + j:cc * 4 + j + 1]
                lv = lambda j: lruv[:, cc * 4 + j:cc * 4 + j + 1]
                tsl = slice(hf * HW_, (hf + 1) * HW_)
                if u - 2 in un:
                    P_ = un[u - 2]
                    for e in (T, V, A, G):
                        W(e, s_v, P_["v_end"])
                        W(e, s_act, P_["a_end"])
                        W(e, s_pe, P_["t_end"])
                        W(e, s_g, P_["g_end"])
                for k in range(8):
                    T.matmul(pxr[p][:], lhsT=win[:, k, cc * 128:(cc + 1) * 128], rhs=hq[:, k, tsl], start=(k == 0), stop=(k == 7))
                for k in range(8):
                    ins = T.matmul(pgr[p][:], lhsT=win[:, k, 512 + cc * 128:512 + (cc + 1) * 128], rhs=hq[:, k, tsl],
                                   start=(k == 0), stop=(k == 7))
                vt = s_pe.inc(ins)
                W(V, s_pe, vt)
                W(A, s_pe, vt)
                v(V.tensor_copy(out=xpad[p][:, 0:3], in_=xtail[:, cc, :]))
                v(V.tensor_copy(out=xpad[p][:, 3:HW_ + 3], in_=pxr[p][:]))
                v(V.tensor_copy(out=xtail[:, cc, :], in_=xpad[p][:, HW_:HW_ + 3]))
                v(V.tensor_scalar(out=xc[p][:], in0=xpad[p][:, 0:HW_], scalar1=cw(0), scalar2=lv(0), op0=ALU.mult, op1=ALU.add))
                for j in range(1, 4):
                    vxc = v(V.scalar_tensor_tensor(out=xc[p][:], in0=xpad[p][:, j:j + HW_], scalar=cw(j), in1=xc[p][:],
                                                   op0=ALU.mult, op1=ALU.add))
                va = a(A.activation(out=gg[p][:], in_=pgr[p][:], func=AF.Identity))
                W(G, s_v, vxc)
                vxb = g(G.tensor_copy(out=xcb[p][:], in_=xc[p][:]))
                W(T, s_g, vxb)
                W(G, s_act, va)
                g(G.tensor_tensor(out=g2[p][:], in0=gg[p][:], in1=gg[p][:], op=ALU.mult))
                g(G.tensor_scalar(out=g2[p][:], in0=g2[p][:], scalar1=0.044715, scalar2=1.0, op0=ALU.mult, op1=ALU.add))
                vg = g(G.tensor_tensor(out=g2[p][:], in0=g2[p][:], in1=gg[p][:], op=ALU.mult))
                ins = T.matmul(pgt[p][:], lhsT=rgbd_b[:, cc, :], rhs=xcb[p][:], start=True, stop=True)
                vt = s_pe.inc(ins)
                W(A, s_pe, vt)
                va = a(A.activation(out=r_t[p][:], in_=pgt[p][:], func=AF.Sigmoid, bias=lv(1), scale=1.0))
                W(T, s_act, va)
                ins = T.matmul(pgt[p][:], lhsT=igbd_b[:, cc, :], rhs=xcb[p][:], start=True, stop=True)
                vt = s_pe.inc(ins)
                W(A, s_pe, vt)
                a(A.activation(out=ig_t[p][:], in_=pgt[p][:], func=AF.Sigmoid, bias=lv(2), scale=1.0))
                W(A, s_g, vg)
                a(A.activation(out=g2[p][:], in_=g2[p][:], func=AF.Sigmoid, scale=1.5957691216))
                va = a(A.activation(out=r_t[p][:], in_=r_t[p][:], func=AF.Exp, scale=cA[:, cc:cc + 1]))
                U["a"] = va
                U["t_end"] = vt
                U["g_end"] = vg
                U["cc"] = cc
                U["q"] = q
                U["hf"] = hf

            def S2a(u):
                p = u % 2
                U = un[u]
                cc, q, hf = U["cc"], U["q"], U["hf"]
                W(V, s_act, U["a"])
                v(V.tensor_tensor(out=b_t[p][:], in0=r_t[p][:], in1=r_t[p][:], op=ALU.mult))
                v(V.tensor_scalar(out=b_t[p][:], in0=b_t[p][:], scalar1=-1.0, scalar2=1.0, op0=ALU.mult, op1=ALU.add))
                vb = v(V.tensor_scalar(out=b_t[p][:], in0=b_t[p][:], scalar1=1e-30, scalar2=None, op0=ALU.max))
                W(A, s_v, vb)
                va = a(A.activation(out=b_t[p][:], in_=b_t[p][:], func=AF.Sqrt))
                U["a_end"] = va

            def S2b(u):
                p = u % 2
                U = un[u]
                cc, q, hf = U["cc"], U["q"], U["hf"]
                W(V, s_act, U["a_end"])
                v(V.tensor_tensor(out=b_t[p][:], in0=b_t[p][:], in1=ig_t[p][:], op=ALU.mult))
                v(V.tensor_tensor(out=b_t[p][:], in0=b_t[p][:], in1=xc[p][:], op=ALU.mult))
                v(V.tensor_tensor_scan(out=h_t[p][:], data0=r_t[p][:], data1=b_t[p][:], initial=hlast[:, cc:cc + 1],
                                       op0=ALU.mult, op1=ALU.add))
                v(V.tensor_copy(out=hlast[:, cc:cc + 1], in_=h_t[p][:, HW_ - 1:HW_]))
                v(V.tensor_tensor(out=g2[p][:], in0=g2[p][:], in1=gg[p][:], op=ALU.mult))
                t0 = q * 1024 + hf * HW_
                U["v_end"] = v(V.tensor_tensor(out=ylruT[:, cc, t0:t0 + HW_], in0=g2[p][:], in1=h_t[p][:], op=ALU.mult))

            u = 0
            for q in range(4):
                hq = hT[q % 2]
                v_h = emit_norm_transpose(es, list(range(q * 8, q * 8 + 8)),
                                          lambda j, hq=hq: hq[:, :, j * 128:(j + 1) * 128], xn_bufs, pT, xt, sg, hist2)
                barrier(allsig)
                first = u
                for cc in range(4):
                    for hf in range(2):
                        if u - 1 >= first:
                            S2a(u - 1)
                        S1(u, q, cc, hf, hq)
                        if u - 1 >= first:
                            S2b(u - 1)
                        u += 1
                S2a(u - 1)
                S2b(u - 1)
                barrier(allsig)
            if dbg and stop == 3:
                dq = dbg_tensor("ylru", [128, 16384], BF16)
                s_out.dma(SY.dma_start(out=dq, in_=RA[:, 0:16384]))
                d2 = dbg_tensor("rstd3", [128, 32])
                s_out.dma(SY.dma_start(out=d2, in_=rstdall[:, :]))
                d3 = dbg_tensor("hT23", [128, 16384], BF16)
                s_out.dma(SY.dma_start(out=d3, in_=RA[:, 24576:40960]))
        if stop <= 3:
            W(SY, s_out)
            return nc, dbg_out

        M8all = sb(top, "M8all", [128, 32, 8])
        Lall = sb(top, "Lall", [128, 32, 32])
        rank_all = sb(top, "rank_all", [128, 32, 32])
        cnt_run = sb(top, "cnt_run", [128, 32])
        dest4f = sb(top, "dest4f", [128, 32, 4])
        dest4i = sb(top, "dest4i", [128, 128], I32)
        p4 = sb(top, "p4", [128, 32, 4])
        widx = sb(top, "widx", [128, NBP], I32)
        bidx = sb(top, "bidx", [128, NBP], I32)

        with contextlib.ExitStack() as es:
            wout = RA[:, 16384:24576].rearrange("p (k n) -> p k n", k=8)
            xt = [sb(es, f"x3_{i}", [128, D]) for i in range(2)]
            x1 = [sb(es, f"x1_{i}", [128, D]) for i in range(2)]
            t1 = sb(es, "t1", [128, D])
            h2 = sb(es, "h2", [128, D])
            h2b = [sb(es, f"h2b{i}", [128, D], BF16) for i in range(2)]
            h2T = sb(es, "h2T", [128, 8, 128])
            ysq = sb(es, "ysq", [128, 8, 128], BF16)
            rw = sb(es, "rw", [128, 8, 32])
            rb_row = sb(es, "rb_row", [128, 32])
            outg = sb(es, "outg", [128, 8])
            sd2 = sb(es, "sd2", [128, 2])
            rs2 = sb(es, "rs2", [128, 2])
            ss3 = sb(es, "ss3", [128, 1])
            rs3 = sb(es, "rs3", [128, 1])
            Mf = sb(es, "Mf", [128, 32])
            Mbf = sb(es, "Mbf", [128, 32], BF16)
            PA = [ps(es, f"PA{i}", [128, 512]) for i in range(2)]
            PL = [ps(es, f"PL{i}", [128, 512]) for i in range(2)]
            ptr = ps(es, "ptr", [128, 4, 128])
            psm = ps(es, "psm", [128, 512])
            psl = ps(es, "psl", [128, 512])
            psr = ps(es, "psr", [128, 512])
            s_d = sig(es, "3d", 2)
            s_v = sig(es, "3v")
            s_act = sig(es, "3a")
            s_pe = sig(es, "3t")
            s_st = sig(es, "3st", 4)
            allsig = [s_d, s_v, s_act, s_pe, s_st]

            def v(ins):
                val = s_v.inc(ins); W(V, s_v, val); return val

            def a(ins):
                val = s_act.inc(ins); W(A, s_act, val); return val

            s_d.dma(SY.dma_start(out=rw[:], in_=rw_d.rearrange("(k p) n -> p k n", p=128)))
            s_d.dma(SY.dma_start(out=rb_row[:], in_=rb_d.broadcast_to([128, 32])))
            s_d.dma(SY.dma_start(out=outg[:], in_=outg_d))
            v(V.memset(cnt_run[:], 0.0))
            wov = wout_d.rearrange("(k p) n -> p k n", p=128)
            for k in range(8):
                vd = s_d.dma(SY.dma_start(out=t1[:], in_=wov[:, k, :]))
                W(V, s_d, vd)
                vv = v(V.tensor_scalar(out=wout[:, k, :], in0=t1[:], scalar1=outg[:, k:k + 1], scalar2=None, op0=ALU.mult))
                W(SY, s_v, vv)
            barrier(allsig)
            s_g = sig(es, "3g")
            allsig.append(s_g)
            st3 = {tt: {} for tt in range(NT)}

            def vv(ins):
                return s_v.inc(ins)

            def stA(tt):
                b = tt % 2
                S_ = st3[tt]
                tsl = slice(tt * 128, (tt + 1) * 128)
                if tt - 2 >= 0:
                    W(SY, s_g, st3[tt - 2]["x1"])
                S_["x"] = s_d.dma(SY.dma_start(out=xt[b][:], in_=x_d[tsl, :]), lane=b)
                if tt - 1 >= 0:
                    W(G, s_pe, st3[tt - 1]["stats"])
                s_g.inc(G.tensor_tensor(out=ysq[:, 0:4, :], in0=yattnT[:, :, tsl], in1=yattnT[:, :, tsl], op=ALU.mult))
                vq = s_g.inc(G.tensor_tensor(out=ysq[:, 4:8, :], in0=ylruT[:, :, tsl], in1=ylruT[:, :, tsl], op=ALU.mult))
                W(T, s_g, vq)
                if tt - 1 >= 0:
                    W(T, s_act, st3[tt - 1]["sd2"])
                for gI in range(2):
                    for c in range(4):
                        ins = T.matmul(psm[:, gI:gI + 1], lhsT=ysq[:, gI * 4 + c, :], rhs=ones_bf[:, 0:1], start=(c == 0),
                                       stop=(c == 3))
                S_["stats"] = s_pe.inc(ins)
                if tt - 1 >= 0:
                    W(T, s_v, st3[tt - 1]["comb"])
                for dh in range(2):
                    for c in range(4):
                        T.matmul(PA[dh][:], lhsT=yattnT[:, c, tsl], rhs=wout[:, c, dh * 512:(dh + 1) * 512], start=(c == 0),
                                 stop=(c == 3))
                    for c in range(4):
                        ins = T.matmul(PL[dh][:], lhsT=ylruT[:, c, tsl], rhs=wout[:, 4 + c, dh * 512:(dh + 1) * 512],
                                       start=(c == 0), stop=(c == 3))
                S_["op"] = s_pe.inc(ins)
                W(A, s_pe, S_["stats"])
                if tt - 1 >= 0:
                    W(A, s_v, st3[tt - 1]["rs2"])
                S_["sd2"] = s_act.inc(A.activation(out=sd2[:], in_=psm[:, 0:2], func=AF.Sqrt, scale=1.0 / 512, bias=EPS))

            def stB1(tt):
                b = tt % 2
                S_ = st3[tt]
                tsl = slice(tt * 128, (tt + 1) * 128)
                W(V, s_act, S_["sd2"])
                S_["rs2"] = vv(V.reciprocal(out=rs2[:], in_=sd2[:]))
                W(V, s_v, S_["rs2"])
                W(V, s_pe, S_["op"])
                W(V, s_d, S_["x"])
                if tt - 1 >= 0:
                    W(V, s_act, st3[tt - 1]["sq"])
                for dh in range(2):
                    cs = slice(dh * 512, (dh + 1) * 512)
                    v_ = vv(V.tensor_scalar(out=t1[:, cs], in0=PA[dh][:], scalar1=rs2[:, 0:1], scalar2=None, op0=ALU.mult))
                    W(V, s_v, v_)
                    v_ = vv(V.scalar_tensor_tensor(out=t1[:, cs], in0=PL[dh][:], scalar=rs2[:, 1:2], in1=t1[:, cs], op0=ALU.mult,
                                                   op1=ALU.add))
                S_["comb"] = v_
                W(G, s_v, v_)
                vg_ = s_g.inc(G.tensor_tensor(out=t1[:], in0=t1[:], in1=gm_row, op=ALU.mult))
                W(G, s_g, vg_)
                if tt - 2 >= 0:
                    W(G, s_st, st3[tt - 2]["x1st"])
                    W(G, s_v, st3[tt - 2]["h2"])
                S_["x1"] = s_g.inc(G.tensor_tensor(out=x1[b][:], in0=t1[:], in1=xt[b][:], op=ALU.add))
                W(SY, s_g, S_["x1"])
                S_["x1st"] = s_st.dma(SY.dma_start(out=X1[tsl, :], in_=x1[b][:]), lane=b)
                W(A, s_g, S_["x1"])
                va = s_act.inc(A.activation(out=t1[:], in_=x1[b][:], func=AF.Square, accum_out=ss3[:]))
                S_["sq"] = va
                W(A, s_act, va)
                if tt - 1 >= 0:
                    W(A, s_v, st3[tt - 1]["rs3"])
                S_["ss3"] = s_act.inc(A.activation(out=ss3[:], in_=ss3[:], func=AF.Sqrt, scale=1.0 / D, bias=EPS))

            def stB2(tt):
                b = tt % 2
                S_ = st3[tt]
                tsl = slice(tt * 128, (tt + 1) * 128)
                W(V, s_act, S_["ss3"])
                S_["rs3"] = vv(V.reciprocal(out=rs3[:], in_=ss3[:]))
                W(V, s_v, S_["rs3"])
                if tt - 1 >= 0:
                    W(V, s_pe, st3[tt - 1]["tr"])
                    W(V, s_act, st3[tt - 1]["h2b"])
                v_ = vv(V.scalar_tensor_tensor(out=h2[:], in0=x1[b][:], scalar=rs3[:, 0:1], in1=Af_row, op0=ALU.mult,
                                               op1=ALU.mult))
                W(V, s_v, v_)
                S_["h2"] = vv(V.tensor_tensor(out=h2[:], in0=h2[:], in1=shf_row, op=ALU.add))
                W(A, s_v, S_["h2"])
                if tt - 2 >= 0:
                    W(A, s_st, st3[tt - 2]["h2st"])
                S_["h2b"] = s_act.inc(A.activation(out=h2b[b][:], in_=h2[:], func=AF.Copy))
                W(SY, s_act, S_["h2b"])
                S_["h2st"] = s_st.dma(SY.dma_start(out=H2[tsl, :], in_=h2b[b][:]), lane=2 + b)
                W(T, s_v, S_["h2"])
                if tt - 1 >= 0:
                    W(T, s_act, st3[tt - 1]["h2T"])
                for half in range(2):
                    if half == 1:
                        W(T, s_act, S_["h2T"])
                    for k in range(4):
                        kk = half * 4 + k
                        ins = T.transpose(ptr[:, k, :], h2[:, kk * 128:(kk + 1) * 128], ident_f[:])
                    S_["tr"] = s_pe.inc(ins)
                    W(A, s_pe, S_["tr"])
                    if tt - 1 >= 0 and half == 0:
                        W(A, s_pe, st3[tt - 1]["rt"])
                    S_["h2T"] = s_act.inc(A.activation(out=h2T[:, half * 4:(half + 1) * 4, :], in_=ptr[:], func=AF.Identity))
                W(T, s_act, S_["h2T"])
                if tt - 1 >= 0:
                    W(T, s_v, st3[tt - 1]["L"])
                for k in range(8):
                    ins = T.matmul(psl[:, 32:64], lhsT=h2T[:, k, :], rhs=rw[:, k, :], start=(k == 0), stop=(k == 7))
                S_["rt"] = s_pe.inc(ins)

            def stC(tt):
                S_ = st3[tt]
                W(V, s_pe, S_["rt"])
                S_["L"] = vv(V.tensor_tensor(out=Lall[:, tt, :], in0=psl[:, 32:64], in1=rb_row[:], op=ALU.add))
                W(V, s_v, S_["L"])
                v_ = vv(V.max(out=M8all[:, tt, :], in_=Lall[:, tt, :]))
                W(V, s_v, v_)
                if tt - 1 >= 0:
                    W(V, s_pe, st3[tt - 1]["rk"])
                v_ = vv(V.tensor_scalar(out=Mf[:], in0=Lall[:, tt, :], scalar1=M8all[:, tt, 3:4], scalar2=None, op0=ALU.is_ge))
                W(V, s_v, v_)
                vm = vv(V.tensor_copy(out=Mbf[:], in_=Mf[:]))
                W(T, s_v, vm)
                if tt - 1 >= 0:
                    W(T, s_v, st3[tt - 1]["cnt"])
                T.matmul(psr[:, 64:96], lhsT=tri_bf[:], rhs=Mbf[:], start=True, stop=True)
                ins = T.matmul(psr[:, 96:128], lhsT=ones_bf[:], rhs=Mbf[:], start=True, stop=True)
                S_["rk"] = s_pe.inc(ins)

            def stC2(tt):
                S_ = st3[tt]
                W(V, s_pe, S_["rk"])
                if tt - 1 >= 0:
                    W(V, s_v, st3[tt - 1]["cnt"])
                v_ = vv(V.tensor_tensor(out=rank_all[:, tt, :], in0=psr[:, 64:96], in1=cnt_run[:], op=ALU.add))
                W(V, s_v, v_)
                S_["cnt"] = vv(V.tensor_tensor(out=cnt_run[:], in0=psr[:, 96:128], in1=cnt_run[:], op=ALU.add))

            stA(0)
            for tt in range(NT):
                stB1(tt)
                if tt + 1 < NT:
                    stA(tt + 1)
                if tt - 1 >= 0:
                    stC(tt - 1)
                stB2(tt)
                if tt - 1 >= 0:
                    stC2(tt - 1)
            stC(NT - 1)
            stC2(NT - 1)
            barrier(allsig)

        with contextlib.ExitStack() as es:
            padded = sb(es, "padded", [128, 32])
            pad_end = sb(es, "pad_end", [128, 32])
            pad_start = sb(es, "pad_start", [128, 32])
            tmp32 = sb(es, "tmp32", [128, 32])
            destE = sb(es, "destE", [128, 32, 32])
            oh = sb(es, "oh", [128, 32, 32])
            E4 = sb(es, "E4", [128, 32, 4])
            den = sb(es, "den", [128, 32])
            tokid = sb(es, "tokid", [128, 32, 16], I32)
            zt = sb(es, "zt", [128, NSLOT * 16 // 128], I32)
            jv_i = sb(es, "jv_i", [128, NBP], I32)
            jv = sb(es, "jv", [128, NBP])
            pid_i = sb(es, "pid_i", [128, NBP], I32)
            pid = sb(es, "pid", [128, NBP])
            cmp = sb(es, "cmp", [128, NBP, 32])
            be = sb(es, "be", [128, NBP])
            s_v = sig(es, "4v")
            s_act = sig(es, "4a")
            s_g = sig(es, "4g")
            s_z = sig(es, "4z")

            def v(ins):
                val = s_v.inc(ins); W(V, s_v, val); return val

            def g(ins):
                val = s_g.inc(ins); W(G, s_g, val); return val

            g(G.iota(out=tokid[:], pattern=[[128, 32], [0, 16]], base=0, channel_multiplier=1))
            g(G.iota(out=jv_i[:], pattern=[[BLK, NBP]], base=0, channel_multiplier=0))
            g(G.iota(out=pid_i[:], pattern=[[0, NBP]], base=0, channel_multiplier=1))
            g(G.memset(zt[:], 0))
            s_z.dma(G.dma_start(out=SLOT.rearrange("(p f) c -> p (f c)", p=128), in_=zt[:]))
            W(V, s_g)
            v(V.tensor_copy(out=jv[:], in_=jv_i[:]))
            v(V.tensor_copy(out=pid[:], in_=pid_i[:]))
            v(V.tensor_tensor(out=destE[:], in0=cnt_run[:, :].unsqueeze(2).to_broadcast([128, 32, 32]),
                              in1=jv[:, 0:32].unsqueeze(1).to_broadcast([128, 32, 32]), op=ALU.is_gt))
            v(V.tensor_reduce(out=tmp32[:], in_=destE[:], axis=AX.X, op=ALU.add))
            v(V.tensor_scalar(out=padded[:], in0=tmp32[:], scalar1=float(BLK), scalar2=None, op0=ALU.mult))
            v(V.tensor_tensor_scan(out=pad_end[:], data0=ones_f[:, 0:32], data1=padded[:], initial=0.0, op0=ALU.mult,
                                   op1=ALU.add))
            v(V.tensor_tensor(out=pad_start[:], in0=pad_end[:], in1=padded[:], op=ALU.subtract))
            v(V.tensor_tensor(out=destE[:], in0=rank_all[:], in1=pad_start[:, :].unsqueeze(1).to_broadcast([128, 32, 32]),
                              op=ALU.add))
            for k in range(4):
                v(V.tensor_tensor(out=oh[:], in0=Lall[:], in1=M8all[:, :, k:k + 1].to_broadcast([128, 32, 32]),
                                  op=ALU.is_equal))
                v(V.tensor_tensor(out=oh[:], in0=oh[:], in1=destE[:], op=ALU.mult))
                v(V.tensor_reduce(out=dest4f[:, :, k], in_=oh[:], axis=AX.X, op=ALU.add))
            v(V.tensor_copy(out=dest4i[:], in_=dest4f[:, :, :].rearrange("p a b -> p (a b)")))
            vv = v(V.tensor_tensor(out=E4[:], in0=M8all[:, :, 0:4], in1=M8all[:, :, 0:1].to_broadcast([128, 32, 4]),
                                   op=ALU.subtract))
            W(A, s_v, vv)
            va = s_act.inc(A.activation(out=E4[:], in_=E4[:], func=AF.Exp))
            W(V, s_act, va)
            v(V.tensor_reduce(out=den[:], in_=E4[:], axis=AX.X, op=ALU.add))
            v(V.reciprocal(out=den[:], in_=den[:]))
            v(V.tensor_tensor(out=p4[:], in0=E4[:], in1=den[:, :].unsqueeze(2).to_broadcast([128, 32, 4]), op=ALU.mult))
            v(V.tensor_tensor(out=cmp[:], in0=pad_end[:, :].unsqueeze(1).to_broadcast([128, NBP, 32]),
                              in1=jv[:, :].unsqueeze(2).to_broadcast([128, NBP, 32]), op=ALU.is_le))
            v(V.tensor_reduce(out=be[:], in_=cmp[:], axis=AX.X, op=ALU.add))
            v(V.tensor_scalar(out=be[:], in0=be[:], scalar1=31.0, scalar2=None, op0=ALU.min))
            v(V.tensor_copy(out=bidx[:], in_=be[:]))
            v(V.scalar_tensor_tensor(out=be[:], in0=be[:], scalar=128.0, in1=pid[:], op0=ALU.mult, op1=ALU.add))
            v(V.tensor_scalar(out=jv[:], in0=jv[:], scalar1=pad_end[:, 31:32], scalar2=None, op0=ALU.is_lt))
            v(V.tensor_scalar(out=pid[:], in0=pid[:], scalar1=0.0, scalar2=None, op0=ALU.is_equal))
            v(V.tensor_tensor(out=jv[:], in0=jv[:], in1=pid[:], op=ALU.max))
            v(V.tensor_scalar(out=be[:], in0=be[:], scalar1=-100000.0, scalar2=None, op0=ALU.add))
            v(V.tensor_tensor(out=be[:], in0=be[:], in1=jv[:], op=ALU.mult))
            v(V.tensor_scalar(out=be[:], in0=be[:], scalar1=100000.0, scalar2=None, op0=ALU.add))
            v(V.tensor_copy(out=widx[:], in_=be[:]))
            W(G, s_v)
            W(G, s_z)
            for tt in range(NT):
                for k in range(4):
                    s_z.dma(G.indirect_dma_start(out=SLOT[:, :],
                                                 out_offset=bass.IndirectOffsetOnAxis(ap=dest4i[:, tt * 4 + k:tt * 4 + k + 1], axis=0),
                                                 in_=tokid[:, tt, :], in_offset=None))
            barrier([s_v, s_act, s_g, s_z])
            if dbg and stop == 4:
                for nm, t_, shp, dt_ in [("Lall", Lall, [128, 1024], F32), ("M8all", M8all, [128, 256], F32),
                                         ("dest4i", dest4i, [128, 128], I32), ("p4", p4, [128, 128], F32),
                                         ("widx", widx, [128, NBP], I32), ("bidx", bidx, [128, NBP], I32),
                                         ("cnt", cnt_run, [128, 32], F32)]:
                    dd = dbg_tensor(nm, shp, dt_)
                    ap_ = t_[:] if len(t_.shape) == 2 else t_[:, :, :].rearrange("p a b -> p (a b)")
                    s_out.dma(SY.dma_start(out=dd, in_=ap_))
                dd = dbg_tensor("slot", [NSLOT, 16], I32)
                s_out.dma(SY.dma_start(out=dd, in_=SLOT))
                dd = dbg_tensor("x1", [S, D])
                s_out.dma(SY.dma_start(out=dd, in_=X1))
                dd = dbg_tensor("h2", [S, D], BF16)
                s_out.dma(SY.dma_start(out=dd, in_=H2))
        if stop <= 4:
            W(SY, s_out)
            return nc, dbg_out

        with contextlib.ExitStack() as es:
            wbuf = [[RA[:, (par * 3 + m) * 8192:(par * 3 + m + 1) * 8192].rearrange("p (k n) -> p k n", k=8)
                     for m in range(3)] for par in range(2)]
            xb = [RB[:, par * 4096:par * 4096 + QB * 1024].rearrange("p (q d) -> p q d", q=QB) for par in range(2)]
            xbT = RB[:, 8192:8192 + 8 * BLK].rearrange("p (k s) -> p k s", k=8)
            actT0 = RB[:, 12288:12288 + 8 * BLK].rearrange("p (k s) -> p k s", k=8)
            actT1 = sb(es, "actT1", [128, 8, BLK], BF16)
            actT = [actT0, actT1]
            bgt = [sb(es, f"bgt{i}", [128, 8]) for i in range(2)]
            but = [sb(es, f"but{i}", [128, 8]) for i in range(2)]
            bdt = [sb(es, f"bdt{i}", [128, D]) for i in range(2)]
            tki = [sb(es, f"tki{i}", [128, QB, 16], I32) for i in range(2)]
            ysb = sb(es, "ysb", [128, QB, D])
            g_t = [sb(es, f"g_t{i}", [128, BLK]) for i in range(2)]
            sg_t = [sb(es, f"sg_t{i}", [128, BLK]) for i in range(2)]
            u_t = [sb(es, f"u_t{i}", [128, BLK]) for i in range(2)]
            ptx = [ps(es, f"ptx{i}", [128, QB, 128], BF16) for i in range(2)]
            pg = [ps(es, f"pg{i}", [128, BLK]) for i in range(2)]
            pu = [ps(es, f"pu{i}", [128, BLK]) for i in range(2)]
            py = [ps(es, f"py{i}", [128, 512]) for i in range(2)]
            s_tk = sig(es, "5tk", 2)
            s_xb = sig(es, "5xb", 2)
            s_wgu = sig(es, "5wgu", 2)
            s_wd = sig(es, "5wd", 2)
            s_pe = sig(es, "5t")
            s_v = sig(es, "5v")
            s_act = sig(es, "5a")
            s_yd = sig(es, "5y")
            blk = {j: {} for j in range(NBLK)}
            wsrc = (wg_d, wu_d, wd_d)
            bc_reg = G.to_reg(4095)

            def wgather(sg_, par, m, j):
                for c4 in range(4):
                    val = sg_.dma(G.indirect_dma_start(
                        out=RA[:, (par * 3 + m) * 8192 + c4 * 2048:(par * 3 + m) * 8192 + (c4 + 1) * 2048], out_offset=None,
                        in_=wsrc[m][0], in_offset=bass.IndirectOffsetOnAxis(ap=widx[:, j:j + 1], axis=0),
                        element_offset=c4 * 4096 * 2048, bounds_check=bc_reg, oob_is_err=False), lane=par)
                return val

            def loads_gu(j):
                par = j % 2
                st = blk[j]
                if j - 2 >= 0:
                    W(G, s_pe, blk[j - 2]["gu_done"])
                    W(G, s_v, blk[j - 2]["sw_done"])
                    W(G, s_act, blk[j - 2]["sw_act"])
                for q in range(QB):
                    vtk = s_tk.dma(G.dma_start(out=tki[par][:, q, :],
                                               in_=SLOT[j * BLK + q * 128:j * BLK + (q + 1) * 128, :]), lane=par)
                W(G, s_tk, vtk)
                for q in range(QB):
                    st["xb"] = s_xb.dma(G.indirect_dma_start(
                        out=xb[par][:, q, :], out_offset=None, in_=H2[:, :],
                        in_offset=bass.IndirectOffsetOnAxis(ap=tki[par][:, q, 0:1], axis=0)), lane=par)
                wgather(s_wgu, par, 0, j)
                wgather(s_wgu, par, 1, j)
                s_wgu.dma(G.indirect_dma_start(out=bgt[par][:], out_offset=None, in_=bg_d[:, :],
                                               in_offset=bass.IndirectOffsetOnAxis(ap=widx[:, j:j + 1], axis=0),
                                               bounds_check=bc_reg, oob_is_err=False), lane=par)
                st["wgu"] = s_wgu.dma(G.indirect_dma_start(out=but[par][:], out_offset=None, in_=bu_d[:, :],
                                                           in_offset=bass.IndirectOffsetOnAxis(ap=widx[:, j:j + 1], axis=0),
                                                           bounds_check=bc_reg, oob_is_err=False), lane=par)

            def loads_d(j):
                par = j % 2
                st = blk[j]
                if j - 2 >= 0:
                    W(G, s_pe, blk[j - 2]["d_done"])
                    W(G, s_v, blk[j - 2]["y_done"])
                wgather(s_wd, par, 2, j)
                st["wd"] = s_wd.dma(G.indirect_dma_start(out=bdt[par][:], out_offset=None, in_=bd_d[:, :],
                                                         in_offset=bass.IndirectOffsetOnAxis(ap=bidx[:, j:j + 1], axis=0)), lane=par)

            cnt5 = {"pgi": 0, "pyi": 0}
            pg_free = {}
            py_free = {}

            def TK(j, k):
                par = j % 2
                st = blk[j]
                if k == 0:
                    W(T, s_xb, st["xb"])
                    st["txe"] = {}
                ev_done = st["txe"]
                pb = ptx[k % 2]
                if k >= 2:
                    W(T, s_v, ev_done[k - 2])
                elif j > 0:
                    W(T, s_v, blk[j - 1]["txe"][6 + k])
                for q in range(QB):
                    ins = T.transpose(pb[:, q, :], xb[par][:, q, k * 128:(k + 1) * 128], ident_bf[:])
                vt = s_pe.inc(ins)
                W(V, s_pe, vt)
                ev_done[k] = s_v.inc(V.tensor_copy(out=xbT[:, k, :], in_=pb[:, :, :].rearrange("p q s -> p (q s)")))

            def GU(j):
                par = j % 2
                st = blk[j]
                ev_done = st["txe"]
                W(T, s_v, ev_done[7])
                W(T, s_wgu, st["wgu"])
                W(V, s_wgu, st["wgu"])
                W(A, s_wgu, st["wgu"])
                if j - 2 >= 0:
                    W(V, s_pe, blk[j - 2]["d_done"])
                at = actT[j % 2]
                for fc in range(8):
                    pgi = cnt5["pgi"]
                    gb = pgi % 2
                    if pgi >= 2:
                        W(T, s_v, pg_free[pgi - 2])
                    for k in range(8):
                        T.matmul(pg[gb][:], lhsT=wbuf[par][0][:, k, fc * 128:(fc + 1) * 128], rhs=xbT[:, k, :],
                                 start=(k == 0), stop=(k == 7))
                    for k in range(8):
                        ins = T.matmul(pu[gb][:], lhsT=wbuf[par][1][:, k, fc * 128:(fc + 1) * 128], rhs=xbT[:, k, :],
                                       start=(k == 0), stop=(k == 7))
                    vt = s_pe.inc(ins)
                    W(V, s_pe, vt)
                    W(A, s_pe, vt)
                    v1 = s_v.inc(V.tensor_scalar(out=g_t[gb][:], in0=pg[gb][:], scalar1=bgt[par][:, fc:fc + 1], scalar2=7.0,
                                                 op0=ALU.add, op1=ALU.min))
                    W(A, s_v, v1)
                    a1 = s_act.inc(A.activation(out=sg_t[gb][:], in_=g_t[gb][:], func=AF.Sigmoid, scale=1.702))
                    a2 = s_act.inc(A.activation(out=u_t[gb][:], in_=pu[gb][:], func=AF.Identity,
                                                bias=but[par][:, fc:fc + 1], scale=1.0))
                    W(V, s_act, a2)
                    v2 = s_v.inc(V.tensor_scalar(out=u_t[gb][:], in0=u_t[gb][:], scalar1=7.0, scalar2=-7.0, op0=ALU.min,
                                                 op1=ALU.max))
                    v3 = s_v.inc(V.tensor_tensor(out=g_t[gb][:], in0=g_t[gb][:], in1=sg_t[gb][:], op=ALU.mult))
                    W(V, s_v, v3)
                    v4 = s_v.inc(V.scalar_tensor_tensor(out=at[:, fc, :], in0=u_t[gb][:], scalar=1.0, in1=g_t[gb][:],
                                                        op0=ALU.add, op1=ALU.mult))
                    pg_free[pgi] = v4
                    cnt5["pgi"] += 1
                st["gu_done"] = vt
                st["sw_done"] = v4
                st["sw_act"] = a2

            def DN(j, jn=None):
                par = j % 2
                st = blk[j]
                at = actT[j % 2]
                W(T, s_v, st["sw_done"])
                W(T, s_wd, st["wd"])
                W(V, s_wd, st["wd"])
                if j - 1 >= 0:
                    W(V, s_yd, blk[j - 1]["yd"])
                for q in range(QB):
                    for dh in range(2):
                        pyi = cnt5["pyi"]
                        yb = pyi % 2
                        if pyi >= 2:
                            W(T, s_v, py_free[pyi - 2])
                        for fc in range(8):
                            ins = T.matmul(py[yb][:], lhsT=at[:, fc, q * 128:(q + 1) * 128],
                                           rhs=wbuf[par][2][:, fc, dh * 512:(dh + 1) * 512], start=(fc == 0), stop=(fc == 7))
                        vt = s_pe.inc(ins)
                        W(V, s_pe, vt)
                        py_free[pyi] = s_v.inc(V.tensor_tensor(out=ysb[:, q, dh * 512:(dh + 1) * 512], in0=py[yb][:],
                                                               in1=bdt[par][:, dh * 512:(dh + 1) * 512], op=ALU.add))
                        cnt5["pyi"] += 1
                        if jn is not None:
                            TK(jn, q * 2 + dh)
                if jn is not None:
                    for k in range(2 * QB, 8):
                        TK(jn, k)
                st["d_done"] = vt
                st["y_done"] = py_free[cnt5["pyi"] - 1]
                W(SY, s_v, st["y_done"])
                st["yd"] = s_yd.dma(SY.dma_start(out=Yd[j * BLK:(j + 1) * BLK, :].rearrange("(q p) d -> p q d", p=128),
                                                 in_=ysb[:]))

            loads_gu(0)
            loads_d(0)
            loads_gu(1)
            loads_d(1)
            for k in range(8):
                TK(0, k)
            for j in range(NBLK + 1):
                if j < NBLK:
                    GU(j)
                    if j + 2 < NBLK:
                        loads_gu(j + 2)
                if j >= 1:
                    DN(j - 1, j + 1 if j + 1 < NBLK else None)
                    if j + 1 < NBLK:
                        loads_d(j + 1)
                elif NBLK > 1:
                    for k in range(8):
                        TK(1, k)
            barrier([s_pe, s_v, s_act, s_yd, s_wgu, s_wd, s_xb, s_tk])

        if stop <= 5:
            if dbg:
                dd = dbg_tensor("Y", [NSLOT, D])
                s_out.dma(SY.dma_start(out=dd, in_=Yd))
            W(SY, s_out)
            return nc, dbg_out

        with contextlib.ExitStack() as es:
            RAf = RA[:, :].bitcast(F32)

            class _V:
                def __init__(self, ap): self.ap = ap
                def __getitem__(self, idx): return self.ap

            def fv(i):
                return _V(RAf[:, i * D:(i + 1) * D])
            yg = [[fv(i * 4 + k) for k in range(4)] for i in range(3)]
            x1t = [fv(12 + i) for i in range(3)]
            ot = [fv(17 + i) for i in range(2)]
            ss6 = sb(es, "ss6", [128, 32])
            rs6 = sb(es, "rs6", [128, 32])
            s_g = sig(es, "6g", 3)
            s_d = sig(es, "6d", 5)
            s_v = sig(es, "6v")
            s_act = sig(es, "6a")
            hist = {}

            def v(ins):
                val = s_v.inc(ins); W(V, s_v, val); return val

            def a(ins):
                val = s_act.inc(ins); W(A, s_act, val); return val

            def loads(tt):
                b = tt % 3
                if tt - 3 >= 0:
                    W(G, s_v, hist[tt - 3]["acc"])
                    W(SY, s_v, hist[tt - 3]["acc"])
                for k in range(4):
                    vg = s_g.dma(G.indirect_dma_start(out=yg[b][k][:], out_offset=None, in_=Yd[:, :],
                                                      in_offset=bass.IndirectOffsetOnAxis(
                                                          ap=dest4i[:, tt * 4 + k:tt * 4 + k + 1], axis=0)), lane=b)
                vd = s_d.dma(SY.dma_start(out=x1t[b][:], in_=X1[tt * 128:(tt + 1) * 128, :]), lane=b)
                hist[tt] = {"g": vg, "d": vd}

            accs = [fv(15), fv(16)]

            def comb(tt):
                b = tt % 2
                b3 = tt % 3
                ac = accs[b]
                W(V, s_g, hist[tt]["g"])
                W(V, s_d, hist[tt]["d"])
                v(V.tensor_scalar(out=ac[:], in0=yg[b3][0][:], scalar1=p4[:, tt, 0:1], scalar2=None, op0=ALU.mult))
                for k in range(1, 4):
                    v(V.scalar_tensor_tensor(out=ac[:], in0=yg[b3][k][:], scalar=p4[:, tt, k:k + 1], in1=ac[:],
                                             op0=ALU.mult, op1=ALU.add))
                v(V.tensor_tensor(out=ac[:], in0=ac[:], in1=gf_row, op=ALU.mult))
                vacc = v(V.tensor_tensor(out=ac[:], in0=ac[:], in1=x1t[b3][:], op=ALU.add))
                hist[tt]["acc"] = vacc
                W(A, s_v, vacc)
                if tt - 2 >= 0:
                    W(A, s_d, hist[tt - 2]["od"])
                va = s_act.inc(A.activation(out=ot[b][:], in_=ac[:], func=AF.Square, accum_out=ss6[:, tt:tt + 1]))
                hist[tt]["sq"] = va

            def fin(tt):
                b = tt % 2
                ac = accs[b]
                W(V, s_act, hist[tt]["sd"])
                v(V.reciprocal(out=rs6[:, tt:tt + 1], in_=ss6[:, tt:tt + 1]))
                vo = v(V.scalar_tensor_tensor(out=ot[b][:], in0=ac[:], scalar=rs6[:, tt:tt + 1], in1=fing_row[:], op0=ALU.mult,
                                              op1=ALU.mult))
                hist[tt]["fin"] = vo
                W(SY, s_v, vo)
                hist[tt]["od"] = s_d.dma(SY.dma_start(out=out_d[tt * 128:(tt + 1) * 128, :], in_=ot[b][:]), lane=3 + b)

            def sqrt_(tt):
                W(A, s_act, hist[tt]["sq"])
                hist[tt]["sd"] = s_act.inc(A.activation(out=ss6[:, tt:tt + 1], in_=ss6[:, tt:tt + 1], func=AF.Sqrt,
                                                        scale=1.0 / D, bias=EPS))

            loads(0)
            loads(1)
            loads(2)
            for tt in range(NT + 1):
                if tt < NT:
                    if tt - 2 >= 0:
                        W(V, s_v, hist[tt - 2]["fin"])
                    comb(tt)
                    sqrt_(tt)
                if tt - 1 >= 0:
                    fin(tt - 1)
                    if tt + 2 < NT:
                        loads(tt + 2)
            barrier([s_d, s_v, s_act, s_g])
        W(SY, s_out)
    return nc, dbg_out


def _layout_w(w):
    w5 = w.reshape(32, 4, 2, 128, 1024)
    return np.ascontiguousarray(w5.transpose(1, 0, 3, 2, 4)).reshape(4, 4096, 2048)


_SHARED_CACHE = {}


def make_in_maps(inp, ncores=8):
    f = lambda a: np.ascontiguousarray(a, dtype=np.float32)
    sh = {
        "ada_w": f(inp["ada_w"][0]),
        "ada_b_row": f(inp["ada_b"][0].reshape(1, -1)),
        "ada_b_col": f(inp["ada_b"][0].reshape(48, 128).T),
        "mixg_col": f(inp["mix_norm_g"][0].reshape(8, 128).T),
        "w_in": f(inp["w_in"][0]),
        "convw_col": f(inp["conv_w"][0].T.reshape(4, 128, 4).transpose(1, 0, 2).reshape(128, 16)),
        "lru_cols": f(np.stack([inp["conv_b"][0], inp["rg_b"][0], inp["ig_b"][0], inp["lru_lambda"][0]], axis=-1)
                      .reshape(4, 128, 4).transpose(1, 0, 2).reshape(128, 16)),
        "rg_w": f(inp["rg_w"][0]),
        "ig_w": f(inp["ig_w"][0]),
        "outg_col": f(np.concatenate([inp["attn_out_g"][0], inp["lru_out_g"][0]]).reshape(8, 128).T),
        "w_out": f(inp["w_out"][0]),
        "ffn_g_row": f(inp["ffn_norm_g"][0].reshape(1, -1)),
        "final_g_row": f(inp["final_norm_g"].reshape(1, -1)),
        "router_w": f(inp["router_w"][0]),
        "router_b_row": f(inp["router_b"][0].reshape(1, -1)),
        "wg": _layout_w(f(inp["exp_w_gate"][0])),
        "wu": _layout_w(f(inp["exp_w_up"][0])),
        "wd": _layout_w(f(inp["exp_w_down"][0])),
        "bg": f(inp["exp_b_gate"][0].reshape(32, 8, 128).transpose(0, 2, 1).reshape(4096, 8)),
        "bu": f(inp["exp_b_up"][0].reshape(32, 8, 128).transpose(0, 2, 1).reshape(4096, 8)),
        "bd": f(inp["exp_b_down"][0]),
    }
    maps = []
    for b in range(ncores):
        m = dict(sh)
        m["x"] = f(inp["x"][b])
        m["c_col"] = f(inp["c"][b].reshape(8, 128).T)
        maps.append(m)
    return maps


def kernel(**inputs):
    nc, _ = build_program()
    maps = make_in_maps(inputs, 8)
    res = run_bass_kernel_spmd(nc, maps, core_ids=list(range(8)))
    return np.stack([np.asarray(r["out"], dtype=np.float32) for r in res.results], axis=0)
```

```python
import contextlib
import numpy as np
import concourse.bass as bass
import concourse.mybir as mybir
from concourse.bass_utils import run_bass_kernel_spmd

F32 = mybir.dt.float32
BF16 = mybir.dt.bfloat16
I32 = mybir.dt.int32
ALU = mybir.AluOpType
AF = mybir.ActivationFunctionType
AX = mybir.AxisListType

S = 4096
D = 1024
NT = S // 128
EPS = 1e-6
BLK = 512
QB = BLK // 128
NBLK = 32 + (S * 4 - 32) // BLK
NBP = 80
NSLOT = NBLK * BLK


class Sig:
    def __init__(self, nc, es, name, lanes=1):
        self.sems = [es.enter_context(nc.semaphore(f"{name}_l{i}")) for i in range(lanes)]
        self.cnt = [0] * lanes
        self.lanes = lanes

    @property
    def sem(self):
        return self.sems[0]

    @property
    def n(self):
        return self.cnt[0]

    def inc(self, ins, k=1, lane=0):
        ins.then_inc(self.sems[lane], k)
        self.cnt[lane] += k
        return self.cnt[lane] if self.lanes == 1 else (lane, self.cnt[lane])

    def dma(self, ins, lane=0):
        return self.inc(ins, 16, lane)


def W(eng, sig, val=None):
    if val is None:
        for l in range(sig.lanes):
            if sig.cnt[l] > 0:
                eng.wait_ge(sig.sems[l], sig.cnt[l])
        return
    if isinstance(val, tuple):
        lane, v = val
    else:
        lane, v = 0, val
    if v > 0:
        eng.wait_ge(sig.sems[lane], v)


def build_program(stop=99, dbg=None):
    nc = bass.Bass("TRN2", target_bir_lowering=False)
    dbg_out = {}

    def din(name, shape, dt=F32):
        return nc.dram_tensor(name, list(shape), dt, kind="ExternalInput").ap()

    x_d = din("x", [S, D])
    ccol_d = din("c_col", [128, 8])
    adaw_d = din("ada_w", [D, 6 * D])
    adab_row_d = din("ada_b_row", [1, 6 * D])
    adab_col_d = din("ada_b_col", [128, 48])
    mixg_d = din("mixg_col", [128, 8])
    win_d = din("w_in", [D, 2560])
    convw_d = din("convw_col", [128, 16])
    lruv_d = din("lru_cols", [128, 16])
    rgw_d = din("rg_w", [8, 64, 64])
    igw_d = din("ig_w", [8, 64, 64])
    outg_d = din("outg_col", [128, 8])
    wout_d = din("w_out", [D, D])
    ffng_d = din("ffn_g_row", [1, D])
    fing_d = din("final_g_row", [1, D])
    rw_d = din("router_w", [D, 32])
    rb_d = din("router_b_row", [1, 32])
    wg_d = din("wg", [4, 4096, 2048])
    wu_d = din("wu", [4, 4096, 2048])
    wd_d = din("wd", [4, 4096, 2048])
    bg_d = din("bg", [4096, 8])
    bu_d = din("bu", [4096, 8])
    bd_d = din("bd", [32, D])
    out_d = nc.dram_tensor("out", [S, D], F32, kind="ExternalOutput").ap()
    X1 = nc.dram_tensor("X1s", [S, D], F32, kind="Internal").ap()
    H2 = nc.dram_tensor("H2s", [S, D], BF16, kind="Internal").ap()
    Yd = nc.dram_tensor("Ys", [NSLOT, D], F32, kind="Internal").ap()
    SLOT = nc.dram_tensor("SLOTs", [NSLOT, 16], I32, kind="Internal").ap()

    def dbg_tensor(name, shape, dt=F32):
        t = nc.dram_tensor("dbg_" + name, list(shape), dt, kind="ExternalOutput").ap()
        dbg_out[name] = t
        return t

    V, A, G, T, SY = nc.vector, nc.scalar, nc.gpsimd, nc.tensor, nc.sync

    with contextlib.ExitStack() as top:
        def sb(es, name, shape, dt=F32):
            return es.enter_context(nc.sbuf_tensor(name, list(shape), dt))

        def ps(es, name, shape, dt=F32):
            return es.enter_context(nc.psum_tensor(name, list(shape), dt))

        sig_i = [0]

        def sig(es, name, lanes=1):
            sig_i[0] += 1
            return Sig(nc, es, f"{name}_{sig_i[0]}", lanes)

        s_out = sig(top, "out")

        ident_bf = sb(top, "ident_bf", [128, 128], BF16)
        ident_f = sb(top, "ident_f", [128, 128], F32)
        ones_f = sb(top, "ones_f", [128, 128], F32)
        ones_bf = sb(top, "ones_bf", [128, 128], BF16)
        negones_bf = sb(top, "negones_bf", [128, 128], BF16)
        negU_bf = sb(top, "negU_bf", [128, 128], BF16)
        tri_bf = sb(top, "tri_bf", [128, 128], BF16)
        zeros_bf = sb(top, "zeros_bf", [128, 512], BF16)
        s_c = sig(top, "const")
        s_cv = sig(top, "constv")
        s_cv.inc(V.memset(ones_f[:], 1.0))
        s_cv.inc(V.memset(ones_bf[:], 1.0))
        s_cv.inc(V.memset(negones_bf[:], -1.0))
        s_cv.inc(V.memset(zeros_bf[:], 0.0))
        W(G, s_cv)
        s_c.inc(G.affine_select(out=ident_f[:], in_=ones_f[:], pattern=[[-1, 128]], compare_op=ALU.is_equal,
                                fill=0.0, base=0, channel_multiplier=1))
        s_c.inc(G.affine_select(out=ident_bf[:], in_=ones_bf[:], pattern=[[-1, 128]], compare_op=ALU.is_equal,
                                fill=0.0, base=0, channel_multiplier=1))
        s_c.inc(G.affine_select(out=negU_bf[:], in_=negones_bf[:], pattern=[[-1, 128]], compare_op=ALU.is_ge,
                                fill=0.0, base=0, channel_multiplier=1))
        s_c.inc(G.affine_select(out=tri_bf[:], in_=ones_bf[:], pattern=[[1, 128]], compare_op=ALU.is_gt,
                                fill=0.0, base=0, channel_multiplier=-1))
        for e in (V, A, T, SY):
            W(e, s_c)
        W(G, s_c)

        modrow = sb(top, "modrow", [128, 4 * D])
        modcol = sb(top, "modcol", [128, 16])
        Am = sb(top, "Am", [128, 8])
        fing_row = sb(top, "fing_row", [128, D])
        gm_row = modrow[:, 0:D]
        shf_row = modrow[:, D:2 * D]
        Af_row = modrow[:, 2 * D:3 * D]
        gf_row = modrow[:, 3 * D:4 * D]
        Bm = modcol[:, 0:8]

        ssall = sb(top, "ssall", [128, 32])
        rstdall = sb(top, "rstdall", [128, 32])
        with contextlib.ExitStack() as es:
            xs = [sb(es, f"xs{i}", [128, D]) for i in range(4)]
            junk = sb(es, "junk0", [128, D], BF16)
            s_x = sig(es, "0x", 4)
            s_a = sig(es, "0a")
            s_v0 = sig(es, "0v")
            vxs = {}
            vas = {}
            for tt in range(NT):
                if tt >= 4:
                    W(SY, s_a, vas[tt - 4])
                vxs[tt] = s_x.dma(SY.dma_start(out=xs[tt % 4][:], in_=x_d[tt * 128:(tt + 1) * 128, :]), lane=tt % 4)
                W(A, s_x, vxs[tt])
                vas[tt] = s_a.inc(A.activation(out=junk[:], in_=xs[tt % 4][:], func=AF.Square, accum_out=ssall[:, tt:tt + 1]))
            W(A, s_a)
            va = s_a.inc(A.activation(out=ssall[:], in_=ssall[:], func=AF.Sqrt, scale=1.0 / D, bias=EPS))
            W(V, s_a, va)
            s_v0.inc(V.reciprocal(out=rstdall[:], in_=ssall[:]))
            barrier_early = [s_x, s_a, s_v0]
            for e in (V, A, T, SY, G):
                for s_ in barrier_early:
                    W(e, s_)

        with contextlib.ExitStack() as es:
            ccol = sb(es, "ccol", [128, 8])
            scol = sb(es, "scol", [128, 8])
            scb = sb(es, "scb", [128, 8, 128], BF16)
            scol_b = sb(es, "scol_b", [128, 8], BF16)
            adab_col = sb(es, "adab_col", [128, 48])
            mixg = sb(es, "mixg", [128, 8])
            ffng_row = sb(es, "ffng_row", [128, D])
            adab_row = sb(es, "adab_row", [128, 512])
            aw = [sb(es, f"aw{i}", [128, 8, 512], BF16) for i in range(2)]
            pr = [ps(es, f"p0r{i}", [128, 512]) for i in range(2)]
            pc = ps(es, "p0c", [128, 16])
            s_ld = sig(es, "p0ld")
            s_aw = sig(es, "p0aw", 2)
            s_awfree = sig(es, "p0awf")
            s_a = sig(es, "p0a")
            s_v = sig(es, "p0v")
            s_t = sig(es, "p0t")
            s_b = sig(es, "p0b")
            s_ld.dma(SY.dma_start(out=ccol[:], in_=ccol_d))
            s_ld.dma(SY.dma_start(out=adab_col[:], in_=adab_col_d))
            s_ld.dma(SY.dma_start(out=mixg[:], in_=mixg_d))
            s_ld.dma(SY.dma_start(out=ffng_row[:], in_=ffng_d.broadcast_to([128, D])))
            s_ld.dma(SY.dma_start(out=fing_row[:], in_=fing_d.broadcast_to([128, D])))
            W(A, s_ld)
            s_a.inc(A.activation(out=scol[:], in_=ccol[:], func=AF.Silu))
            W(V, s_a)
            W(V, s_ld)
            for k in range(8):
                s_v.inc(V.tensor_scalar(out=scb[:, k, :], in0=ones_f[:], scalar1=scol[:, k:k + 1], scalar2=None,
                                        op0=ALU.mult))
            s_v.inc(V.tensor_copy(out=scol_b[:], in_=scol[:]))
            aw_view = adaw_d.rearrange("(k p) n -> p k n", p=128)
            W(T, s_v)
            tdone = {}
            vev = {}
            for n in range(12):
                b = n % 2
                if n >= 2:
                    W(G, s_t, tdone[n - 2])
                v_aw = s_aw.dma(G.dma_start(out=aw[b][:], in_=aw_view[:, :, n * 512:(n + 1) * 512]), lane=b)
                W(T, s_aw, v_aw)
                if n < 4:
                    for fcl in range(4):
                        fc = n * 4 + fcl
                        for k in range(8):
                            ins = T.matmul(pc[:, fc:fc + 1], lhsT=aw[b][:, k, fcl * 128:(fcl + 1) * 128],
                                           rhs=scol_b[:, k:k + 1], start=(k == 0), stop=(k == 7))
                    tdone[n] = s_t.inc(ins)
                else:
                    if n - 2 >= 4:
                        W(T, s_v, vev[n - 2])
                    for k in range(8):
                        ins = T.matmul(pr[b][:], lhsT=scb[:, k, :], rhs=aw[b][:, k, :], start=(k == 0), stop=(k == 7))
                    tdone[n] = s_t.inc(ins)
                    if n - 1 >= 4:
                        W(SY, s_v, vev[n - 1])
                    v_b = s_b.dma(SY.dma_start(out=adab_row[:], in_=adab_row_d[:, n * 512:(n + 1) * 512]
                                               .broadcast_to([128, 512])))
                    W(V, s_b, v_b)
                    W(V, s_t, tdone[n])
                    vev[n] = s_v.inc(V.tensor_tensor(out=modrow[:, (n - 4) * 512:(n - 3) * 512], in0=pr[b][:],
                                                     in1=adab_row[:], op=ALU.add))
            W(V, s_t)
            s_v.inc(V.tensor_tensor(out=modcol[:], in0=pc[:], in1=adab_col[:, 0:16], op=ALU.add))
            W(V, s_v)
            s_v.inc(V.scalar_tensor_tensor(out=Am[:], in0=modcol[:, 8:16], scalar=1.0, in1=mixg[:], op0=ALU.add,
                                           op1=ALU.mult))
            s_v.inc(V.scalar_tensor_tensor(out=modrow[:, 2 * D:3 * D], in0=modrow[:, 2 * D:3 * D], scalar=1.0,
                                           in1=ffng_row[:], op0=ALU.add, op1=ALU.mult))
            for e in (A, T, SY, G):
                W(e, s_v)
            W(V, s_v)
            if dbg:
                d1 = dbg_tensor("modrow", [128, 4 * D])
                d2 = dbg_tensor("modcol", [128, 16])
                s_out.dma(SY.dma_start(out=d1, in_=modrow[:]))
                s_out.dma(SY.dma_start(out=d2, in_=modcol[:]))

        if stop <= 0:
            W(SY, s_out)
            return nc, dbg_out

        RA = sb(top, "RA", [128, 49152], BF16)
        RB = sb(top, "RB", [128, 16384], BF16)
        qT = RA[:, 0:16384].rearrange("p (c t) -> p c t", c=4)
        kT = RA[:, 16384:32768].rearrange("p (c t) -> p c t", c=4)
        vtok = RA[:, 32768:49152].rearrange("p (n f) -> p n f", n=32)
        yattnT = RB[:, :].rearrange("p (c t) -> p c t", c=4)
        cnt = {"tile": 0}

        def emit_norm_transpose(es, tiles, hT_of, xn_bufs, pT, xt, sg, hist):
            s_x, s_act, s_v, s_pe = sg
            st = hist
            base = len(st)

            def stage1(jl, tt):
                j = base + jl
                b = j % 2
                if j - 2 in st:
                    W(SY, s_v, st[j - 2]["xn"])
                vx = s_x.dma(SY.dma_start(out=xt[b][:], in_=x_d[tt * 128:(tt + 1) * 128, :]), lane=b)
                W(V, s_x, vx)
                if j - 2 in st:
                    W(V, s_pe, st[j - 2]["tr"])
                v3 = s_v.inc(V.tensor_scalar(out=xn_bufs[b], in0=xt[b][:], scalar1=rstdall[:, tt:tt + 1], scalar2=None,
                                             op0=ALU.mult))
                W(T, s_v, v3)
                if j - 2 in st:
                    W(T, s_v, st[j - 2]["ev"])
                for k in range(8):
                    ins = T.transpose(pT[b][:, k, :], xn_bufs[b][:, k * 128:(k + 1) * 128], ident_bf[:])
                st[j] = {"xn": v3, "tr": s_pe.inc(ins)}

            def stage2(jl):
                j = base + jl
                b = j % 2
                W(V, s_pe, st[j]["tr"])
                dst = hT_of(jl)
                for k in range(8):
                    ins = V.tensor_scalar(out=dst[:, k, :], in0=pT[b][:, k, :], scalar1=Am[:, k:k + 1],
                                          scalar2=Bm[:, k:k + 1], op0=ALU.mult, op1=ALU.add)
                st[j]["ev"] = s_v.inc(ins)

            for jl, tt in enumerate(tiles):
                stage1(jl, tt)
                if jl >= 1:
                    stage2(jl - 1)
            stage2(len(tiles) - 1)
            return st[base + len(tiles) - 1]["ev"]

        with contextlib.ExitStack() as es:
            win = RB[:, 0:12288].rearrange("p (k n) -> p k n", k=8)
            xn_bufs = [RB[:, 12288 + i * 1024:12288 + (i + 1) * 1024] for i in range(2)]
            hT = [sb(es, f"hT{i}", [128, 8, 1024], BF16) for i in range(2)]
            xt = [sb(es, f"xt{i}", [128, D]) for i in range(2)]
            pT = [ps(es, f"pT{i}", [128, 8, 128], BF16) for i in range(2)]
            pj = [ps(es, f"pj{i}", [128, 512]) for i in range(4)]
            s_w = sig(es, "p1w")
            sg = (sig(es, "p1x", 2), sig(es, "p1a"), sig(es, "p1v"), sig(es, "p1t"))
            s_x, s_act, s_v, s_pe = sg
            s_ea = sig(es, "p1ea")
            s_ev = sig(es, "p1ev")
            wv = win_d.rearrange("(k p) n -> p k n", p=128)
            for kh in range(2):
                s_w.dma(G.dma_start(out=win[:, kh * 4:(kh + 1) * 4, :], in_=wv[:, kh * 4:(kh + 1) * 4, 0:1536]))
            W(T, s_w)
            gi = 0
            ev_hist = {}
            hist1 = {}
            hT_last_pe = {}
            vh_q = {}

            def emit_nt(q):
                if q >= 2:
                    W(V, s_pe, hT_last_pe[q - 2])
                hq_ = hT[q % 2]
                vh_q[q] = emit_norm_transpose(es, list(range(q * 8, q * 8 + 8)),
                                              lambda j, hq_=hq_: hq_[:, :, j * 128:(j + 1) * 128], xn_bufs, pT, xt, sg, hist1)

            emit_nt(0)
            for q in range(4):
                hq = hT[q % 2]
                if q + 1 < 4:
                    emit_nt(q + 1)
                W(T, s_v, vh_q[q])
                groups = [("qk", ec, th) for ec in range(8) for th in range(2)] + [("v", tt, 0) for tt in range(8)]
                for (kind, a_, b_) in groups:
                    pb = pj[gi % 4]
                    if gi >= 4:
                        sg_, val_ = ev_hist[gi - 4]
                        W(T, sg_, val_)
                    for k in range(8):
                        if kind == "qk":
                            ins = T.matmul(pb[:], lhsT=win[:, k, a_ * 128:(a_ + 1) * 128],
                                           rhs=hq[:, k, b_ * 512:(b_ + 1) * 512], start=(k == 0), stop=(k == 7))
                        else:
                            ins = T.matmul(pb[:], lhsT=hq[:, k, a_ * 128:(a_ + 1) * 128], rhs=win[:, k, 1024:1536],
                                           start=(k == 0), stop=(k == 7))
                    vt = s_pe.inc(ins)
                    hT_last_pe[q] = vt
                    if kind == "qk":
                        tok0 = q * 1024 + b_ * 512
                        if a_ < 4:
                            dst = qT[:, a_, tok0:tok0 + 512]
                            scale = 0.125
                        else:
                            dst = kT[:, a_ - 4, tok0:tok0 + 512]
                            scale = 1.0
                    else:
                        dst = vtok[:, q * 8 + a_, :]
                        scale = 1.0
                    if gi % 2 == 0:
                        W(A, s_pe, vt)
                        ev_hist[gi] = (s_ea, s_ea.inc(A.activation(out=dst, in_=pb[:], func=AF.Copy, scale=scale)))
                    else:
                        W(V, s_pe, vt)
                        ev_hist[gi] = (s_ev, s_ev.inc(V.tensor_scalar(out=dst, in0=pb[:], scalar1=scale, scalar2=None,
                                                                      op0=ALU.mult)))
                    gi += 1
            for e in (A, V, T, G, SY):
                W(e, s_ea)
                W(e, s_ev)
                W(e, s_pe)
            if dbg and stop == 1:
                dq = dbg_tensor("qkv", [128, 49152], BF16)
                s_out.dma(SY.dma_start(out=dq, in_=RA[:, :]))
                d2 = dbg_tensor("rstd", [128, 32])
                s_out.dma(SY.dma_start(out=d2, in_=rstdall[:, :]))
                d3 = dbg_tensor("hT1", [128, 8192], BF16)
                s_out.dma(SY.dma_start(out=d3, in_=hT[1][:, :, :]))
                d4 = dbg_tensor("Am", [128, 8])
                s_out.dma(SY.dma_start(out=d4, in_=Am[:, :]))
        if stop <= 1:
            W(SY, s_out)
            return nc, dbg_out

        with contextlib.ExitStack() as es:
            e_t = [sb(es, f"e_t{i}", [128, 512]) for i in range(3)]
            S_t = [sb(es, f"S_t{i}", [128, 512], BF16) for i in range(3)]
            w_t = [sb(es, f"w_t{i}", [128, 512], BF16) for i in range(2)]
            R_t = [sb(es, f"R_t{i}", [128, 512], BF16) for i in range(2)]
            mask = [sb(es, f"mask{i}", [128, 512], BF16) for i in range(4)]
            pz = [ps(es, f"pz{i}", [128, 512]) for i in range(2)]
            pw = [ps(es, f"pw{i}", [128, 512]) for i in range(3)]
            po = [ps(es, f"po{i}", [128, 512]) for i in range(2)]
            s_pe = sig(es, "a_pe")
            s_act = sig(es, "a_act")
            s_pool = sig(es, "a_pool")
            s_v = sig(es, "a_v")
            for rel in range(4):
                s_pool.inc(G.affine_select(out=mask[rel][:], in_=zeros_bf[:], pattern=[[1, 512]], compare_op=ALU.is_gt,
                                           fill=-30000.0, base=-128 * rel, channel_multiplier=-1))
            W(T, s_pool)
            pairs = []
            groups = []
            for h in range(8):
                for c in range(8):
                    g = len(groups)
                    kbs = list(range(4 * c + 3, -1, -1))
                    groups.append((h, c, len(pairs), len(pairs) + len(kbs) - 1))
                    for kb in kbs:
                        pairs.append((h, c, kb, g))
            n = len(pairs)
            vA, vB, vC, vD, vE, vF, vG = {}, {}, {}, {}, {}, {}, {}
            gev = {}

            def ops(j):
                h, c, kb, g = pairs[j]
                hp = (h % 2) * 64
                kh = kT[hp:hp + 64, h // 2, kb * 128:(kb + 1) * 128]
                qh = qT[hp:hp + 64, h // 2, c * 512:(c + 1) * 512]
                rel = kb - 4 * c
                first = (j == groups[g][2])
                last = (j == groups[g][3])
                return h, c, kb, g, hp, kh, qh, rel, first, last

            def colsl(j):
                rel = pairs[j][2] - 4 * pairs[j][1]
                return slice(128 * rel, 512) if rel >= 1 else slice(0, 512)

            def zmm(out, j, stop_):
                h, c, kb, g, hp, kh, qh, rel, first, last = ops(j)
                cs = colsl(j)
                if rel >= 0:
                    T.matmul(out[:, cs], lhsT=kh, rhs=qh[:, cs], start=True, stop=False)
                    return T.matmul(out[:, cs], lhsT=ident_bf[:], rhs=mask[rel][:, cs], start=False, stop=stop_)
                return T.matmul(out[:, cs], lhsT=kh, rhs=qh[:, cs], start=True, stop=stop_)

            def st_A(j):
                if j - 2 >= 0:
                    W(T, s_act, vB[j - 2])
                vA[j] = s_pe.inc(zmm(pz[j % 2], j, True))

            def st_B(j):
                W(A, s_pe, vA[j])
                cs = colsl(j)
                vB[j] = s_act.inc(A.activation(out=e_t[j % 3][:, cs], in_=pz[j % 2][:, cs], func=AF.Exp))

            def st_C(j):
                W(A, s_act, vB[j])
                if j - 3 >= 0:
                    W(A, s_pe, vD[j - 3])
                    if (j - 3) in vE:
                        W(A, s_pool, vE[j - 3])
                cs = colsl(j)
                vC[j] = s_act.inc(A.activation(out=S_t[j % 3][:, cs], in_=e_t[j % 3][:, cs], func=AF.Ln, bias=1.0, scale=1.0))

            def st_E(j):
                h, c, kb, g, hp, kh, qh, rel, first, last = ops(j)
                if last:
                    return
                cs = colsl(j)
                W(G, s_act, vC[j])
                if j - 1 >= 0:
                    W(G, s_pe, vD[j - 1])
                Rn = R_t[(j + 1) % 2]
                if cs.start > 0:
                    s_pool.inc(G.memset(Rn[:, 0:cs.start], 0.0))
                if first:
                    vE[j] = s_pool.inc(G.tensor_copy(out=Rn[:, cs], in_=S_t[j % 3][:, cs]))
                else:
                    W(G, s_pool, vE[j - 1])
                    vE[j] = s_pool.inc(G.tensor_tensor(out=Rn[:, cs], in0=R_t[j % 2][:, cs], in1=S_t[j % 3][:, cs],
                                                       op=ALU.add))

            def st_D(j):
                h, c, kb, g, hp, kh, qh, rel, first, last = ops(j)
                W(T, s_act, vC[j])
                if j - 3 >= 0:
                    W(T, s_act, vF[j - 3])
                if not first:
                    W(T, s_pool, vE[j - 1])
                cs = colsl(j)
                zmm(pw[j % 3], j, False)
                ins = T.matmul(pw[j % 3][:, cs], lhsT=negU_bf[:], rhs=S_t[j % 3][:, cs], start=False, stop=first)
                if not first:
                    ins = T.matmul(pw[j % 3][:, cs], lhsT=negones_bf[:], rhs=R_t[j % 2][:, cs], start=False, stop=True)
                vD[j] = s_pe.inc(ins)

            def st_F(j):
                W(A, s_pe, vD[j])
                if j - 2 >= 0:
                    W(A, s_pe, vG[j - 2])
                cs = colsl(j)
                vF[j] = s_act.inc(A.activation(out=w_t[j % 2][:, cs], in_=pw[j % 3][:, cs], func=AF.Exp))

            def st_G(j):
                h, c, kb, g, hp, kh, qh, rel, first, last = ops(j)
                W(T, s_act, vF[j])
                if first and g - 2 >= 0:
                    W(T, s_v, gev[g - 2])
                cs = colsl(j)
                if first:
                    T.matmul(po[g % 2][hp:hp + 64, :], lhsT=zeros_bf[:, 0:64], rhs=mask[0][:], start=True, stop=False)
                ins = T.matmul(po[g % 2][hp:hp + 64, cs], lhsT=vtok[:, kb, h * 64:(h + 1) * 64], rhs=w_t[j % 2][:, cs],
                               start=False, stop=last)
                vG[j] = s_pe.inc(ins)
                if last:
                    W(V, s_pe, vG[j])
                    gev[g] = s_v.inc(V.tensor_copy(out=yattnT[hp:hp + 64, h // 2, c * 512:(c + 1) * 512],
                                                   in_=po[g % 2][hp:hp + 64, :]))

            for i in range(-3, n + 2):
                if 0 <= i + 3 < n:
                    st_A(i + 3)
                if 0 <= i + 2 < n:
                    st_B(i + 2)
                if 0 <= i + 1 < n:
                    st_C(i + 1)
                if 0 <= i < n:
                    st_D(i)
                if 0 <= i + 1 < n:
                    st_E(i + 1)
                if 0 <= i - 1 < n:
                    st_F(i - 1)
                if 0 <= i - 2 < n:
                    st_G(i - 2)
            for e in (A, V, T, G, SY):
                W(e, s_v)
                W(e, s_pe)
                W(e, s_act)
                W(e, s_pool)
            if dbg and stop == 2:
                dq = dbg_tensor("yattn", [128, 16384], BF16)
                s_out.dma(SY.dma_start(out=dq, in_=RB[:, :]))
        if stop <= 2:
            W(SY, s_out)
            return nc, dbg_out

        ENG = (V, A, G, T, SY)

        def barrier(sigs, engines=ENG):
            for e in engines:
                for s_ in sigs:
                    W(e, s_)

        ylruT = RA[:, 0:16384].rearrange("p (c t) -> p c t", c=4)
        with contextlib.ExitStack() as es:
            win = RA[:, 16384:24576].rearrange("p (k n) -> p k n", k=8)
            hT = [RA[:, 24576 + i * 8192:24576 + (i + 1) * 8192].rearrange("p (k t) -> p k t", k=8) for i in range(2)]
            xn_bufs = [RA[:, 40960 + i * 1024:40960 + (i + 1) * 1024] for i in range(2)]
            xt = [sb(es, f"xtb{i}", [128, D]) for i in range(2)]
            HW_ = 512
            xpad = [sb(es, f"xpad{i}", [128, HW_ + 3]) for i in range(2)]
            xc = [sb(es, f"xc{i}", [128, HW_]) for i in range(2)]
            r_t = [sb(es, f"r_t{i}", [128, HW_]) for i in range(2)]
            ig_t = [sb(es, f"ig_t{i}", [128, HW_]) for i in range(2)]
            b_t = [sb(es, f"b_t{i}", [128, HW_]) for i in range(2)]
            h_t = [sb(es, f"h_t{i}", [128, HW_]) for i in range(2)]
            gg = [sb(es, f"gg{i}", [128, HW_]) for i in range(2)]
            g2 = [sb(es, f"g2{i}", [128, HW_]) for i in range(2)]
            xcb = [sb(es, f"xcb{i}", [128, HW_], BF16) for i in range(2)]
            rgbd_b = sb(es, "rgbd_b", [128, 4, 128], BF16)
            igbd_b = sb(es, "igbd_b", [128, 4, 128], BF16)
            rgbd = sb(es, "rgbd", [128, 4, 128])
            igbd = sb(es, "igbd", [128, 4, 128])
            convw = sb(es, "convw", [128, 16])
            lruv = sb(es, "lruv", [128, 16])
            cA = sb(es, "cA", [128, 4])
            tmp4 = sb(es, "tmp4", [128, 4])
            hlast = sb(es, "hlast", [128, 4])
            xtail = sb(es, "xtail", [128, 4, 3])
            pT = [ps(es, f"pTb{i}", [128, 8, 128], BF16) for i in range(2)]
            pxr = [ps(es, f"pxr{i}", [128, HW_]) for i in range(2)]
            pgr = [ps(es, f"pgr{i}", [128, HW_]) for i in range(2)]
            pgt = [ps(es, f"pgt{i}", [128, HW_]) for i in range(2)]
            s_w = sig(es, "lw")
            sg = (sig(es, "lx", 2), sig(es, "la"), sig(es, "lv"), sig(es, "lt"))
            s_x, s_act, s_v, s_pe = sg
            s_g = sig(es, "lg")
            allsig = [s_w, s_x, s_act, s_v, s_pe, s_g]

            def v(ins):
                val = s_v.inc(ins); W(V, s_v, val); return val

            def a(ins):
                val = s_act.inc(ins); W(A, s_act, val); return val

            def g(ins):
                val = s_g.inc(ins); W(G, s_g, val); return val

            wv = win_d.rearrange("(k p) n -> p k n", p=128)
            for kh in range(2):
                s_w.dma(G.dma_start(out=win[:, kh * 4:(kh + 1) * 4, :], in_=wv[:, kh * 4:(kh + 1) * 4, 1536:2560]))
            v(V.memset(rgbd[:], 0.0))
            v(V.memset(igbd[:], 0.0))
            v(V.memset(hlast[:], 0.0))
            v(V.memset(xtail[:], 0.0))
            W(SY, s_v)
            for cc in range(4):
                for hh in range(2):
                    s_w.dma(SY.dma_start(out=rgbd[hh * 64:(hh + 1) * 64, cc, hh * 64:(hh + 1) * 64], in_=rgw_d[2 * cc + hh]))
                    s_w.dma(SY.dma_start(out=igbd[hh * 64:(hh + 1) * 64, cc, hh * 64:(hh + 1) * 64], in_=igw_d[2 * cc + hh]))
            s_w.dma(SY.dma_start(out=convw[:], in_=convw_d))
            s_w.dma(SY.dma_start(out=lruv[:], in_=lruv_d))
            barrier([s_w])
            v(V.tensor_copy(out=rgbd_b[:], in_=rgbd[:]))
            v(V.tensor_copy(out=igbd_b[:], in_=igbd[:]))
            lam = lruv[:, :].rearrange("p (c f) -> p c f", f=4)[:, :, 3]
            a(A.activation(out=tmp4[:], in_=lam, func=AF.Exp, scale=-1.0))
            a(A.activation(out=tmp4[:], in_=tmp4[:], func=AF.Ln, bias=1.0, scale=1.0))
            W(V, s_act)
            v(V.tensor_scalar(out=cA[:], in0=tmp4[:], scalar1=-8.0, scalar2=None, op0=ALU.mult))
            barrier(allsig)
            hist2 = {}
            un = {}

            def S1(u, q, cc, hf, hq):
                p = u % 2
                U = un[u] = {}
                cw = lambda j: convw[:, cc * 4 + j:cc * 4 + j + 1]
                lv = lambda j: lruv[:, cc * 4 + j:cc * 4 + j + 1]
                tsl = slice(hf * HW_, (hf + 1) * HW_)
                if u - 2 in un:
                    P_ = un[u - 2]
                    for e in (T, V, A, G):
                        W(e, s_v, P_["v_end"])
                        W(e, s_act, P_["a_end"])
                        W(e, s_pe, P_["t_end"])
                        W(e, s_g, P_["g_end"])
                for k in range(8):
                    T.matmul(pxr[p][:], lhsT=win[:, k, cc * 128:(cc + 1) * 128], rhs=hq[:, k, tsl], start=(k == 0), stop=(k == 7))
                for k in range(8):
                    ins = T.matmul(pgr[p][:], lhsT=win[:, k, 512 + cc * 128:512 + (cc + 1) * 128], rhs=hq[:, k, tsl],
                                   start=(k == 0), stop=(k == 7))
                vt = s_pe.inc(ins)
                W(V, s_pe, vt)
                W(A, s_pe, vt)
                v(V.tensor_copy(out=xpad[p][:, 0:3], in_=xtail[:, cc, :]))
                v(V.tensor_copy(out=xpad[p][:, 3:HW_ + 3], in_=pxr[p][:]))
                v(V.tensor_copy(out=xtail[:, cc, :], in_=xpad[p][:, HW_:HW_ + 3]))
                v(V.tensor_scalar(out=xc[p][:], in0=xpad[p][:, 0:HW_], scalar1=cw(0), scalar2=lv(0), op0=ALU.mult, op1=ALU.add))
                for j in range(1, 4):
                    vxc = v(V.scalar_tensor_tensor(out=xc[p][:], in0=xpad[p][:, j:j + HW_], scalar=cw(j), in1=xc[p][:],
                                                   op0=ALU.mult, op1=ALU.add))
                va = a(A.activation(out=gg[p][:], in_=pgr[p][:], func=AF.Identity))
                W(G, s_v, vxc)
                vxb = g(G.tensor_copy(out=xcb[p][:], in_=xc[p][:]))
                W(T, s_g, vxb)
                W(G, s_act, va)
                g(G.tensor_tensor(out=g2[p][:], in0=gg[p][:], in1=gg[p][:], op=ALU.mult))
                g(G.tensor_scalar(out=g2[p][:], in0=g2[p][:], scalar1=0.044715, scalar2=1.0, op0=ALU.mult, op1=ALU.add))
                vg = g(G.tensor_tensor(out=g2[p][:], in0=g2[p][:], in1=gg[p][:], op=ALU.mult))
                ins = T.matmul(pgt[p][:], lhsT=rgbd_b[:, cc, :], rhs=xcb[p][:], start=True, stop=True)
                vt = s_pe.inc(ins)
                W(A, s_pe, vt)
                va = a(A.activation(out=r_t[p][:], in_=pgt[p][:], func=AF.Sigmoid, bias=lv(1), scale=1.0))
                W(T, s_act, va)
                ins = T.matmul(pgt[p][:], lhsT=igbd_b[:, cc, :], rhs=xcb[p][:], start=True, stop=True)
                vt = s_pe.inc(ins)
                W(A, s_pe, vt)
                a(A.activation(out=ig_t[p][:], in_=pgt[p][:], func=AF.Sigmoid, bias=lv(2), scale=1.0))
                W(A, s_g, vg)
                vgs = a(A.activation(out=g2[p][:], in_=g2[p][:], func=AF.Sigmoid, scale=1.5957691216))
                W(G, s_act, vgs)
                vg = g(G.tensor_tensor(out=g2[p][:], in0=g2[p][:], in1=gg[p][:], op=ALU.mult))
                va = a(A.activation(out=r_t[p][:], in_=r_t[p][:], func=AF.Exp, scale=cA[:, cc:cc + 1]))
                U["a"] = va
                U["t_end"] = vt
                U["g_end"] = vg
                U["cc"] = cc
                U["q"] = q
                U["hf"] = hf

            def S2a(u):
                p = u % 2
                U = un[u]
                cc, q, hf = U["cc"], U["q"], U["hf"]
                W(V, s_act, U["a"])
                v(V.scalar_tensor_tensor(out=b_t[p][:], in0=r_t[p][:], scalar=-1.0, in1=r_t[p][:], op0=ALU.mult,
                                         op1=ALU.mult))
                vb = v(V.tensor_scalar(out=b_t[p][:], in0=b_t[p][:], scalar1=1.0, scalar2=1e-30, op0=ALU.add,
                                       op1=ALU.max))
                W(A, s_v, vb)
                va = a(A.activation(out=b_t[p][:], in_=b_t[p][:], func=AF.Sqrt))
                U["a_end"] = va

            def S2b(u):
                p = u % 2
                U = un[u]
                cc, q, hf = U["cc"], U["q"], U["hf"]
                W(V, s_act, U["a_end"])
                v(V.tensor_tensor(out=b_t[p][:], in0=b_t[p][:], in1=ig_t[p][:], op=ALU.mult))
                v(V.tensor_tensor(out=b_t[p][:], in0=b_t[p][:], in1=xc[p][:], op=ALU.mult))
                v(V.tensor_tensor_scan(out=h_t[p][:], data0=r_t[p][:], data1=b_t[p][:], initial=hlast[:, cc:cc + 1],
                                       op0=ALU.mult, op1=ALU.add))
                v(V.tensor_copy(out=hlast[:, cc:cc + 1], in_=h_t[p][:, HW_ - 1:HW_]))
                W(V, s_g, U["g_end"])
                t0 = q * 1024 + hf * HW_
                U["v_end"] = v(V.tensor_tensor(out=ylruT[:, cc, t0:t0 + HW_], in0=g2[p][:], in1=h_t[p][:], op=ALU.mult))

            u = 0
            for q in range(4):
                hq = hT[q % 2]
                v_h = emit_norm_transpose(es, list(range(q * 8, q * 8 + 8)),
                                          lambda j, hq=hq: hq[:, :, j * 128:(j + 1) * 128], xn_bufs, pT, xt, sg, hist2)
                barrier(allsig)
                first = u
                for cc in range(4):
                    for hf in range(2):
                        if u - 1 >= first:
                            S2a(u - 1)
                        S1(u, q, cc, hf, hq)
                        if u - 1 >= first:
                            S2b(u - 1)
                        u += 1
                S2a(u - 1)
                S2b(u - 1)
                barrier(allsig)
            if dbg and stop == 3:
                dq = dbg_tensor("ylru", [128, 16384], BF16)
                s_out.dma(SY.dma_start(out=dq, in_=RA[:, 0:16384]))
                d2 = dbg_tensor("rstd3", [128, 32])
                s_out.dma(SY.dma_start(out=d2, in_=rstdall[:, :]))
                d3 = dbg_tensor("hT23", [128, 16384], BF16)
                s_out.dma(SY.dma_start(out=d3, in_=RA[:, 24576:40960]))
        if stop <= 3:
            W(SY, s_out)
            return nc, dbg_out

        M8all = sb(top, "M8all", [128, 32, 8])
        Lall = sb(top, "Lall", [128, 32, 32])
        rank_all = sb(top, "rank_all", [128, 32, 32])
        cnt_run = sb(top, "cnt_run", [128, 32])
        dest4f = sb(top, "dest4f", [128, 32, 4])
        dest4i = sb(top, "dest4i", [128, 128], I32)
        p4 = sb(top, "p4", [128, 32, 4])
        widx = sb(top, "widx", [128, NBP], I32)
        bidx = sb(top, "bidx", [128, NBP], I32)

        with contextlib.ExitStack() as es:
            wout = RA[:, 16384:24576].rearrange("p (k n) -> p k n", k=8)
            xt = [sb(es, f"x3_{i}", [128, D]) for i in range(2)]
            x1 = [sb(es, f"x1_{i}", [128, D]) for i in range(2)]
            t1 = sb(es, "t1", [128, D])
            h2 = sb(es, "h2", [128, D])
            h2b = [sb(es, f"h2b{i}", [128, D], BF16) for i in range(2)]
            h2T = sb(es, "h2T", [128, 8, 128])
            ysq = sb(es, "ysq", [128, 8, 128], BF16)
            rw = sb(es, "rw", [128, 8, 32])
            rb_row = sb(es, "rb_row", [128, 32])
            outg = sb(es, "outg", [128, 8])
            sd2 = sb(es, "sd2", [128, 2])
            rs2 = sb(es, "rs2", [128, 2])
            ss3 = sb(es, "ss3", [128, 1])
            rs3 = sb(es, "rs3", [128, 1])
            Mf = sb(es, "Mf", [128, 32])
            Mbf = sb(es, "Mbf", [128, 32], BF16)
            PA = [ps(es, f"PA{i}", [128, 512]) for i in range(2)]
            PL = [ps(es, f"PL{i}", [128, 512]) for i in range(2)]
            ptr = ps(es, "ptr", [128, 4, 128])
            psm = ps(es, "psm", [128, 512])
            psl = ps(es, "psl", [128, 512])
            psr = ps(es, "psr", [128, 512])
            s_d = sig(es, "3d", 2)
            s_v = sig(es, "3v")
            s_act = sig(es, "3a")
            s_pe = sig(es, "3t")
            s_st = sig(es, "3st", 4)
            allsig = [s_d, s_v, s_act, s_pe, s_st]

            def v(ins):
                val = s_v.inc(ins); W(V, s_v, val); return val

            def a(ins):
                val = s_act.inc(ins); W(A, s_act, val); return val

            s_d.dma(SY.dma_start(out=rw[:], in_=rw_d.rearrange("(k p) n -> p k n", p=128)))
            s_d.dma(SY.dma_start(out=rb_row[:], in_=rb_d.broadcast_to([128, 32])))
            s_d.dma(SY.dma_start(out=outg[:], in_=outg_d))
            v(V.memset(cnt_run[:], 0.0))
            wov = wout_d.rearrange("(k p) n -> p k n", p=128)
            for k in range(8):
                vd = s_d.dma(SY.dma_start(out=t1[:], in_=wov[:, k, :]))
                W(V, s_d, vd)
                vv = v(V.tensor_scalar(out=wout[:, k, :], in0=t1[:], scalar1=outg[:, k:k + 1], scalar2=None, op0=ALU.mult))
                W(SY, s_v, vv)
            barrier(allsig)
            s_g = sig(es, "3g")
            allsig.append(s_g)
            st3 = {tt: {} for tt in range(NT)}

            def vv(ins):
                return s_v.inc(ins)

            def stA(tt):
                b = tt % 2
                S_ = st3[tt]
                tsl = slice(tt * 128, (tt + 1) * 128)
                if tt - 2 >= 0:
                    W(SY, s_g, st3[tt - 2]["x1"])
                S_["x"] = s_d.dma(SY.dma_start(out=xt[b][:], in_=x_d[tsl, :]), lane=b)
                if tt - 1 >= 0:
                    W(G, s_pe, st3[tt - 1]["stats"])
                s_g.inc(G.tensor_tensor(out=ysq[:, 0:4, :], in0=yattnT[:, :, tsl], in1=yattnT[:, :, tsl], op=ALU.mult))
                vq = s_g.inc(G.tensor_tensor(out=ysq[:, 4:8, :], in0=ylruT[:, :, tsl], in1=ylruT[:, :, tsl], op=ALU.mult))
                W(T, s_g, vq)
                if tt - 1 >= 0:
                    W(T, s_act, st3[tt - 1]["sd2"])
                for gI in range(2):
                    for c in range(4):
                        ins = T.matmul(psm[:, gI:gI + 1], lhsT=ysq[:, gI * 4 + c, :], rhs=ones_bf[:, 0:1], start=(c == 0),
                                       stop=(c == 3))
                S_["stats"] = s_pe.inc(ins)
                if tt - 1 >= 0:
                    W(T, s_v, st3[tt - 1]["comb"])
                for dh in range(2):
                    for c in range(4):
                        T.matmul(PA[dh][:], lhsT=yattnT[:, c, tsl], rhs=wout[:, c, dh * 512:(dh + 1) * 512], start=(c == 0),
                                 stop=(c == 3))
                    for c in range(4):
                        ins = T.matmul(PL[dh][:], lhsT=ylruT[:, c, tsl], rhs=wout[:, 4 + c, dh * 512:(dh + 1) * 512],
                                       start=(c == 0), stop=(c == 3))
                S_["op"] = s_pe.inc(ins)
                W(A, s_pe, S_["stats"])
                if tt - 1 >= 0:
                    W(A, s_v, st3[tt - 1]["rs2"])
                S_["sd2"] = s_act.inc(A.activation(out=sd2[:], in_=psm[:, 0:2], func=AF.Sqrt, scale=1.0 / 512, bias=EPS))

            def stB1(tt):
                b = tt % 2
                S_ = st3[tt]
                tsl = slice(tt * 128, (tt + 1) * 128)
                W(V, s_act, S_["sd2"])
                S_["rs2"] = vv(V.reciprocal(out=rs2[:], in_=sd2[:]))
                W(V, s_v, S_["rs2"])
                W(V, s_pe, S_["op"])
                W(V, s_d, S_["x"])
                if tt - 1 >= 0:
                    W(V, s_act, st3[tt - 1]["sq"])
                for dh in range(2):
                    cs = slice(dh * 512, (dh + 1) * 512)
                    v_ = vv(V.tensor_scalar(out=t1[:, cs], in0=PA[dh][:], scalar1=rs2[:, 0:1], scalar2=None, op0=ALU.mult))
                    W(V, s_v, v_)
                    v_ = vv(V.scalar_tensor_tensor(out=t1[:, cs], in0=PL[dh][:], scalar=rs2[:, 1:2], in1=t1[:, cs], op0=ALU.mult,
                                                   op1=ALU.add))
                S_["comb"] = v_
                W(G, s_v, v_)
                vg_ = s_g.inc(G.tensor_tensor(out=t1[:], in0=t1[:], in1=gm_row, op=ALU.mult))
                W(G, s_g, vg_)
                if tt - 2 >= 0:
                    W(G, s_st, st3[tt - 2]["x1st"])
                    W(G, s_v, st3[tt - 2]["h2"])
                S_["x1"] = s_g.inc(G.tensor_tensor(out=x1[b][:], in0=t1[:], in1=xt[b][:], op=ALU.add))
                W(SY, s_g, S_["x1"])
                S_["x1st"] = s_st.dma(SY.dma_start(out=X1[tsl, :], in_=x1[b][:]), lane=b)
                W(A, s_g, S_["x1"])
                va = s_act.inc(A.activation(out=t1[:], in_=x1[b][:], func=AF.Square, accum_out=ss3[:]))
                S_["sq"] = va
                W(A, s_act, va)
                if tt - 1 >= 0:
                    W(A, s_v, st3[tt - 1]["rs3"])
                S_["ss3"] = s_act.inc(A.activation(out=ss3[:], in_=ss3[:], func=AF.Sqrt, scale=1.0 / D, bias=EPS))

            def stB2(tt):
                b = tt % 2
                S_ = st3[tt]
                tsl = slice(tt * 128, (tt + 1) * 128)
                W(V, s_act, S_["ss3"])
                S_["rs3"] = vv(V.reciprocal(out=rs3[:], in_=ss3[:]))
                W(V, s_v, S_["rs3"])
                if tt - 1 >= 0:
                    W(V, s_pe, st3[tt - 1]["tr"])
                    W(V, s_act, st3[tt - 1]["h2b"])
                v_ = vv(V.scalar_tensor_tensor(out=h2[:], in0=x1[b][:], scalar=rs3[:, 0:1], in1=Af_row, op0=ALU.mult,
                                               op1=ALU.mult))
                W(V, s_v, v_)
                S_["h2"] = vv(V.tensor_tensor(out=h2[:], in0=h2[:], in1=shf_row, op=ALU.add))
                W(A, s_v, S_["h2"])
                if tt - 2 >= 0:
                    W(A, s_st, st3[tt - 2]["h2st"])
                S_["h2b"] = s_act.inc(A.activation(out=h2b[b][:], in_=h2[:], func=AF.Copy))
                W(SY, s_act, S_["h2b"])
                S_["h2st"] = s_st.dma(SY.dma_start(out=H2[tsl, :], in_=h2b[b][:]), lane=2 + b)
                W(T, s_v, S_["h2"])
                if tt - 1 >= 0:
                    W(T, s_act, st3[tt - 1]["h2T"])
                for half in range(2):
                    if half == 1:
                        W(T, s_act, S_["h2T"])
                    for k in range(4):
                        kk = half * 4 + k
                        ins = T.transpose(ptr[:, k, :], h2[:, kk * 128:(kk + 1) * 128], ident_f[:])
                    S_["tr"] = s_pe.inc(ins)
                    W(A, s_pe, S_["tr"])
                    if tt - 1 >= 0 and half == 0:
                        W(A, s_pe, st3[tt - 1]["rt"])
                    S_["h2T"] = s_act.inc(A.activation(out=h2T[:, half * 4:(half + 1) * 4, :], in_=ptr[:], func=AF.Identity))
                W(T, s_act, S_["h2T"])
                if tt - 1 >= 0:
                    W(T, s_v, st3[tt - 1]["L"])
                for k in range(8):
                    ins = T.matmul(psl[:, 32:64], lhsT=h2T[:, k, :], rhs=rw[:, k, :], start=(k == 0), stop=(k == 7))
                S_["rt"] = s_pe.inc(ins)

            def stC(tt):
                S_ = st3[tt]
                W(V, s_pe, S_["rt"])
                S_["L"] = vv(V.tensor_tensor(out=Lall[:, tt, :], in0=psl[:, 32:64], in1=rb_row[:], op=ALU.add))
                W(V, s_v, S_["L"])
                v_ = vv(V.max(out=M8all[:, tt, :], in_=Lall[:, tt, :]))
                W(V, s_v, v_)
                if tt - 1 >= 0:
                    W(V, s_pe, st3[tt - 1]["rk"])
                v_ = vv(V.tensor_scalar(out=Mf[:], in0=Lall[:, tt, :], scalar1=M8all[:, tt, 3:4], scalar2=None, op0=ALU.is_ge))
                W(V, s_v, v_)
                vm = vv(V.tensor_copy(out=Mbf[:], in_=Mf[:]))
                W(T, s_v, vm)
                if tt - 1 >= 0:
                    W(T, s_v, st3[tt - 1]["cnt"])
                T.matmul(psr[:, 64:96], lhsT=tri_bf[:], rhs=Mbf[:], start=True, stop=True)
                ins = T.matmul(psr[:, 96:128], lhsT=ones_bf[:], rhs=Mbf[:], start=True, stop=True)
                S_["rk"] = s_pe.inc(ins)

            def stC2(tt):
                S_ = st3[tt]
                W(V, s_pe, S_["rk"])
                if tt - 1 >= 0:
                    W(V, s_v, st3[tt - 1]["cnt"])
                v_ = vv(V.tensor_tensor(out=rank_all[:, tt, :], in0=psr[:, 64:96], in1=cnt_run[:], op=ALU.add))
                W(V, s_v, v_)
                S_["cnt"] = vv(V.tensor_tensor(out=cnt_run[:], in0=psr[:, 96:128], in1=cnt_run[:], op=ALU.add))

            stA(0)
            for tt in range(NT):
                stB1(tt)
                if tt + 1 < NT:
                    stA(tt + 1)
                if tt - 1 >= 0:
                    stC(tt - 1)
                stB2(tt)
                if tt - 1 >= 0:
                    stC2(tt - 1)
            stC(NT - 1)
            stC2(NT - 1)
            barrier(allsig)

        with contextlib.ExitStack() as es:
            padded = sb(es, "padded", [128, 32])
            pad_end = sb(es, "pad_end", [128, 32])
            pad_start = sb(es, "pad_start", [128, 32])
            tmp32 = sb(es, "tmp32", [128, 32])
            destE = sb(es, "destE", [128, 32, 32])
            oh = sb(es, "oh", [128, 32, 32])
            E4 = sb(es, "E4", [128, 32, 4])
            den = sb(es, "den", [128, 32])
            tokid = sb(es, "tokid", [128, 32, 16], I32)
            zt = sb(es, "zt", [128, NSLOT * 16 // 128], I32)
            jv_i = sb(es, "jv_i", [128, NBP], I32)
            jv = sb(es, "jv", [128, NBP])
            pid_i = sb(es, "pid_i", [128, NBP], I32)
            pid = sb(es, "pid", [128, NBP])
            cmp = sb(es, "cmp", [128, NBP, 32])
            be = sb(es, "be", [128, NBP])
            s_v = sig(es, "4v")
            s_act = sig(es, "4a")
            s_g = sig(es, "4g")
            s_z = sig(es, "4z")

            def v(ins):
                val = s_v.inc(ins); W(V, s_v, val); return val

            def g(ins):
                val = s_g.inc(ins); W(G, s_g, val); return val

            g(G.iota(out=tokid[:], pattern=[[128, 32], [0, 16]], base=0, channel_multiplier=1))
            g(G.iota(out=jv_i[:], pattern=[[BLK, NBP]], base=0, channel_multiplier=0))
            g(G.iota(out=pid_i[:], pattern=[[0, NBP]], base=0, channel_multiplier=1))
            g(G.memset(zt[:], 0))
            s_z.dma(G.dma_start(out=SLOT.rearrange("(p f) c -> p (f c)", p=128), in_=zt[:]))
            W(V, s_g)
            v(V.tensor_copy(out=jv[:], in_=jv_i[:]))
            v(V.tensor_copy(out=pid[:], in_=pid_i[:]))
            v(V.tensor_tensor(out=destE[:], in0=cnt_run[:, :].unsqueeze(2).to_broadcast([128, 32, 32]),
                              in1=jv[:, 0:32].unsqueeze(1).to_broadcast([128, 32, 32]), op=ALU.is_gt))
            v(V.tensor_reduce(out=tmp32[:], in_=destE[:], axis=AX.X, op=ALU.add))
            v(V.tensor_scalar(out=padded[:], in0=tmp32[:], scalar1=float(BLK), scalar2=None, op0=ALU.mult))
            v(V.tensor_tensor_scan(out=pad_end[:], data0=ones_f[:, 0:32], data1=padded[:], initial=0.0, op0=ALU.mult,
                                   op1=ALU.add))
            v(V.tensor_tensor(out=pad_start[:], in0=pad_end[:], in1=padded[:], op=ALU.subtract))
            v(V.tensor_tensor(out=destE[:], in0=rank_all[:], in1=pad_start[:, :].unsqueeze(1).to_broadcast([128, 32, 32]),
                              op=ALU.add))
            for k in range(4):
                v(V.tensor_tensor(out=oh[:], in0=Lall[:], in1=M8all[:, :, k:k + 1].to_broadcast([128, 32, 32]),
                                  op=ALU.is_equal))
                v(V.tensor_tensor(out=oh[:], in0=oh[:], in1=destE[:], op=ALU.mult))
                v(V.tensor_reduce(out=dest4f[:, :, k], in_=oh[:], axis=AX.X, op=ALU.add))
            v(V.tensor_copy(out=dest4i[:], in_=dest4f[:, :, :].rearrange("p a b -> p (a b)")))
            vv = v(V.tensor_tensor(out=E4[:], in0=M8all[:, :, 0:4], in1=M8all[:, :, 0:1].to_broadcast([128, 32, 4]),
                                   op=ALU.subtract))
            W(A, s_v, vv)
            va = s_act.inc(A.activation(out=E4[:], in_=E4[:], func=AF.Exp))
            W(V, s_act, va)
            v(V.tensor_reduce(out=den[:], in_=E4[:], axis=AX.X, op=ALU.add))
            v(V.reciprocal(out=den[:], in_=den[:]))
            v(V.tensor_tensor(out=p4[:], in0=E4[:], in1=den[:, :].unsqueeze(2).to_broadcast([128, 32, 4]), op=ALU.mult))
            v(V.tensor_tensor(out=cmp[:], in0=pad_end[:, :].unsqueeze(1).to_broadcast([128, NBP, 32]),
                              in1=jv[:, :].unsqueeze(2).to_broadcast([128, NBP, 32]), op=ALU.is_le))
            v(V.tensor_reduce(out=be[:], in_=cmp[:], axis=AX.X, op=ALU.add))
            v(V.tensor_scalar(out=be[:], in0=be[:], scalar1=31.0, scalar2=None, op0=ALU.min))
            v(V.tensor_copy(out=bidx[:], in_=be[:]))
            v(V.scalar_tensor_tensor(out=be[:], in0=be[:], scalar=128.0, in1=pid[:], op0=ALU.mult, op1=ALU.add))
            v(V.tensor_scalar(out=jv[:], in0=jv[:], scalar1=pad_end[:, 31:32], scalar2=None, op0=ALU.is_lt))
            v(V.tensor_scalar(out=pid[:], in0=pid[:], scalar1=0.0, scalar2=None, op0=ALU.is_equal))
            v(V.tensor_tensor(out=jv[:], in0=jv[:], in1=pid[:], op=ALU.max))
            v(V.tensor_scalar(out=be[:], in0=be[:], scalar1=-100000.0, scalar2=None, op0=ALU.add))
            v(V.tensor_tensor(out=be[:], in0=be[:], in1=jv[:], op=ALU.mult))
            v(V.tensor_scalar(out=be[:], in0=be[:], scalar1=100000.0, scalar2=None, op0=ALU.add))
            v(V.tensor_copy(out=widx[:], in_=be[:]))
            W(G, s_v)
            W(G, s_z)
            for tt in range(NT):
                for k in range(4):
                    s_z.dma(G.indirect_dma_start(out=SLOT[:, :],
                                                 out_offset=bass.IndirectOffsetOnAxis(ap=dest4i[:, tt * 4 + k:tt * 4 + k + 1], axis=0),
                                                 in_=tokid[:, tt, :], in_offset=None))
            barrier([s_v, s_act, s_g, s_z])
            if dbg and stop == 4:
                for nm, t_, shp, dt_ in [("Lall", Lall, [128, 1024], F32), ("M8all", M8all, [128, 256], F32),
                                         ("dest4i", dest4i, [128, 128], I32), ("p4", p4, [128, 128], F32),
                                         ("widx", widx, [128, NBP], I32), ("bidx", bidx, [128, NBP], I32),
                                         ("cnt", cnt_run, [128, 32], F32)]:
                    dd = dbg_tensor(nm, shp, dt_)
                    ap_ = t_[:] if len(t_.shape) == 2 else t_[:, :, :].rearrange("p a b -> p (a b)")
                    s_out.dma(SY.dma_start(out=dd, in_=ap_))
                dd = dbg_tensor("slot", [NSLOT, 16], I32)
                s_out.dma(SY.dma_start(out=dd, in_=SLOT))
                dd = dbg_tensor("x1", [S, D])
                s_out.dma(SY.dma_start(out=dd, in_=X1))
                dd = dbg_tensor("h2", [S, D], BF16)
                s_out.dma(SY.dma_start(out=dd, in_=H2))
        if stop <= 4:
            W(SY, s_out)
            return nc, dbg_out

        with contextlib.ExitStack() as es:
            wbuf = [[RA[:, (par * 3 + m) * 8192:(par * 3 + m + 1) * 8192].rearrange("p (k n) -> p k n", k=8)
                     for m in range(3)] for par in range(2)]
            xb = [RB[:, par * 4096:par * 4096 + QB * 1024].rearrange("p (q d) -> p q d", q=QB) for par in range(2)]
            xbT = RB[:, 8192:8192 + 8 * BLK].rearrange("p (k s) -> p k s", k=8)
            actT0 = RB[:, 12288:12288 + 8 * BLK].rearrange("p (k s) -> p k s", k=8)
            actT1 = sb(es, "actT1", [128, 8, BLK], BF16)
            actT = [actT0, actT1]
            bgt = [sb(es, f"bgt{i}", [128, 8]) for i in range(2)]
            but = [sb(es, f"but{i}", [128, 8]) for i in range(2)]
            bdt = [sb(es, f"bdt{i}", [128, D]) for i in range(2)]
            tki = [sb(es, f"tki{i}", [128, QB, 16], I32) for i in range(2)]
            ysb = sb(es, "ysb", [128, QB, D])
            g_t = [sb(es, f"g_t{i}", [128, BLK]) for i in range(2)]
            sg_t = [sb(es, f"sg_t{i}", [128, BLK]) for i in range(2)]
            u_t = [sb(es, f"u_t{i}", [128, BLK]) for i in range(2)]
            ptx = [ps(es, f"ptx{i}", [128, QB, 128], BF16) for i in range(2)]
            pg = [ps(es, f"pg{i}", [128, BLK]) for i in range(2)]
            pu = [ps(es, f"pu{i}", [128, BLK]) for i in range(2)]
            py = [ps(es, f"py{i}", [128, 512]) for i in range(2)]
            s_tk = sig(es, "5tk", 2)
            s_xb = sig(es, "5xb", 2)
            s_wgu = sig(es, "5wgu", 2)
            s_wd = sig(es, "5wd", 2)
            s_pe = sig(es, "5t")
            s_v = sig(es, "5v")
            s_act = sig(es, "5a")
            s_yd = sig(es, "5y")
            blk = {j: {} for j in range(NBLK)}
            wsrc = (wg_d, wu_d, wd_d)
            bc_reg = G.to_reg(4095)

            def wgather(sg_, par, m, j):
                for c4 in range(4):
                    val = sg_.dma(G.indirect_dma_start(
                        out=RA[:, (par * 3 + m) * 8192 + c4 * 2048:(par * 3 + m) * 8192 + (c4 + 1) * 2048], out_offset=None,
                        in_=wsrc[m][0], in_offset=bass.IndirectOffsetOnAxis(ap=widx[:, j:j + 1], axis=0),
                        element_offset=c4 * 4096 * 2048, bounds_check=bc_reg, oob_is_err=False), lane=par)
                return val

            def loads_gu(j):
                par = j % 2
                st = blk[j]
                if j - 2 >= 0:
                    W(G, s_pe, blk[j - 2]["gu_done"])
                    W(G, s_v, blk[j - 2]["sw_done"])
                    W(G, s_act, blk[j - 2]["sw_act"])
                for q in range(QB):
                    vtk = s_tk.dma(G.dma_start(out=tki[par][:, q, :],
                                               in_=SLOT[j * BLK + q * 128:j * BLK + (q + 1) * 128, :]), lane=par)
                W(G, s_tk, vtk)
                for q in range(QB):
                    st["xb"] = s_xb.dma(G.indirect_dma_start(
                        out=xb[par][:, q, :], out_offset=None, in_=H2[:, :],
                        in_offset=bass.IndirectOffsetOnAxis(ap=tki[par][:, q, 0:1], axis=0)), lane=par)
                wgather(s_wgu, par, 0, j)
                wgather(s_wgu, par, 1, j)
                s_wgu.dma(G.indirect_dma_start(out=bgt[par][:], out_offset=None, in_=bg_d[:, :],
                                               in_offset=bass.IndirectOffsetOnAxis(ap=widx[:, j:j + 1], axis=0),
                                               bounds_check=bc_reg, oob_is_err=False), lane=par)
                st["wgu"] = s_wgu.dma(G.indirect_dma_start(out=but[par][:], out_offset=None, in_=bu_d[:, :],
                                                           in_offset=bass.IndirectOffsetOnAxis(ap=widx[:, j:j + 1], axis=0),
                                                           bounds_check=bc_reg, oob_is_err=False), lane=par)

            def loads_d(j):
                par = j % 2
                st = blk[j]
                if j - 2 >= 0:
                    W(G, s_pe, blk[j - 2]["d_done"])
                    W(G, s_v, blk[j - 2]["y_done"])
                wgather(s_wd, par, 2, j)
                st["wd"] = s_wd.dma(G.indirect_dma_start(out=bdt[par][:], out_offset=None, in_=bd_d[:, :],
                                                         in_offset=bass.IndirectOffsetOnAxis(ap=bidx[:, j:j + 1], axis=0)), lane=par)

            cnt5 = {"pgi": 0, "pyi": 0}
            pg_free = {}
            py_free = {}

            def TK(j, k):
                par = j % 2
                st = blk[j]
                if k == 0:
                    W(T, s_xb, st["xb"])
                    st["txe"] = {}
                ev_done = st["txe"]
                pb = ptx[k % 2]
                if k >= 2:
                    W(T, s_v, ev_done[k - 2])
                elif j > 0:
                    W(T, s_v, blk[j - 1]["txe"][6 + k])
                for q in range(QB):
                    ins = T.transpose(pb[:, q, :], xb[par][:, q, k * 128:(k + 1) * 128], ident_bf[:])
                vt = s_pe.inc(ins)
                W(V, s_pe, vt)
                ev_done[k] = s_v.inc(V.tensor_copy(out=xbT[:, k, :], in_=pb[:, :, :].rearrange("p q s -> p (q s)")))

            def GU(j):
                par = j % 2
                st = blk[j]
                ev_done = st["txe"]
                W(T, s_v, ev_done[7])
                W(T, s_wgu, st["wgu"])
                W(V, s_wgu, st["wgu"])
                W(A, s_wgu, st["wgu"])
                if j - 2 >= 0:
                    W(V, s_pe, blk[j - 2]["d_done"])
                at = actT[j % 2]
                for fc in range(8):
                    pgi = cnt5["pgi"]
                    gb = pgi % 2
                    if pgi >= 2:
                        W(T, s_v, pg_free[pgi - 2])
                    for k in range(8):
                        T.matmul(pg[gb][:], lhsT=wbuf[par][0][:, k, fc * 128:(fc + 1) * 128], rhs=xbT[:, k, :],
                                 start=(k == 0), stop=(k == 7))
                    for k in range(8):
                        ins = T.matmul(pu[gb][:], lhsT=wbuf[par][1][:, k, fc * 128:(fc + 1) * 128], rhs=xbT[:, k, :],
                                       start=(k == 0), stop=(k == 7))
                    vt = s_pe.inc(ins)
                    W(V, s_pe, vt)
                    W(A, s_pe, vt)
                    v1 = s_v.inc(V.tensor_scalar(out=g_t[gb][:], in0=pg[gb][:], scalar1=bgt[par][:, fc:fc + 1], scalar2=7.0,
                                                 op0=ALU.add, op1=ALU.min))
                    W(A, s_v, v1)
                    a1 = s_act.inc(A.activation(out=sg_t[gb][:], in_=g_t[gb][:], func=AF.Sigmoid, scale=1.702))
                    a2 = s_act.inc(A.activation(out=u_t[gb][:], in_=pu[gb][:], func=AF.Identity,
                                                bias=but[par][:, fc:fc + 1], scale=1.0))
                    W(V, s_act, a2)
                    v2 = s_v.inc(V.tensor_scalar(out=u_t[gb][:], in0=u_t[gb][:], scalar1=7.0, scalar2=-7.0, op0=ALU.min,
                                                 op1=ALU.max))
                    v3 = s_v.inc(V.tensor_tensor(out=g_t[gb][:], in0=g_t[gb][:], in1=sg_t[gb][:], op=ALU.mult))
                    W(V, s_v, v3)
                    v4 = s_v.inc(V.scalar_tensor_tensor(out=at[:, fc, :], in0=u_t[gb][:], scalar=1.0, in1=g_t[gb][:],
                                                        op0=ALU.add, op1=ALU.mult))
                    pg_free[pgi] = v4
                    cnt5["pgi"] += 1
                st["gu_done"] = vt
                st["sw_done"] = v4
                st["sw_act"] = a2

            def DN(j, jn=None):
                par = j % 2
                st = blk[j]
                at = actT[j % 2]
                W(T, s_v, st["sw_done"])
                W(T, s_wd, st["wd"])
                W(V, s_wd, st["wd"])
                if j - 1 >= 0:
                    W(V, s_yd, blk[j - 1]["yd"])
                for q in range(QB):
                    for dh in range(2):
                        pyi = cnt5["pyi"]
                        yb = pyi % 2
                        if pyi >= 2:
                            W(T, s_v, py_free[pyi - 2])
                        for fc in range(8):
                            ins = T.matmul(py[yb][:], lhsT=at[:, fc, q * 128:(q + 1) * 128],
                                           rhs=wbuf[par][2][:, fc, dh * 512:(dh + 1) * 512], start=(fc == 0), stop=(fc == 7))
                        vt = s_pe.inc(ins)
                        W(V, s_pe, vt)
                        py_free[pyi] = s_v.inc(V.tensor_tensor(out=ysb[:, q, dh * 512:(dh + 1) * 512], in0=py[yb][:],
                                                               in1=bdt[par][:, dh * 512:(dh + 1) * 512], op=ALU.add))
                        cnt5["pyi"] += 1
                        if jn is not None:
                            TK(jn, q * 2 + dh)
                if jn is not None:
                    for k in range(2 * QB, 8):
                        TK(jn, k)
                st["d_done"] = vt
                st["y_done"] = py_free[cnt5["pyi"] - 1]
                W(SY, s_v, st["y_done"])
                st["yd"] = s_yd.dma(SY.dma_start(out=Yd[j * BLK:(j + 1) * BLK, :].rearrange("(q p) d -> p q d", p=128),
                                                 in_=ysb[:]))

            loads_gu(0)
            loads_d(0)
            loads_gu(1)
            loads_d(1)
            for k in range(8):
                TK(0, k)
            for j in range(NBLK + 1):
                if j < NBLK:
                    GU(j)
                    if j + 2 < NBLK:
                        loads_gu(j + 2)
                if j >= 1:
                    DN(j - 1, j + 1 if j + 1 < NBLK else None)
                    if j + 1 < NBLK:
                        loads_d(j + 1)
                elif NBLK > 1:
                    for k in range(8):
                        TK(1, k)
            barrier([s_pe, s_v, s_act, s_yd, s_wgu, s_wd, s_xb, s_tk])

        if stop <= 5:
            if dbg:
                dd = dbg_tensor("Y", [NSLOT, D])
                s_out.dma(SY.dma_start(out=dd, in_=Yd))
            W(SY, s_out)
            return nc, dbg_out

        with contextlib.ExitStack() as es:
            RAf = RA[:, :].bitcast(F32)

            class _V:
                def __init__(self, ap): self.ap = ap
                def __getitem__(self, idx): return self.ap

            def fv(i):
                return _V(RAf[:, i * D:(i + 1) * D])
            yg = [[fv(i * 4 + k) for k in range(4)] for i in range(3)]
            x1t = [fv(12 + i) for i in range(3)]
            ot = [fv(17 + i) for i in range(2)]
            ss6 = sb(es, "ss6", [128, 32])
            rs6 = sb(es, "rs6", [128, 32])
            s_g = sig(es, "6g", 3)
            s_d = sig(es, "6d", 5)
            s_v = sig(es, "6v")
            s_act = sig(es, "6a")
            hist = {}

            def v(ins):
                val = s_v.inc(ins); W(V, s_v, val); return val

            def a(ins):
                val = s_act.inc(ins); W(A, s_act, val); return val

            def loads(tt):
                b = tt % 3
                if tt - 3 >= 0:
                    W(G, s_v, hist[tt - 3]["acc"])
                    W(SY, s_v, hist[tt - 3]["acc"])
                for k in range(4):
                    vg = s_g.dma(G.indirect_dma_start(out=yg[b][k][:], out_offset=None, in_=Yd[:, :],
                                                      in_offset=bass.IndirectOffsetOnAxis(
                                                          ap=dest4i[:, tt * 4 + k:tt * 4 + k + 1], axis=0)), lane=b)
                vd = s_d.dma(SY.dma_start(out=x1t[b][:], in_=X1[tt * 128:(tt + 1) * 128, :]), lane=b)
                hist[tt] = {"g": vg, "d": vd}

            accs = [fv(15), fv(16)]

            def comb(tt):
                b = tt % 2
                b3 = tt % 3
                ac = accs[b]
                W(V, s_g, hist[tt]["g"])
                W(V, s_d, hist[tt]["d"])
                v(V.tensor_scalar(out=ac[:], in0=yg[b3][0][:], scalar1=p4[:, tt, 0:1], scalar2=None, op0=ALU.mult))
                for k in range(1, 4):
                    v(V.scalar_tensor_tensor(out=ac[:], in0=yg[b3][k][:], scalar=p4[:, tt, k:k + 1], in1=ac[:],
                                             op0=ALU.mult, op1=ALU.add))
                v(V.tensor_tensor(out=ac[:], in0=ac[:], in1=gf_row, op=ALU.mult))
                vacc = v(V.tensor_tensor(out=ac[:], in0=ac[:], in1=x1t[b3][:], op=ALU.add))
                hist[tt]["acc"] = vacc
                W(A, s_v, vacc)
                if tt - 2 >= 0:
                    W(A, s_d, hist[tt - 2]["od"])
                va = s_act.inc(A.activation(out=ot[b][:], in_=ac[:], func=AF.Square, accum_out=ss6[:, tt:tt + 1]))
                hist[tt]["sq"] = va

            def fin(tt):
                b = tt % 2
                ac = accs[b]
                W(V, s_act, hist[tt]["sd"])
                v(V.reciprocal(out=rs6[:, tt:tt + 1], in_=ss6[:, tt:tt + 1]))
                vo = v(V.scalar_tensor_tensor(out=ot[b][:], in0=ac[:], scalar=rs6[:, tt:tt + 1], in1=fing_row[:], op0=ALU.mult,
                                              op1=ALU.mult))
                hist[tt]["fin"] = vo
                W(SY, s_v, vo)
                hist[tt]["od"] = s_d.dma(SY.dma_start(out=out_d[tt * 128:(tt + 1) * 128, :], in_=ot[b][:]), lane=3 + b)

            def sqrt_(tt):
                W(A, s_act, hist[tt]["sq"])
                hist[tt]["sd"] = s_act.inc(A.activation(out=ss6[:, tt:tt + 1], in_=ss6[:, tt:tt + 1], func=AF.Sqrt,
                                                        scale=1.0 / D, bias=EPS))

            loads(0)
            loads(1)
            loads(2)
            for tt in range(NT + 1):
                if tt < NT:
                    if tt - 2 >= 0:
                        W(V, s_v, hist[tt - 2]["fin"])
                    comb(tt)
                    sqrt_(tt)
                if tt - 1 >= 0:
                    fin(tt - 1)
                    if tt + 2 < NT:
                        loads(tt + 2)
            barrier([s_d, s_v, s_act, s_g])
        W(SY, s_out)
    return nc, dbg_out


def _layout_w(w):
    w5 = w.reshape(32, 4, 2, 128, 1024)
    return np.ascontiguousarray(w5.transpose(1, 0, 3, 2, 4)).reshape(4, 4096, 2048)


_SHARED_CACHE = {}


def make_in_maps(inp, ncores=8):
    f = lambda a: np.ascontiguousarray(a, dtype=np.float32)
    sh = {
        "ada_w": f(inp["ada_w"][0]),
        "ada_b_row": f(inp["ada_b"][0].reshape(1, -1)),
        "ada_b_col": f(inp["ada_b"][0].reshape(48, 128).T),
        "mixg_col": f(inp["mix_norm_g"][0].reshape(8, 128).T),
        "w_in": f(inp["w_in"][0]),
        "convw_col": f(inp["conv_w"][0].T.reshape(4, 128, 4).transpose(1, 0, 2).reshape(128, 16)),
        "lru_cols": f(np.stack([inp["conv_b"][0], inp["rg_b"][0], inp["ig_b"][0], inp["lru_lambda"][0]], axis=-1)
                      .reshape(4, 128, 4).transpose(1, 0, 2).reshape(128, 16)),
        "rg_w": f(inp["rg_w"][0]),
        "ig_w": f(inp["ig_w"][0]),
        "outg_col": f(np.concatenate([inp["attn_out_g"][0], inp["lru_out_g"][0]]).reshape(8, 128).T),
        "w_out": f(inp["w_out"][0]),
        "ffn_g_row": f(inp["ffn_norm_g"][0].reshape(1, -1)),
        "final_g_row": f(inp["final_norm_g"].reshape(1, -1)),
        "router_w": f(inp["router_w"][0]),
        "router_b_row": f(inp["router_b"][0].reshape(1, -1)),
        "wg": _layout_w(f(inp["exp_w_gate"][0])),
        "wu": _layout_w(f(inp["exp_w_up"][0])),
        "wd": _layout_w(f(inp["exp_w_down"][0])),
        "bg": f(inp["exp_b_gate"][0].reshape(32, 8, 128).transpose(0, 2, 1).reshape(4096, 8)),
        "bu": f(inp["exp_b_up"][0].reshape(32, 8, 128).transpose(0, 2, 1).reshape(4096, 8)),
        "bd": f(inp["exp_b_down"][0]),
    }
    maps = []
    for b in range(ncores):
        m = dict(sh)
        m["x"] = f(inp["x"][b])
        m["c_col"] = f(inp["c"][b].reshape(8, 128).T)
        maps.append(m)
    return maps


def kernel(**inputs):
    nc, _ = build_program()
    maps = make_in_maps(inputs, 8)
    res = run_bass_kernel_spmd(nc, maps, core_ids=list(range(8)))
    return np.stack([np.asarray(r["out"], dtype=np.float32) for r in res.results], axis=0)
```

```python
import contextlib
import numpy as np
import concourse.bass as bass
import concourse.mybir as mybir
from concourse.bass_utils import run_bass_kernel_spmd

F32 = mybir.dt.float32
BF16 = mybir.dt.bfloat16
I32 = mybir.dt.int32
ALU = mybir.AluOpType
AF = mybir.ActivationFunctionType
AX = mybir.AxisListType

S = 4096
D = 1024
NT = S // 128
EPS = 1e-6
BLK = 512
QB = BLK // 128
NBLK = 32 + (S * 4 - 32) // BLK
NBP = 80
NSLOT = NBLK * BLK


class Sig:
    def __init__(self, nc, es, name, lanes=1):
        self.sems = [es.enter_context(nc.semaphore(f"{name}_l{i}")) for i in range(lanes)]
        self.cnt = [0] * lanes
        self.lanes = lanes

    @property
    def sem(self):
        return self.sems[0]

    @property
    def n(self):
        return self.cnt[0]

    def inc(self, ins, k=1, lane=0):
        ins.then_inc(self.sems[lane], k)
        self.cnt[lane] += k
        return self.cnt[lane] if self.lanes == 1 else (lane, self.cnt[lane])

    def dma(self, ins, lane=0):
        return self.inc(ins, 16, lane)


def W(eng, sig, val=None):
    if val is None:
        for l in range(sig.lanes):
            if sig.cnt[l] > 0:
                eng.wait_ge(sig.sems[l], sig.cnt[l])
        return
    if isinstance(val, tuple):
        lane, v = val
    else:
        lane, v = 0, val
    if v > 0:
        eng.wait_ge(sig.sems[lane], v)


def build_program(stop=99, dbg=None):
    nc = bass.Bass("TRN2", target_bir_lowering=False)
    dbg_out = {}

    def din(name, shape, dt=F32):
        return nc.dram_tensor(name, list(shape), dt, kind="ExternalInput").ap()

    x_d = din("x", [S, D])
    ccol_d = din("c_col", [128, 8])
    adaw_d = din("ada_w", [D, 6 * D])
    adab_row_d = din("ada_b_row", [1, 6 * D])
    adab_col_d = din("ada_b_col", [128, 48])
    mixg_d = din("mixg_col", [128, 8])
    win_d = din("w_in", [D, 2560])
    convw_d = din("convw_col", [128, 16])
    lruv_d = din("lru_cols", [128, 16])
    rgw_d = din("rg_w", [8, 64, 64])
    igw_d = din("ig_w", [8, 64, 64])
    outg_d = din("outg_col", [128, 8])
    wout_d = din("w_out", [D, D])
    ffng_d = din("ffn_g_row", [1, D])
    fing_d = din("final_g_row", [1, D])
    rw_d = din("router_w", [D, 32])
    rb_d = din("router_b_row", [1, 32])
    wg_d = din("wg", [4, 4096, 2048])
    wu_d = din("wu", [4, 4096, 2048])
    wd_d = din("wd", [4, 4096, 2048])
    bg_d = din("bg", [4096, 8])
    bu_d = din("bu", [4096, 8])
    bd_d = din("bd", [32, D])
    out_d = nc.dram_tensor("out", [S, D], F32, kind="ExternalOutput").ap()
    X1 = nc.dram_tensor("X1s", [S, D], F32, kind="Internal").ap()
    H2 = nc.dram_tensor("H2s", [S, D], BF16, kind="Internal").ap()
    Yd = nc.dram_tensor("Ys", [NSLOT, D], F32, kind="Internal").ap()
    SLOT = nc.dram_tensor("SLOTs", [NSLOT, 16], I32, kind="Internal").ap()

    def dbg_tensor(name, shape, dt=F32):
        t = nc.dram_tensor("dbg_" + name, list(shape), dt, kind="ExternalOutput").ap()
        dbg_out[name] = t
        return t

    V, A, G, T, SY = nc.vector, nc.scalar, nc.gpsimd, nc.tensor, nc.sync

    with contextlib.ExitStack() as top:
        def sb(es, name, shape, dt=F32):
            return es.enter_context(nc.sbuf_tensor(name, list(shape), dt))

        def ps(es, name, shape, dt=F32):
            return es.enter_context(nc.psum_tensor(name, list(shape), dt))

        sig_i = [0]

        def sig(es, name, lanes=1):
            sig_i[0] += 1
            return Sig(nc, es, f"{name}_{sig_i[0]}", lanes)

        s_out = sig(top, "out")

        ident_bf = sb(top, "ident_bf", [128, 128], BF16)
        ident_f = sb(top, "ident_f", [128, 128], F32)
        ones_f = sb(top, "ones_f", [128, 128], F32)
        ones_bf = sb(top, "ones_bf", [128, 128], BF16)
        negones_bf = sb(top, "negones_bf", [128, 128], BF16)
        negU_bf = sb(top, "negU_bf", [128, 128], BF16)
        tri_bf = sb(top, "tri_bf", [128, 128], BF16)
        zeros_bf = sb(top, "zeros_bf", [128, 512], BF16)
        s_c = sig(top, "const")
        s_cv = sig(top, "constv")
        s_cv.inc(V.memset(ones_f[:], 1.0))
        s_cv.inc(V.memset(ones_bf[:], 1.0))
        s_cv.inc(V.memset(negones_bf[:], -1.0))
        s_cv.inc(V.memset(zeros_bf[:], 0.0))
        W(G, s_cv)
        s_c.inc(G.affine_select(out=ident_f[:], in_=ones_f[:], pattern=[[-1, 128]], compare_op=ALU.is_equal,
                                fill=0.0, base=0, channel_multiplier=1))
        s_c.inc(G.affine_select(out=ident_bf[:], in_=ones_bf[:], pattern=[[-1, 128]], compare_op=ALU.is_equal,
                                fill=0.0, base=0, channel_multiplier=1))
        s_c.inc(G.affine_select(out=negU_bf[:], in_=negones_bf[:], pattern=[[-1, 128]], compare_op=ALU.is_ge,
                                fill=0.0, base=0, channel_multiplier=1))
        s_c.inc(G.affine_select(out=tri_bf[:], in_=ones_bf[:], pattern=[[1, 128]], compare_op=ALU.is_gt,
                                fill=0.0, base=0, channel_multiplier=-1))
        for e in (V, A, T, SY):
            W(e, s_c)
        W(G, s_c)

        modrow = sb(top, "modrow", [128, 4 * D])
        modcol = sb(top, "modcol", [128, 16])
        Am = sb(top, "Am", [128, 8])
        fing_row = sb(top, "fing_row", [128, D])
        gm_row = modrow[:, 0:D]
        shf_row = modrow[:, D:2 * D]
        Af_row = modrow[:, 2 * D:3 * D]
        gf_row = modrow[:, 3 * D:4 * D]
        Bm = modcol[:, 0:8]

        ssall = sb(top, "ssall", [128, 32])
        rstdall = sb(top, "rstdall", [128, 32])
        with contextlib.ExitStack() as es:
            xs = [sb(es, f"xs{i}", [128, D]) for i in range(4)]
            junk = sb(es, "junk0", [128, D], BF16)
            s_x = sig(es, "0x", 4)
            s_a = sig(es, "0a")
            s_v0 = sig(es, "0v")
            vxs = {}
            vas = {}
            for tt in range(NT):
                if tt >= 4:
                    W(SY, s_a, vas[tt - 4])
                vxs[tt] = s_x.dma(SY.dma_start(out=xs[tt % 4][:], in_=x_d[tt * 128:(tt + 1) * 128, :]), lane=tt % 4)
                W(A, s_x, vxs[tt])
                vas[tt] = s_a.inc(A.activation(out=junk[:], in_=xs[tt % 4][:], func=AF.Square, accum_out=ssall[:, tt:tt + 1]))
            W(A, s_a)
            va = s_a.inc(A.activation(out=ssall[:], in_=ssall[:], func=AF.Sqrt, scale=1.0 / D, bias=EPS))
            W(V, s_a, va)
            s_v0.inc(V.reciprocal(out=rstdall[:], in_=ssall[:]))
            barrier_early = [s_x, s_a, s_v0]
            for e in (V, A, T, SY, G):
                for s_ in barrier_early:
                    W(e, s_)

        with contextlib.ExitStack() as es:
            ccol = sb(es, "ccol", [128, 8])
            scol = sb(es, "scol", [128, 8])
            scb = sb(es, "scb", [128, 8, 128], BF16)
            scol_b = sb(es, "scol_b", [128, 8], BF16)
            adab_col = sb(es, "adab_col", [128, 48])
            mixg = sb(es, "mixg", [128, 8])
            ffng_row = sb(es, "ffng_row", [128, D])
            adab_row = sb(es, "adab_row", [128, 512])
            aw = [sb(es, f"aw{i}", [128, 8, 512], BF16) for i in range(2)]
            pr = [ps(es, f"p0r{i}", [128, 512]) for i in range(2)]
            pc = ps(es, "p0c", [128, 16])
            s_ld = sig(es, "p0ld")
            s_aw = sig(es, "p0aw", 2)
            s_awfree = sig(es, "p0awf")
            s_a = sig(es, "p0a")
            s_v = sig(es, "p0v")
            s_t = sig(es, "p0t")
            s_b = sig(es, "p0b")
            s_ld.dma(SY.dma_start(out=ccol[:], in_=ccol_d))
            s_ld.dma(SY.dma_start(out=adab_col[:], in_=adab_col_d))
            s_ld.dma(SY.dma_start(out=mixg[:], in_=mixg_d))
            s_ld.dma(SY.dma_start(out=ffng_row[:], in_=ffng_d.broadcast_to([128, D])))
            s_ld.dma(SY.dma_start(out=fing_row[:], in_=fing_d.broadcast_to([128, D])))
            W(A, s_ld)
            s_a.inc(A.activation(out=scol[:], in_=ccol[:], func=AF.Silu))
            W(V, s_a)
            W(V, s_ld)
            for k in range(8):
                s_v.inc(V.tensor_scalar(out=scb[:, k, :], in0=ones_f[:], scalar1=scol[:, k:k + 1], scalar2=None,
                                        op0=ALU.mult))
            s_v.inc(V.tensor_copy(out=scol_b[:], in_=scol[:]))
            aw_view = adaw_d.rearrange("(k p) n -> p k n", p=128)
            W(T, s_v)
            tdone = {}
            vev = {}
            for n in range(12):
                b = n % 2
                if n >= 2:
                    W(G, s_t, tdone[n - 2])
                v_aw = s_aw.dma(G.dma_start(out=aw[b][:], in_=aw_view[:, :, n * 512:(n + 1) * 512]), lane=b)
                W(T, s_aw, v_aw)
                if n < 4:
                    for fcl in range(4):
                        fc = n * 4 + fcl
                        for k in range(8):
                            ins = T.matmul(pc[:, fc:fc + 1], lhsT=aw[b][:, k, fcl * 128:(fcl + 1) * 128],
                                           rhs=scol_b[:, k:k + 1], start=(k == 0), stop=(k == 7))
                    tdone[n] = s_t.inc(ins)
                else:
                    if n - 2 >= 4:
                        W(T, s_v, vev[n - 2])
                    for k in range(8):
                        ins = T.matmul(pr[b][:], lhsT=scb[:, k, :], rhs=aw[b][:, k, :], start=(k == 0), stop=(k == 7))
                    tdone[n] = s_t.inc(ins)
                    if n - 1 >= 4:
                        W(SY, s_v, vev[n - 1])
                    v_b = s_b.dma(SY.dma_start(out=adab_row[:], in_=adab_row_d[:, n * 512:(n + 1) * 512]
                                               .broadcast_to([128, 512])))
                    W(V, s_b, v_b)
                    W(V, s_t, tdone[n])
                    vev[n] = s_v.inc(V.tensor_tensor(out=modrow[:, (n - 4) * 512:(n - 3) * 512], in0=pr[b][:],
                                                     in1=adab_row[:], op=ALU.add))
            W(V, s_t)
            s_v.inc(V.tensor_tensor(out=modcol[:], in0=pc[:], in1=adab_col[:, 0:16], op=ALU.add))
            W(V, s_v)
            s_v.inc(V.scalar_tensor_tensor(out=Am[:], in0=modcol[:, 8:16], scalar=1.0, in1=mixg[:], op0=ALU.add,
                                           op1=ALU.mult))
            s_v.inc(V.scalar_tensor_tensor(out=modrow[:, 2 * D:3 * D], in0=modrow[:, 2 * D:3 * D], scalar=1.0,
                                           in1=ffng_row[:], op0=ALU.add, op1=ALU.mult))
            for e in (A, T, SY, G):
                W(e, s_v)
            W(V, s_v)
            if dbg:
                d1 = dbg_tensor("modrow", [128, 4 * D])
                d2 = dbg_tensor("modcol", [128, 16])
                s_out.dma(SY.dma_start(out=d1, in_=modrow[:]))
                s_out.dma(SY.dma_start(out=d2, in_=modcol[:]))

        if stop <= 0:
            W(SY, s_out)
            return nc, dbg_out

        RA = sb(top, "RA", [128, 49152], BF16)
        RB = sb(top, "RB", [128, 16384], BF16)
        qT = RA[:, 0:16384].rearrange("p (c t) -> p c t", c=4)
        kT = RA[:, 16384:32768].rearrange("p (c t) -> p c t", c=4)
        vtok = RA[:, 32768:49152].rearrange("p (n f) -> p n f", n=32)
        yattnT = RB[:, :].rearrange("p (c t) -> p c t", c=4)
        cnt = {"tile": 0}

        def emit_norm_transpose(es, tiles, hT_of, xn_bufs, pT, xt, sg, hist):
            s_x, s_act, s_v, s_pe = sg
            st = hist
            base = len(st)

            def stage1(jl, tt):
                j = base + jl
                b = j % 2
                if j - 2 in st:
                    W(SY, s_v, st[j - 2]["xn"])
                vx = s_x.dma(SY.dma_start(out=xt[b][:], in_=x_d[tt * 128:(tt + 1) * 128, :]), lane=b)
                W(V, s_x, vx)
                if j - 2 in st:
                    W(V, s_pe, st[j - 2]["tr"])
                v3 = s_v.inc(V.tensor_scalar(out=xn_bufs[b], in0=xt[b][:], scalar1=rstdall[:, tt:tt + 1], scalar2=None,
                                             op0=ALU.mult))
                W(T, s_v, v3)
                if j - 2 in st:
                    W(T, s_v, st[j - 2]["ev"])
                for k in range(8):
                    ins = T.transpose(pT[b][:, k, :], xn_bufs[b][:, k * 128:(k + 1) * 128], ident_bf[:])
                st[j] = {"xn": v3, "tr": s_pe.inc(ins)}

            def stage2(jl):
                j = base + jl
                b = j % 2
                W(V, s_pe, st[j]["tr"])
                dst = hT_of(jl)
                for k in range(8):
                    ins = V.tensor_scalar(out=dst[:, k, :], in0=pT[b][:, k, :], scalar1=Am[:, k:k + 1],
                                          scalar2=Bm[:, k:k + 1], op0=ALU.mult, op1=ALU.add)
                st[j]["ev"] = s_v.inc(ins)

            for jl, tt in enumerate(tiles):
                stage1(jl, tt)
                if jl >= 1:
                    stage2(jl - 1)
            stage2(len(tiles) - 1)
            return st[base + len(tiles) - 1]["ev"]

        with contextlib.ExitStack() as es:
            win = RB[:, 0:12288].rearrange("p (k n) -> p k n", k=8)
            xn_bufs = [RB[:, 12288 + i * 1024:12288 + (i + 1) * 1024] for i in range(2)]
            hT = [sb(es, f"hT{i}", [128, 8, 1024], BF16) for i in range(2)]
            xt = [sb(es, f"xt{i}", [128, D]) for i in range(2)]
            pT = [ps(es, f"pT{i}", [128, 8, 128], BF16) for i in range(2)]
            pj = [ps(es, f"pj{i}", [128, 512]) for i in range(4)]
            s_w = sig(es, "p1w")
            sg = (sig(es, "p1x", 2), sig(es, "p1a"), sig(es, "p1v"), sig(es, "p1t"))
            s_x, s_act, s_v, s_pe = sg
            s_ea = sig(es, "p1ea")
            s_ev = sig(es, "p1ev")
            wv = win_d.rearrange("(k p) n -> p k n", p=128)
            for kh in range(2):
                s_w.dma(G.dma_start(out=win[:, kh * 4:(kh + 1) * 4, :], in_=wv[:, kh * 4:(kh + 1) * 4, 0:1536]))
            W(T, s_w)
            gi = 0
            ev_hist = {}
            hist1 = {}
            hT_last_pe = {}
            vh_q = {}

            def emit_nt(q):
                if q >= 2:
                    W(V, s_pe, hT_last_pe[q - 2])
                hq_ = hT[q % 2]
                vh_q[q] = emit_norm_transpose(es, list(range(q * 8, q * 8 + 8)),
                                              lambda j, hq_=hq_: hq_[:, :, j * 128:(j + 1) * 128], xn_bufs, pT, xt, sg, hist1)

            emit_nt(0)
            for q in range(4):
                hq = hT[q % 2]
                if q + 1 < 4:
                    emit_nt(q + 1)
                W(T, s_v, vh_q[q])
                groups = [("qk", ec, th) for ec in range(8) for th in range(2)] + [("v", tt, 0) for tt in range(8)]
                for (kind, a_, b_) in groups:
                    pb = pj[gi % 4]
                    if gi >= 4:
                        sg_, val_ = ev_hist[gi - 4]
                        W(T, sg_, val_)
                    for k in range(8):
                        if kind == "qk":
                            ins = T.matmul(pb[:], lhsT=win[:, k, a_ * 128:(a_ + 1) * 128],
                                           rhs=hq[:, k, b_ * 512:(b_ + 1) * 512], start=(k == 0), stop=(k == 7))
                        else:
                            ins = T.matmul(pb[:], lhsT=hq[:, k, a_ * 128:(a_ + 1) * 128], rhs=win[:, k, 1024:1536],
                                           start=(k == 0), stop=(k == 7))
                    vt = s_pe.inc(ins)
                    hT_last_pe[q] = vt
                    if kind == "qk":
                        tok0 = q * 1024 + b_ * 512
                        if a_ < 4:
                            dst = qT[:, a_, tok0:tok0 + 512]
                            scale = 0.125
                        else:
                            dst = kT[:, a_ - 4, tok0:tok0 + 512]
                            scale = 1.0
                    else:
                        dst = vtok[:, q * 8 + a_, :]
                        scale = 1.0
                    if gi % 2 == 0:
                        W(A, s_pe, vt)
                        ev_hist[gi] = (s_ea, s_ea.inc(A.activation(out=dst, in_=pb[:], func=AF.Copy, scale=scale)))
                    else:
                        W(V, s_pe, vt)
                        ev_hist[gi] = (s_ev, s_ev.inc(V.tensor_scalar(out=dst, in0=pb[:], scalar1=scale, scalar2=None,
                                                                      op0=ALU.mult)))
                    gi += 1
            for e in (A, V, T, G, SY):
                W(e, s_ea)
                W(e, s_ev)
                W(e, s_pe)
            if dbg and stop == 1:
                dq = dbg_tensor("qkv", [128, 49152], BF16)
                s_out.dma(SY.dma_start(out=dq, in_=RA[:, :]))
                d2 = dbg_tensor("rstd", [128, 32])
                s_out.dma(SY.dma_start(out=d2, in_=rstdall[:, :]))
                d3 = dbg_tensor("hT1", [128, 8192], BF16)
                s_out.dma(SY.dma_start(out=d3, in_=hT[1][:, :, :]))
                d4 = dbg_tensor("Am", [128, 8])
                s_out.dma(SY.dma_start(out=d4, in_=Am[:, :]))
        if stop <= 1:
            W(SY, s_out)
            return nc, dbg_out

        with contextlib.ExitStack() as es:
            e_t = [sb(es, f"e_t{i}", [128, 512]) for i in range(3)]
            S_t = [sb(es, f"S_t{i}", [128, 512], BF16) for i in range(3)]
            w_t = [sb(es, f"w_t{i}", [128, 512], BF16) for i in range(2)]
            R_t = [sb(es, f"R_t{i}", [128, 512], BF16) for i in range(2)]
            mask = [sb(es, f"mask{i}", [128, 512], BF16) for i in range(4)]
            pz = [ps(es, f"pz{i}", [128, 512]) for i in range(2)]
            pw = [ps(es, f"pw{i}", [128, 512]) for i in range(3)]
            po = [ps(es, f"po{i}", [128, 512]) for i in range(2)]
            s_pe = sig(es, "a_pe")
            s_act = sig(es, "a_act")
            s_pool = sig(es, "a_pool")
            s_v = sig(es, "a_v")
            for rel in range(4):
                s_pool.inc(G.affine_select(out=mask[rel][:], in_=zeros_bf[:], pattern=[[1, 512]], compare_op=ALU.is_gt,
                                           fill=-30000.0, base=-128 * rel, channel_multiplier=-1))
            W(T, s_pool)
            pairs = []
            groups = []
            for h in range(8):
                for c in range(8):
                    g = len(groups)
                    kbs = list(range(4 * c + 3, -1, -1))
                    groups.append((h, c, len(pairs), len(pairs) + len(kbs) - 1))
                    for kb in kbs:
                        pairs.append((h, c, kb, g))
            n = len(pairs)
            vA, vB, vC, vD, vE, vF, vG = {}, {}, {}, {}, {}, {}, {}
            gev = {}

            def ops(j):
                h, c, kb, g = pairs[j]
                hp = (h % 2) * 64
                kh = kT[hp:hp + 64, h // 2, kb * 128:(kb + 1) * 128]
                qh = qT[hp:hp + 64, h // 2, c * 512:(c + 1) * 512]
                rel = kb - 4 * c
                first = (j == groups[g][2])
                last = (j == groups[g][3])
                return h, c, kb, g, hp, kh, qh, rel, first, last

            def colsl(j):
                rel = pairs[j][2] - 4 * pairs[j][1]
                return slice(128 * rel, 512) if rel >= 1 else slice(0, 512)

            def zmm(out, j, stop_):
                h, c, kb, g, hp, kh, qh, rel, first, last = ops(j)
                cs = colsl(j)
                if rel >= 0:
                    T.matmul(out[:, cs], lhsT=kh, rhs=qh[:, cs], start=True, stop=False)
                    return T.matmul(out[:, cs], lhsT=ident_bf[:], rhs=mask[rel][:, cs], start=False, stop=stop_)
                return T.matmul(out[:, cs], lhsT=kh, rhs=qh[:, cs], start=True, stop=stop_)

            def st_A(j):
                if j - 2 >= 0:
                    W(T, s_act, vB[j - 2])
                vA[j] = s_pe.inc(zmm(pz[j % 2], j, True))

            def st_B(j):
                W(A, s_pe, vA[j])
                cs = colsl(j)
                vB[j] = s_act.inc(A.activation(out=e_t[j % 3][:, cs], in_=pz[j % 2][:, cs], func=AF.Exp))

            def st_C(j):
                W(A, s_act, vB[j])
                if j - 3 >= 0:
                    W(A, s_pe, vD[j - 3])
                    if (j - 3) in vE:
                        W(A, s_pool, vE[j - 3])
                cs = colsl(j)
                vC[j] = s_act.inc(A.activation(out=S_t[j % 3][:, cs], in_=e_t[j % 3][:, cs], func=AF.Ln, bias=1.0, scale=1.0))

            def st_E(j):
                h, c, kb, g, hp, kh, qh, rel, first, last = ops(j)
                if last:
                    return
                cs = colsl(j)
                W(G, s_act, vC[j])
                if j - 1 >= 0:
                    W(G, s_pe, vD[j - 1])
                Rn = R_t[(j + 1) % 2]
                if cs.start > 0:
                    s_pool.inc(G.memset(Rn[:, 0:cs.start], 0.0))
                if first:
                    vE[j] = s_pool.inc(G.tensor_copy(out=Rn[:, cs], in_=S_t[j % 3][:, cs]))
                else:
                    W(G, s_pool, vE[j - 1])
                    vE[j] = s_pool.inc(G.tensor_tensor(out=Rn[:, cs], in0=R_t[j % 2][:, cs], in1=S_t[j % 3][:, cs],
                                                       op=ALU.add))

            def st_D(j):
                h, c, kb, g, hp, kh, qh, rel, first, last = ops(j)
                W(T, s_act, vC[j])
                if j - 3 >= 0:
                    W(T, s_act, vF[j - 3])
                if not first:
                    W(T, s_pool, vE[j - 1])
                cs = colsl(j)
                zmm(pw[j % 3], j, False)
                ins = T.matmul(pw[j % 3][:, cs], lhsT=negU_bf[:], rhs=S_t[j % 3][:, cs], start=False, stop=first)
                if not first:
                    ins = T.matmul(pw[j % 3][:, cs], lhsT=negones_bf[:], rhs=R_t[j % 2][:, cs], start=False, stop=True)
                vD[j] = s_pe.inc(ins)

            def st_F(j):
                W(A, s_pe, vD[j])
                if j - 2 >= 0:
                    W(A, s_pe, vG[j - 2])
                cs = colsl(j)
                vF[j] = s_act.inc(A.activation(out=w_t[j % 2][:, cs], in_=pw[j % 3][:, cs], func=AF.Exp))

            def st_G(j):
                h, c, kb, g, hp, kh, qh, rel, first, last = ops(j)
                W(T, s_act, vF[j])
                if first and g - 2 >= 0:
                    W(T, s_v, gev[g - 2])
                cs = colsl(j)
                if first:
                    T.matmul(po[g % 2][hp:hp + 64, :], lhsT=zeros_bf[:, 0:64], rhs=mask[0][:], start=True, stop=False)
                ins = T.matmul(po[g % 2][hp:hp + 64, cs], lhsT=vtok[:, kb, h * 64:(h + 1) * 64], rhs=w_t[j % 2][:, cs],
                               start=False, stop=last)
                vG[j] = s_pe.inc(ins)
                if last:
                    W(V, s_pe, vG[j])
                    gev[g] = s_v.inc(V.tensor_copy(out=yattnT[hp:hp + 64, h // 2, c * 512:(c + 1) * 512],
                                                   in_=po[g % 2][hp:hp + 64, :]))

            for i in range(-3, n + 2):
                if 0 <= i + 3 < n:
                    st_A(i + 3)
                if 0 <= i + 2 < n:
                    st_B(i + 2)
                if 0 <= i + 1 < n:
                    st_C(i + 1)
                if 0 <= i < n:
                    st_D(i)
                if 0 <= i + 1 < n:
                    st_E(i + 1)
                if 0 <= i - 1 < n:
                    st_F(i - 1)
                if 0 <= i - 2 < n:
                    st_G(i - 2)
            for e in (A, V, T, G, SY):
                W(e, s_v)
                W(e, s_pe)
                W(e, s_act)
                W(e, s_pool)
            if dbg and stop == 2:
                dq = dbg_tensor("yattn", [128, 16384], BF16)
                s_out.dma(SY.dma_start(out=dq, in_=RB[:, :]))
        if stop <= 2:
            W(SY, s_out)
            return nc, dbg_out

        ENG = (V, A, G, T, SY)

        def barrier(sigs, engines=ENG):
            for e in engines:
                for s_ in sigs:
                    W(e, s_)

        ylruT = RA[:, 0:16384].rearrange("p (c t) -> p c t", c=4)
        with contextlib.ExitStack() as es:
            win = RA[:, 16384:24576].rearrange("p (k n) -> p k n", k=8)
            hT = [RA[:, 24576 + i * 8192:24576 + (i + 1) * 8192].rearrange("p (k t) -> p k t", k=8) for i in range(2)]
            xn_bufs = [RA[:, 40960 + i * 1024:40960 + (i + 1) * 1024] for i in range(2)]
            xt = [sb(es, f"xtb{i}", [128, D]) for i in range(2)]
            HW_ = 512
            xpad = [sb(es, f"xpad{i}", [128, HW_ + 3]) for i in range(2)]
            xc = [sb(es, f"xc{i}", [128, HW_]) for i in range(2)]
            r_t = [sb(es, f"r_t{i}", [128, HW_]) for i in range(2)]
            ig_t = [sb(es, f"ig_t{i}", [128, HW_]) for i in range(2)]
            b_t = [sb(es, f"b_t{i}", [128, HW_]) for i in range(2)]
            h_t = [sb(es, f"h_t{i}", [128, HW_]) for i in range(2)]
            gg = [sb(es, f"gg{i}", [128, HW_]) for i in range(2)]
            g2 = [sb(es, f"g2{i}", [128, HW_]) for i in range(2)]
            xcb = [sb(es, f"xcb{i}", [128, HW_], BF16) for i in range(2)]
            rgbd_b = sb(es, "rgbd_b", [128, 4, 128], BF16)
            igbd_b = sb(es, "igbd_b", [128, 4, 128], BF16)
            rgbd = sb(es, "rgbd", [128, 4, 128])
            igbd = sb(es, "igbd", [128, 4, 128])
            convw = sb(es, "convw", [128, 16])
            lruv = sb(es, "lruv", [128, 16])
            cA = sb(es, "cA", [128, 4])
            tmp4 = sb(es, "tmp4", [128, 4])
            hlast = sb(es, "hlast", [128, 4])
            xtail = sb(es, "xtail", [128, 4, 3])
            pT = [ps(es, f"pTb{i}", [128, 8, 128], BF16) for i in range(2)]
            pxr = [ps(es, f"pxr{i}", [128, HW_]) for i in range(2)]
            pgr = [ps(es, f"pgr{i}", [128, HW_]) for i in range(2)]
            pgt = [ps(es, f"pgt{i}", [128, HW_]) for i in range(2)]
            s_w = sig(es, "lw")
            sg = (sig(es, "lx", 2), sig(es, "la"), sig(es, "lv"), sig(es, "lt"))
            s_x, s_act, s_v, s_pe = sg
            s_g = sig(es, "lg")
            allsig = [s_w, s_x, s_act, s_v, s_pe, s_g]

            def v(ins):
                val = s_v.inc(ins); W(V, s_v, val); return val

            def a(ins):
                val = s_act.inc(ins); W(A, s_act, val); return val

            def g(ins):
                val = s_g.inc(ins); W(G, s_g, val); return val

            wv = win_d.rearrange("(k p) n -> p k n", p=128)
            for kh in range(2):
                s_w.dma(G.dma_start(out=win[:, kh * 4:(kh + 1) * 4, :], in_=wv[:, kh * 4:(kh + 1) * 4, 1536:2560]))
            v(V.memset(rgbd[:], 0.0))
            v(V.memset(igbd[:], 0.0))
            v(V.memset(hlast[:], 0.0))
            v(V.memset(xtail[:], 0.0))
            W(SY, s_v)
            for cc in range(4):
                for hh in range(2):
                    s_w.dma(SY.dma_start(out=rgbd[hh * 64:(hh + 1) * 64, cc, hh * 64:(hh + 1) * 64], in_=rgw_d[2 * cc + hh]))
                    s_w.dma(SY.dma_start(out=igbd[hh * 64:(hh + 1) * 64, cc, hh * 64:(hh + 1) * 64], in_=igw_d[2 * cc + hh]))
            s_w.dma(SY.dma_start(out=convw[:], in_=convw_d))
            s_w.dma(SY.dma_start(out=lruv[:], in_=lruv_d))
            barrier([s_w])
            v(V.tensor_copy(out=rgbd_b[:], in_=rgbd[:]))
            v(V.tensor_copy(out=igbd_b[:], in_=igbd[:]))
            lam = lruv[:, :].rearrange("p (c f) -> p c f", f=4)[:, :, 3]
            a(A.activation(out=tmp4[:], in_=lam, func=AF.Exp, scale=-1.0))
            a(A.activation(out=tmp4[:], in_=tmp4[:], func=AF.Ln, bias=1.0, scale=1.0))
            W(V, s_act)
            v(V.tensor_scalar(out=cA[:], in0=tmp4[:], scalar1=-8.0, scalar2=None, op0=ALU.mult))
            barrier(allsig)
            hist2 = {}
            un = {}

            def S1(u, q, cc, hf, hq):
                p = u % 2
                U = un[u] = {}
                cw = lambda j: convw[:, cc * 4 + j:cc * 4 + j + 1]
                lv = lambda j: lruv[:, cc * 4 + j:cc * 4 + j + 1]
                tsl = slice(hf * HW_, (hf + 1) * HW_)
                if u - 2 in un:
                    P_ = un[u - 2]
                    for e in (T, V, A, G):
                        W(e, s_v, P_["v_end"])
                        W(e, s_act, P_["a_end"])
                        W(e, s_pe, P_["t_end"])
                        W(e, s_g, P_["g_end"])
                for k in range(8):
                    T.matmul(pxr[p][:], lhsT=win[:, k, cc * 128:(cc + 1) * 128], rhs=hq[:, k, tsl], start=(k == 0), stop=(k == 7))
                for k in range(8):
                    ins = T.matmul(pgr[p][:], lhsT=win[:, k, 512 + cc * 128:512 + (cc + 1) * 128], rhs=hq[:, k, tsl],
                                   start=(k == 0), stop=(k == 7))
                vt = s_pe.inc(ins)
                W(V, s_pe, vt)
                W(A, s_pe, vt)
                v(V.tensor_copy(out=xpad[p][:, 0:3], in_=xtail[:, cc, :]))
                v(V.tensor_copy(out=xpad[p][:, 3:HW_ + 3], in_=pxr[p][:]))
                v(V.tensor_copy(out=xtail[:, cc, :], in_=xpad[p][:, HW_:HW_ + 3]))
                v(V.tensor_scalar(out=xc[p][:], in0=xpad[p][:, 0:HW_], scalar1=cw(0), scalar2=lv(0), op0=ALU.mult, op1=ALU.add))
                for j in range(1, 4):
                    vxc = v(V.scalar_tensor_tensor(out=xc[p][:], in0=xpad[p][:, j:j + HW_], scalar=cw(j), in1=xc[p][:],
                                                   op0=ALU.mult, op1=ALU.add))
                va = a(A.activation(out=gg[p][:], in_=pgr[p][:], func=AF.Identity))
                W(G, s_v, vxc)
                vxb = g(G.tensor_copy(out=xcb[p][:], in_=xc[p][:]))
                W(T, s_g, vxb)
                W(G, s_act, va)
                g(G.tensor_tensor(out=g2[p][:], in0=gg[p][:], in1=gg[p][:], op=ALU.mult))
                g(G.tensor_scalar(out=g2[p][:], in0=g2[p][:], scalar1=0.044715, scalar2=1.0, op0=ALU.mult, op1=ALU.add))
                vg = g(G.tensor_tensor(out=g2[p][:], in0=g2[p][:], in1=gg[p][:], op=ALU.mult))
                ins = T.matmul(pgt[p][:], lhsT=rgbd_b[:, cc, :], rhs=xcb[p][:], start=True, stop=True)
                vt = s_pe.inc(ins)
                W(A, s_pe, vt)
                va = a(A.activation(out=r_t[p][:], in_=pgt[p][:], func=AF.Sigmoid, bias=lv(1), scale=1.0))
                W(T, s_act, va)
                ins = T.matmul(pgt[p][:], lhsT=igbd_b[:, cc, :], rhs=xcb[p][:], start=True, stop=True)
                vt = s_pe.inc(ins)
                W(A, s_pe, vt)
                a(A.activation(out=ig_t[p][:], in_=pgt[p][:], func=AF.Sigmoid, bias=lv(2), scale=1.0))
                W(A, s_g, vg)
                vgs = a(A.activation(out=g2[p][:], in_=g2[p][:], func=AF.Sigmoid, scale=1.5957691216))
                W(G, s_act, vgs)
                vg = g(G.tensor_tensor(out=g2[p][:], in0=g2[p][:], in1=gg[p][:], op=ALU.mult))
                va = a(A.activation(out=r_t[p][:], in_=r_t[p][:], func=AF.Exp, scale=cA[:, cc:cc + 1]))
                U["a"] = va
                U["t_end"] = vt
                U["g_end"] = vg
                U["cc"] = cc
                U["q"] = q
                U["hf"] = hf

            def S2a(u):
                p = u % 2
                U = un[u]
                cc, q, hf = U["cc"], U["q"], U["hf"]
                W(V, s_act, U["a"])
                v(V.scalar_tensor_tensor(out=b_t[p][:], in0=r_t[p][:], scalar=-1.0, in1=r_t[p][:], op0=ALU.mult,
                                         op1=ALU.mult))
                vb = v(V.tensor_scalar(out=b_t[p][:], in0=b_t[p][:], scalar1=1.0, scalar2=1e-30, op0=ALU.add,
                                       op1=ALU.max))
                W(A, s_v, vb)
                va = a(A.activation(out=b_t[p][:], in_=b_t[p][:], func=AF.Sqrt))
                U["a_end"] = va

            def S2b(u):
                p = u % 2
                U = un[u]
                cc, q, hf = U["cc"], U["q"], U["hf"]
                W(V, s_act, U["a_end"])
                v(V.tensor_tensor(out=b_t[p][:], in0=b_t[p][:], in1=ig_t[p][:], op=ALU.mult))
                v(V.tensor_tensor(out=b_t[p][:], in0=b_t[p][:], in1=xc[p][:], op=ALU.mult))
                v(V.tensor_tensor_scan(out=h_t[p][:], data0=r_t[p][:], data1=b_t[p][:], initial=hlast[:, cc:cc + 1],
                                       op0=ALU.mult, op1=ALU.add))
                v(V.tensor_copy(out=hlast[:, cc:cc + 1], in_=h_t[p][:, HW_ - 1:HW_]))
                W(V, s_g, U["g_end"])
                t0 = q * 1024 + hf * HW_
                U["v_end"] = v(V.tensor_tensor(out=ylruT[:, cc, t0:t0 + HW_], in0=g2[p][:], in1=h_t[p][:], op=ALU.mult))

            u = 0
            vhq = {}
            qlast = {}

            def emit_nt(q):
                if q >= 2:
                    W(V, s_pe, qlast[q - 2])
                hq_ = hT[q % 2]
                vhq[q] = emit_norm_transpose(es, list(range(q * 8, q * 8 + 8)),
                                             lambda j, hq_=hq_: hq_[:, :, j * 128:(j + 1) * 128], xn_bufs, pT, xt, sg, hist2)

            emit_nt(0)
            for q in range(4):
                hq = hT[q % 2]
                if q + 1 < 4:
                    emit_nt(q + 1)
                W(T, s_v, vhq[q])
                first = u
                for cc in range(4):
                    for hf in range(2):
                        if u - 1 >= first:
                            S2a(u - 1)
                        S1(u, q, cc, hf, hq)
                        if u - 1 >= first:
                            S2b(u - 1)
                        u += 1
                S2a(u - 1)
                S2b(u - 1)
                qlast[q] = un[u - 1]["t_end"]
            barrier(allsig)
            if dbg and stop == 3:
                dq = dbg_tensor("ylru", [128, 16384], BF16)
                s_out.dma(SY.dma_start(out=dq, in_=RA[:, 0:16384]))
                d2 = dbg_tensor("rstd3", [128, 32])
                s_out.dma(SY.dma_start(out=d2, in_=rstdall[:, :]))
                d3 = dbg_tensor("hT23", [128, 16384], BF16)
                s_out.dma(SY.dma_start(out=d3, in_=RA[:, 24576:40960]))
        if stop <= 3:
            W(SY, s_out)
            return nc, dbg_out

        M8all = sb(top, "M8all", [128, 32, 8])
        Lall = sb(top, "Lall", [128, 32, 32])
        rank_all = sb(top, "rank_all", [128, 32, 32])
        cnt_run = sb(top, "cnt_run", [128, 32])
        dest4f = sb(top, "dest4f", [128, 32, 4])
        dest4i = sb(top, "dest4i", [128, 128], I32)
        p4 = sb(top, "p4", [128, 32, 4])
        widx = sb(top, "widx", [128, NBP], I32)
        bidx = sb(top, "bidx", [128, NBP], I32)

        with contextlib.ExitStack() as es:
            wout = RA[:, 16384:24576].rearrange("p (k n) -> p k n", k=8)
            xt = [sb(es, f"x3_{i}", [128, D]) for i in range(2)]
            x1 = [sb(es, f"x1_{i}", [128, D]) for i in range(2)]
            t1 = sb(es, "t1", [128, D])
            h2 = sb(es, "h2", [128, D])
            h2b = [sb(es, f"h2b{i}", [128, D], BF16) for i in range(2)]
            h2T = sb(es, "h2T", [128, 8, 128])
            ysq = sb(es, "ysq", [128, 8, 128], BF16)
            rw = sb(es, "rw", [128, 8, 32])
            rb_row = sb(es, "rb_row", [128, 32])
            outg = sb(es, "outg", [128, 8])
            sd2 = sb(es, "sd2", [128, 2])
            rs2 = sb(es, "rs2", [128, 2])
            ss3 = sb(es, "ss3", [128, 1])
            rs3 = sb(es, "rs3", [128, 1])
            Mf = sb(es, "Mf", [128, 32])
            Mbf = sb(es, "Mbf", [128, 32], BF16)
            PA = [ps(es, f"PA{i}", [128, 512]) for i in range(2)]
            PL = [ps(es, f"PL{i}", [128, 512]) for i in range(2)]
            ptr = ps(es, "ptr", [128, 4, 128])
            psm = ps(es, "psm", [128, 512])
            psl = ps(es, "psl", [128, 512])
            psr = ps(es, "psr", [128, 512])
            s_d = sig(es, "3d", 2)
            s_v = sig(es, "3v")
            s_act = sig(es, "3a")
            s_pe = sig(es, "3t")
            s_st = sig(es, "3st", 4)
            allsig = [s_d, s_v, s_act, s_pe, s_st]

            def v(ins):
                val = s_v.inc(ins); W(V, s_v, val); return val

            def a(ins):
                val = s_act.inc(ins); W(A, s_act, val); return val

            s_d.dma(SY.dma_start(out=rw[:], in_=rw_d.rearrange("(k p) n -> p k n", p=128)))
            s_d.dma(SY.dma_start(out=rb_row[:], in_=rb_d.broadcast_to([128, 32])))
            s_d.dma(SY.dma_start(out=outg[:], in_=outg_d))
            v(V.memset(cnt_run[:], 0.0))
            wov = wout_d.rearrange("(k p) n -> p k n", p=128)
            for k in range(8):
                vd = s_d.dma(SY.dma_start(out=t1[:], in_=wov[:, k, :]))
                W(V, s_d, vd)
                vv = v(V.tensor_scalar(out=wout[:, k, :], in0=t1[:], scalar1=outg[:, k:k + 1], scalar2=None, op0=ALU.mult))
                W(SY, s_v, vv)
            barrier(allsig)
            s_g = sig(es, "3g")
            allsig.append(s_g)
            st3 = {tt: {} for tt in range(NT)}

            def vv(ins):
                return s_v.inc(ins)

            def stA(tt):
                b = tt % 2
                S_ = st3[tt]
                tsl = slice(tt * 128, (tt + 1) * 128)
                if tt - 2 >= 0:
                    W(SY, s_v, st3[tt - 2]["x1"])
                S_["x"] = s_d.dma(SY.dma_start(out=xt[b][:], in_=x_d[tsl, :]), lane=b)
                if tt - 1 >= 0:
                    W(G, s_pe, st3[tt - 1]["stats"])
                s_g.inc(G.tensor_tensor(out=ysq[:, 0:4, :], in0=yattnT[:, :, tsl], in1=yattnT[:, :, tsl], op=ALU.mult))
                vq = s_g.inc(G.tensor_tensor(out=ysq[:, 4:8, :], in0=ylruT[:, :, tsl], in1=ylruT[:, :, tsl], op=ALU.mult))
                W(T, s_g, vq)
                if tt - 1 >= 0:
                    W(T, s_act, st3[tt - 1]["sd2"])
                for gI in range(2):
                    for c in range(4):
                        ins = T.matmul(psm[:, gI:gI + 1], lhsT=ysq[:, gI * 4 + c, :], rhs=ones_bf[:, 0:1], start=(c == 0),
                                       stop=(c == 3))
                S_["stats"] = s_pe.inc(ins)
                if tt - 1 >= 0:
                    W(T, s_v, st3[tt - 1]["comb"])
                for dh in range(2):
                    for c in range(4):
                        T.matmul(PA[dh][:], lhsT=yattnT[:, c, tsl], rhs=wout[:, c, dh * 512:(dh + 1) * 512], start=(c == 0),
                                 stop=(c == 3))
                    for c in range(4):
                        ins = T.matmul(PL[dh][:], lhsT=ylruT[:, c, tsl], rhs=wout[:, 4 + c, dh * 512:(dh + 1) * 512],
                                       start=(c == 0), stop=(c == 3))
                S_["op"] = s_pe.inc(ins)
                W(A, s_pe, S_["stats"])
                if tt - 1 >= 0:
                    W(A, s_v, st3[tt - 1]["rs2"])
                S_["sd2"] = s_act.inc(A.activation(out=sd2[:], in_=psm[:, 0:2], func=AF.Sqrt, scale=1.0 / 512, bias=EPS))

            def stB1(tt):
                b = tt % 2
                S_ = st3[tt]
                tsl = slice(tt * 128, (tt + 1) * 128)
                W(V, s_act, S_["sd2"])
                S_["rs2"] = vv(V.reciprocal(out=rs2[:], in_=sd2[:]))
                W(V, s_v, S_["rs2"])
                W(V, s_pe, S_["op"])
                W(V, s_d, S_["x"])
                if tt - 1 >= 0:
                    W(V, s_act, st3[tt - 1]["sq"])
                for dh in range(2):
                    cs = slice(dh * 512, (dh + 1) * 512)
                    v_ = vv(V.tensor_scalar(out=t1[:, cs], in0=PA[dh][:], scalar1=rs2[:, 0:1], scalar2=None, op0=ALU.mult))
                    W(V, s_v, v_)
                    v_ = vv(V.scalar_tensor_tensor(out=t1[:, cs], in0=PL[dh][:], scalar=rs2[:, 1:2], in1=t1[:, cs], op0=ALU.mult,
                                                   op1=ALU.add))
                S_["comb"] = v_
                W(V, s_v, v_)
                v_ = vv(V.tensor_tensor(out=t1[:], in0=t1[:], in1=gm_row, op=ALU.mult))
                W(V, s_v, v_)
                if tt - 2 >= 0:
                    W(V, s_st, st3[tt - 2]["x1st"])
                    W(V, s_v, st3[tt - 2]["h2"])
                S_["x1"] = vv(V.tensor_tensor(out=x1[b][:], in0=t1[:], in1=xt[b][:], op=ALU.add))
                W(SY, s_v, S_["x1"])
                S_["x1st"] = s_st.dma(SY.dma_start(out=X1[tsl, :], in_=x1[b][:]), lane=b)
                W(A, s_v, S_["x1"])
                va = s_act.inc(A.activation(out=t1[:], in_=x1[b][:], func=AF.Square, accum_out=ss3[:]))
                S_["sq"] = va
                W(A, s_act, va)
                if tt - 1 >= 0:
                    W(A, s_v, st3[tt - 1]["rs3"])
                S_["ss3"] = s_act.inc(A.activation(out=ss3[:], in_=ss3[:], func=AF.Sqrt, scale=1.0 / D, bias=EPS))

            def stB2(tt):
                b = tt % 2
                S_ = st3[tt]
                tsl = slice(tt * 128, (tt + 1) * 128)
                W(V, s_act, S_["ss3"])
                S_["rs3"] = vv(V.reciprocal(out=rs3[:], in_=ss3[:]))
                W(V, s_v, S_["rs3"])
                if tt - 1 >= 0:
                    W(V, s_pe, st3[tt - 1]["tr"])
                    W(V, s_act, st3[tt - 1]["h2b"])
                v_ = vv(V.scalar_tensor_tensor(out=h2[:], in0=x1[b][:], scalar=rs3[:, 0:1], in1=Af_row, op0=ALU.mult,
                                               op1=ALU.mult))
                W(V, s_v, v_)
                S_["h2"] = vv(V.tensor_tensor(out=h2[:], in0=h2[:], in1=shf_row, op=ALU.add))
                W(A, s_v, S_["h2"])
                if tt - 2 >= 0:
                    W(A, s_st, st3[tt - 2]["h2st"])
                S_["h2b"] = s_act.inc(A.activation(out=h2b[b][:], in_=h2[:], func=AF.Copy))
                W(SY, s_act, S_["h2b"])
                S_["h2st"] = s_st.dma(SY.dma_start(out=H2[tsl, :], in_=h2b[b][:]), lane=2 + b)
                W(T, s_v, S_["h2"])
                if tt - 1 >= 0:
                    W(T, s_act, st3[tt - 1]["h2T"])
                for half in range(2):
                    if half == 1:
                        W(T, s_act, S_["h2T"])
                    for k in range(4):
                        kk = half * 4 + k
                        ins = T.transpose(ptr[:, k, :], h2[:, kk * 128:(kk + 1) * 128], ident_f[:])
                    S_["tr"] = s_pe.inc(ins)
                    W(A, s_pe, S_["tr"])
                    if tt - 1 >= 0 and half == 0:
                        W(A, s_pe, st3[tt - 1]["rt"])
                    S_["h2T"] = s_act.inc(A.activation(out=h2T[:, half * 4:(half + 1) * 4, :], in_=ptr[:], func=AF.Identity))
                W(T, s_act, S_["h2T"])
                if tt - 1 >= 0:
                    W(T, s_v, st3[tt - 1]["L"])
                for k in range(8):
                    ins = T.matmul(psl[:, 32:64], lhsT=h2T[:, k, :], rhs=rw[:, k, :], start=(k == 0), stop=(k == 7))
                S_["rt"] = s_pe.inc(ins)

            def stC(tt):
                S_ = st3[tt]
                W(V, s_pe, S_["rt"])
                S_["L"] = vv(V.tensor_tensor(out=Lall[:, tt, :], in0=psl[:, 32:64], in1=rb_row[:], op=ALU.add))
                W(V, s_v, S_["L"])
                v_ = vv(V.max(out=M8all[:, tt, :], in_=Lall[:, tt, :]))
                W(V, s_v, v_)
                if tt - 1 >= 0:
                    W(V, s_pe, st3[tt - 1]["rk"])
                v_ = vv(V.tensor_scalar(out=Mf[:], in0=Lall[:, tt, :], scalar1=M8all[:, tt, 3:4], scalar2=None, op0=ALU.is_ge))
                W(V, s_v, v_)
                vm = vv(V.tensor_copy(out=Mbf[:], in_=Mf[:]))
                W(T, s_v, vm)
                if tt - 1 >= 0:
                    W(T, s_v, st3[tt - 1]["cnt"])
                T.matmul(psr[:, 64:96], lhsT=tri_bf[:], rhs=Mbf[:], start=True, stop=True)
                ins = T.matmul(psr[:, 96:128], lhsT=ones_bf[:], rhs=Mbf[:], start=True, stop=True)
                S_["rk"] = s_pe.inc(ins)

            def stC2(tt):
                S_ = st3[tt]
                W(V, s_pe, S_["rk"])
                if tt - 1 >= 0:
                    W(V, s_v, st3[tt - 1]["cnt"])
                v_ = vv(V.tensor_tensor(out=rank_all[:, tt, :], in0=psr[:, 64:96], in1=cnt_run[:], op=ALU.add))
                W(V, s_v, v_)
                S_["cnt"] = vv(V.tensor_tensor(out=cnt_run[:], in0=psr[:, 96:128], in1=cnt_run[:], op=ALU.add))

            stA(0)
            for tt in range(NT):
                stB1(tt)
                if tt + 1 < NT:
                    stA(tt + 1)
                if tt - 1 >= 0:
                    stC(tt - 1)
                stB2(tt)
                if tt - 1 >= 0:
                    stC2(tt - 1)
            stC(NT - 1)
            stC2(NT - 1)
            barrier(allsig)

        with contextlib.ExitStack() as es:
            padded = sb(es, "padded", [128, 32])
            pad_end = sb(es, "pad_end", [128, 32])
            pad_start = sb(es, "pad_start", [128, 32])
            tmp32 = sb(es, "tmp32", [128, 32])
            destE = sb(es, "destE", [128, 32, 32])
            oh = sb(es, "oh", [128, 32, 32])
            E4 = sb(es, "E4", [128, 32, 4])
            den = sb(es, "den", [128, 32])
            tokid = sb(es, "tokid", [128, 32, 16], I32)
            zt = sb(es, "zt", [128, NSLOT * 16 // 128], I32)
            jv_i = sb(es, "jv_i", [128, NBP], I32)
            jv = sb(es, "jv", [128, NBP])
            pid_i = sb(es, "pid_i", [128, NBP], I32)
            pid = sb(es, "pid", [128, NBP])
            cmp = sb(es, "cmp", [128, NBP, 32])
            be = sb(es, "be", [128, NBP])
            s_v = sig(es, "4v")
            s_act = sig(es, "4a")
            s_g = sig(es, "4g")
            s_z = sig(es, "4z")

            def v(ins):
                val = s_v.inc(ins); W(V, s_v, val); return val

            def g(ins):
                val = s_g.inc(ins); W(G, s_g, val); return val

            g(G.iota(out=tokid[:], pattern=[[128, 32], [0, 16]], base=0, channel_multiplier=1))
            g(G.iota(out=jv_i[:], pattern=[[BLK, NBP]], base=0, channel_multiplier=0))
            g(G.iota(out=pid_i[:], pattern=[[0, NBP]], base=0, channel_multiplier=1))
            g(G.memset(zt[:], 0))
            s_z.dma(G.dma_start(out=SLOT.rearrange("(p f) c -> p (f c)", p=128), in_=zt[:]))
            W(V, s_g)
            v(V.tensor_copy(out=jv[:], in_=jv_i[:]))
            v(V.tensor_copy(out=pid[:], in_=pid_i[:]))
            v(V.tensor_tensor(out=destE[:], in0=cnt_run[:, :].unsqueeze(2).to_broadcast([128, 32, 32]),
                              in1=jv[:, 0:32].unsqueeze(1).to_broadcast([128, 32, 32]), op=ALU.is_gt))
            v(V.tensor_reduce(out=tmp32[:], in_=destE[:], axis=AX.X, op=ALU.add))
            v(V.tensor_scalar(out=padded[:], in0=tmp32[:], scalar1=float(BLK), scalar2=None, op0=ALU.mult))
            v(V.tensor_tensor_scan(out=pad_end[:], data0=ones_f[:, 0:32], data1=padded[:], initial=0.0, op0=ALU.mult,
                                   op1=ALU.add))
            v(V.tensor_tensor(out=pad_start[:], in0=pad_end[:], in1=padded[:], op=ALU.subtract))
            v(V.tensor_tensor(out=destE[:], in0=rank_all[:], in1=pad_start[:, :].unsqueeze(1).to_broadcast([128, 32, 32]),
                              op=ALU.add))
            for k in range(4):
                v(V.tensor_tensor(out=oh[:], in0=Lall[:], in1=M8all[:, :, k:k + 1].to_broadcast([128, 32, 32]),
                                  op=ALU.is_equal))
                v(V.tensor_tensor(out=oh[:], in0=oh[:], in1=destE[:], op=ALU.mult))
                v(V.tensor_reduce(out=dest4f[:, :, k], in_=oh[:], axis=AX.X, op=ALU.add))
            v(V.tensor_copy(out=dest4i[:], in_=dest4f[:, :, :].rearrange("p a b -> p (a b)")))
            vv = v(V.tensor_tensor(out=E4[:], in0=M8all[:, :, 0:4], in1=M8all[:, :, 0:1].to_broadcast([128, 32, 4]),
                                   op=ALU.subtract))
            W(A, s_v, vv)
            va = s_act.inc(A.activation(out=E4[:], in_=E4[:], func=AF.Exp))
            W(V, s_act, va)
            v(V.tensor_reduce(out=den[:], in_=E4[:], axis=AX.X, op=ALU.add))
            v(V.reciprocal(out=den[:], in_=den[:]))
            v(V.tensor_tensor(out=p4[:], in0=E4[:], in1=den[:, :].unsqueeze(2).to_broadcast([128, 32, 4]), op=ALU.mult))
            v(V.tensor_tensor(out=cmp[:], in0=pad_end[:, :].unsqueeze(1).to_broadcast([128, NBP, 32]),
                              in1=jv[:, :].unsqueeze(2).to_broadcast([128, NBP, 32]), op=ALU.is_le))
            v(V.tensor_reduce(out=be[:], in_=cmp[:], axis=AX.X, op=ALU.add))
            v(V.tensor_scalar(out=be[:], in0=be[:], scalar1=31.0, scalar2=None, op0=ALU.min))
            v(V.tensor_copy(out=bidx[:], in_=be[:]))
            v(V.scalar_tensor_tensor(out=be[:], in0=be[:], scalar=128.0, in1=pid[:], op0=ALU.mult, op1=ALU.add))
            v(V.tensor_scalar(out=jv[:], in0=jv[:], scalar1=pad_end[:, 31:32], scalar2=None, op0=ALU.is_lt))
            v(V.tensor_scalar(out=pid[:], in0=pid[:], scalar1=0.0, scalar2=None, op0=ALU.is_equal))
            v(V.tensor_tensor(out=jv[:], in0=jv[:], in1=pid[:], op=ALU.max))
            v(V.tensor_scalar(out=be[:], in0=be[:], scalar1=-100000.0, scalar2=None, op0=ALU.add))
            v(V.tensor_tensor(out=be[:], in0=be[:], in1=jv[:], op=ALU.mult))
            v(V.tensor_scalar(out=be[:], in0=be[:], scalar1=100000.0, scalar2=None, op0=ALU.add))
            v(V.tensor_copy(out=widx[:], in_=be[:]))
            W(G, s_v)
            W(G, s_z)
            for tt in range(NT):
                for k in range(4):
                    s_z.dma(G.indirect_dma_start(out=SLOT[:, :],
                                                 out_offset=bass.IndirectOffsetOnAxis(ap=dest4i[:, tt * 4 + k:tt * 4 + k + 1], axis=0),
                                                 in_=tokid[:, tt, :], in_offset=None))
            barrier([s_v, s_act, s_g, s_z])
            if dbg and stop == 4:
                for nm, t_, shp, dt_ in [("Lall", Lall, [128, 1024], F32), ("M8all", M8all, [128, 256], F32),
                                         ("dest4i", dest4i, [128, 128], I32), ("p4", p4, [128, 128], F32),
                                         ("widx", widx, [128, NBP], I32), ("bidx", bidx, [128, NBP], I32),
                                         ("cnt", cnt_run, [128, 32], F32)]:
                    dd = dbg_tensor(nm, shp, dt_)
                    ap_ = t_[:] if len(t_.shape) == 2 else t_[:, :, :].rearrange("p a b -> p (a b)")
                    s_out.dma(SY.dma_start(out=dd, in_=ap_))
                dd = dbg_tensor("slot", [NSLOT, 16], I32)
                s_out.dma(SY.dma_start(out=dd, in_=SLOT))
                dd = dbg_tensor("x1", [S, D])
                s_out.dma(SY.dma_start(out=dd, in_=X1))
                dd = dbg_tensor("h2", [S, D], BF16)
                s_out.dma(SY.dma_start(out=dd, in_=H2))
        if stop <= 4:
            W(SY, s_out)
            return nc, dbg_out

        with contextlib.ExitStack() as es:
            wbuf = [[RA[:, (par * 3 + m) * 8192:(par * 3 + m + 1) * 8192].rearrange("p (k n) -> p k n", k=8)
                     for m in range(3)] for par in range(2)]
            xb = [RB[:, par * 4096:par * 4096 + QB * 1024].rearrange("p (q d) -> p q d", q=QB) for par in range(2)]
            xbT = RB[:, 8192:8192 + 8 * BLK].rearrange("p (k s) -> p k s", k=8)
            actT0 = RB[:, 12288:12288 + 8 * BLK].rearrange("p (k s) -> p k s", k=8)
            actT1 = sb(es, "actT1", [128, 8, BLK], BF16)
            actT = [actT0, actT1]
            bgt = [sb(es, f"bgt{i}", [128, 8]) for i in range(2)]
            but = [sb(es, f"but{i}", [128, 8]) for i in range(2)]
            bdt = [sb(es, f"bdt{i}", [128, D]) for i in range(2)]
            tki = [sb(es, f"tki{i}", [128, QB, 16], I32) for i in range(2)]
            ysb = sb(es, "ysb", [128, QB, D])
            g_t = [sb(es, f"g_t{i}", [128, BLK]) for i in range(2)]
            sg_t = [sb(es, f"sg_t{i}", [128, BLK]) for i in range(2)]
            u_t = [sb(es, f"u_t{i}", [128, BLK]) for i in range(2)]
            ptx = [ps(es, f"ptx{i}", [128, QB, 128], BF16) for i in range(2)]
            pg = [ps(es, f"pg{i}", [128, BLK]) for i in range(2)]
            pu = [ps(es, f"pu{i}", [128, BLK]) for i in range(2)]
            py = [ps(es, f"py{i}", [128, 512]) for i in range(2)]
            s_tk = sig(es, "5tk", 2)
            s_xb = sig(es, "5xb", 2)
            s_wgu = sig(es, "5wgu", 2)
            s_wd = sig(es, "5wd", 2)
            s_pe = sig(es, "5t")
            s_v = sig(es, "5v")
            s_act = sig(es, "5a")
            s_yd = sig(es, "5y")
            blk = {j: {} for j in range(NBLK)}
            wsrc = (wg_d, wu_d, wd_d)
            bc_reg = G.to_reg(4095)

            def wgather(sg_, par, m, j):
                for c4 in range(4):
                    val = sg_.dma(G.indirect_dma_start(
                        out=RA[:, (par * 3 + m) * 8192 + c4 * 2048:(par * 3 + m) * 8192 + (c4 + 1) * 2048], out_offset=None,
                        in_=wsrc[m][0], in_offset=bass.IndirectOffsetOnAxis(ap=widx[:, j:j + 1], axis=0),
                        element_offset=c4 * 4096 * 2048, bounds_check=bc_reg, oob_is_err=False), lane=par)
                return val

            def loads_gu(j):
                par = j % 2
                st = blk[j]
                if j - 2 >= 0:
                    W(G, s_pe, blk[j - 2]["gu_done"])
                    W(G, s_v, blk[j - 2]["sw_done"])
                    W(G, s_act, blk[j - 2]["sw_act"])
                for q in range(QB):
                    vtk = s_tk.dma(G.dma_start(out=tki[par][:, q, :],
                                               in_=SLOT[j * BLK + q * 128:j * BLK + (q + 1) * 128, :]), lane=par)
                W(G, s_tk, vtk)
                for q in range(QB):
                    st["xb"] = s_xb.dma(G.indirect_dma_start(
                        out=xb[par][:, q, :], out_offset=None, in_=H2[:, :],
                        in_offset=bass.IndirectOffsetOnAxis(ap=tki[par][:, q, 0:1], axis=0)), lane=par)
                wgather(s_wgu, par, 0, j)
                wgather(s_wgu, par, 1, j)
                s_wgu.dma(G.indirect_dma_start(out=bgt[par][:], out_offset=None, in_=bg_d[:, :],
                                               in_offset=bass.IndirectOffsetOnAxis(ap=widx[:, j:j + 1], axis=0),
                                               bounds_check=bc_reg, oob_is_err=False), lane=par)
                st["wgu"] = s_wgu.dma(G.indirect_dma_start(out=but[par][:], out_offset=None, in_=bu_d[:, :],
                                                           in_offset=bass.IndirectOffsetOnAxis(ap=widx[:, j:j + 1], axis=0),
                                                           bounds_check=bc_reg, oob_is_err=False), lane=par)

            def loads_d(j):
                par = j % 2
                st = blk[j]
                if j - 2 >= 0:
                    W(G, s_pe, blk[j - 2]["d_done"])
                    W(G, s_v, blk[j - 2]["y_done"])
                wgather(s_wd, par, 2, j)
                st["wd"] = s_wd.dma(G.indirect_dma_start(out=bdt[par][:], out_offset=None, in_=bd_d[:, :],
                                                         in_offset=bass.IndirectOffsetOnAxis(ap=bidx[:, j:j + 1], axis=0)), lane=par)

            cnt5 = {"pgi": 0, "pyi": 0}
            pg_free = {}
            py_free = {}

            def TK(j, k):
                par = j % 2
                st = blk[j]
                if k == 0:
                    W(T, s_xb, st["xb"])
                    st["txe"] = {}
                ev_done = st["txe"]
                pb = ptx[k % 2]
                if k >= 2:
                    W(T, s_v, ev_done[k - 2])
                elif j > 0:
                    W(T, s_v, blk[j - 1]["txe"][6 + k])
                for q in range(QB):
                    ins = T.transpose(pb[:, q, :], xb[par][:, q, k * 128:(k + 1) * 128], ident_bf[:])
                vt = s_pe.inc(ins)
                W(V, s_pe, vt)
                ev_done[k] = s_v.inc(V.tensor_copy(out=xbT[:, k, :], in_=pb[:, :, :].rearrange("p q s -> p (q s)")))

            def GU(j):
                par = j % 2
                st = blk[j]
                ev_done = st["txe"]
                W(T, s_v, ev_done[7])
                W(T, s_wgu, st["wgu"])
                W(V, s_wgu, st["wgu"])
                W(A, s_wgu, st["wgu"])
                if j - 2 >= 0:
                    W(V, s_pe, blk[j - 2]["d_done"])
                at = actT[j % 2]
                for fc in range(8):
                    pgi = cnt5["pgi"]
                    gb = pgi % 2
                    if pgi >= 2:
                        W(T, s_v, pg_free[pgi - 2])
                    for k in range(8):
                        T.matmul(pg[gb][:], lhsT=wbuf[par][0][:, k, fc * 128:(fc + 1) * 128], rhs=xbT[:, k, :],
                                 start=(k == 0), stop=(k == 7))
                    for k in range(8):
                        ins = T.matmul(pu[gb][:], lhsT=wbuf[par][1][:, k, fc * 128:(fc + 1) * 128], rhs=xbT[:, k, :],
                                       start=(k == 0), stop=(k == 7))
                    vt = s_pe.inc(ins)
                    W(V, s_pe, vt)
                    W(A, s_pe, vt)
                    v1 = s_v.inc(V.tensor_scalar(out=g_t[gb][:], in0=pg[gb][:], scalar1=bgt[par][:, fc:fc + 1], scalar2=7.0,
                                                 op0=ALU.add, op1=ALU.min))
                    W(A, s_v, v1)
                    a1 = s_act.inc(A.activation(out=sg_t[gb][:], in_=g_t[gb][:], func=AF.Sigmoid, scale=1.702))
                    a2 = s_act.inc(A.activation(out=u_t[gb][:], in_=pu[gb][:], func=AF.Identity,
                                                bias=but[par][:, fc:fc + 1], scale=1.0))
                    W(V, s_act, a2)
                    v2 = s_v.inc(V.tensor_scalar(out=u_t[gb][:], in0=u_t[gb][:], scalar1=7.0, scalar2=-7.0, op0=ALU.min,
                                                 op1=ALU.max))
                    v3 = s_v.inc(V.tensor_tensor(out=g_t[gb][:], in0=g_t[gb][:], in1=sg_t[gb][:], op=ALU.mult))
                    W(V, s_v, v3)
                    v4 = s_v.inc(V.scalar_tensor_tensor(out=at[:, fc, :], in0=u_t[gb][:], scalar=1.0, in1=g_t[gb][:],
                                                        op0=ALU.add, op1=ALU.mult))
                    pg_free[pgi] = v4
                    cnt5["pgi"] += 1
                st["gu_done"] = vt
                st["sw_done"] = v4
                st["sw_act"] = a2

            def DN(j, jn=None):
                par = j % 2
                st = blk[j]
                at = actT[j % 2]
                W(T, s_v, st["sw_done"])
                W(T, s_wd, st["wd"])
                W(V, s_wd, st["wd"])
                if j - 1 >= 0:
                    W(V, s_yd, blk[j - 1]["yd"])
                for q in range(QB):
                    for dh in range(2):
                        pyi = cnt5["pyi"]
                        yb = pyi % 2
                        if pyi >= 2:
                            W(T, s_v, py_free[pyi - 2])
                        for fc in range(8):
                            ins = T.matmul(py[yb][:], lhsT=at[:, fc, q * 128:(q + 1) * 128],
                                           rhs=wbuf[par][2][:, fc, dh * 512:(dh + 1) * 512], start=(fc == 0), stop=(fc == 7))
                        vt = s_pe.inc(ins)
                        W(V, s_pe, vt)
                        py_free[pyi] = s_v.inc(V.tensor_tensor(out=ysb[:, q, dh * 512:(dh + 1) * 512], in0=py[yb][:],
                                                               in1=bdt[par][:, dh * 512:(dh + 1) * 512], op=ALU.add))
                        cnt5["pyi"] += 1
                        if jn is not None:
                            TK(jn, q * 2 + dh)
                if jn is not None:
                    for k in range(2 * QB, 8):
                        TK(jn, k)
                st["d_done"] = vt
                st["y_done"] = py_free[cnt5["pyi"] - 1]
                W(SY, s_v, st["y_done"])
                st["yd"] = s_yd.dma(SY.dma_start(out=Yd[j * BLK:(j + 1) * BLK, :].rearrange("(q p) d -> p q d", p=128),
                                                 in_=ysb[:]))

            loads_gu(0)
            loads_d(0)
            loads_gu(1)
            loads_d(1)
            for k in range(8):
                TK(0, k)
            for j in range(NBLK + 1):
                if j < NBLK:
                    GU(j)
                    if j + 2 < NBLK:
                        loads_gu(j + 2)
                if j >= 1:
                    DN(j - 1, j + 1 if j + 1 < NBLK else None)
                    if j + 1 < NBLK:
                        loads_d(j + 1)
                elif NBLK > 1:
                    for k in range(8):
                        TK(1, k)
            barrier([s_pe, s_v, s_act, s_yd, s_wgu, s_wd, s_xb, s_tk])

        if stop <= 5:
            if dbg:
                dd = dbg_tensor("Y", [NSLOT, D])
                s_out.dma(SY.dma_start(out=dd, in_=Yd))
            W(SY, s_out)
            return nc, dbg_out

        with contextlib.ExitStack() as es:
            RAf = RA[:, :].bitcast(F32)

            class _V:
                def __init__(self, ap): self.ap = ap
                def __getitem__(self, idx): return self.ap

            def fv(i):
                return _V(RAf[:, i * D:(i + 1) * D])
            yg = [[fv(i * 4 + k) for k in range(4)] for i in range(3)]
            x1t = [fv(12 + i) for i in range(3)]
            ot = [fv(17 + i) for i in range(2)]
            ss6 = sb(es, "ss6", [128, 32])
            rs6 = sb(es, "rs6", [128, 32])
            s_g = sig(es, "6g", 3)
            s_d = sig(es, "6d", 5)
            s_v = sig(es, "6v")
            s_act = sig(es, "6a")
            hist = {}

            def v(ins):
                val = s_v.inc(ins); W(V, s_v, val); return val

            def a(ins):
                val = s_act.inc(ins); W(A, s_act, val); return val

            def loads(tt):
                b = tt % 3
                if tt - 3 >= 0:
                    W(G, s_v, hist[tt - 3]["acc"])
                    W(SY, s_v, hist[tt - 3]["acc"])
                for k in range(4):
                    vg = s_g.dma(G.indirect_dma_start(out=yg[b][k][:], out_offset=None, in_=Yd[:, :],
                                                      in_offset=bass.IndirectOffsetOnAxis(
                                                          ap=dest4i[:, tt * 4 + k:tt * 4 + k + 1], axis=0)), lane=b)
                vd = s_d.dma(SY.dma_start(out=x1t[b][:], in_=X1[tt * 128:(tt + 1) * 128, :]), lane=b)
                hist[tt] = {"g": vg, "d": vd}

            accs = [fv(15), fv(16)]

            def comb(tt):
                b = tt % 2
                b3 = tt % 3
                ac = accs[b]
                W(V, s_g, hist[tt]["g"])
                W(V, s_d, hist[tt]["d"])
                v(V.tensor_scalar(out=ac[:], in0=yg[b3][0][:], scalar1=p4[:, tt, 0:1], scalar2=None, op0=ALU.mult))
                for k in range(1, 4):
                    v(V.scalar_tensor_tensor(out=ac[:], in0=yg[b3][k][:], scalar=p4[:, tt, k:k + 1], in1=ac[:],
                                             op0=ALU.mult, op1=ALU.add))
                v(V.tensor_tensor(out=ac[:], in0=ac[:], in1=gf_row, op=ALU.mult))
                vacc = v(V.tensor_tensor(out=ac[:], in0=ac[:], in1=x1t[b3][:], op=ALU.add))
                hist[tt]["acc"] = vacc
                W(A, s_v, vacc)
                if tt - 2 >= 0:
                    W(A, s_d, hist[tt - 2]["od"])
                va = s_act.inc(A.activation(out=ot[b][:], in_=ac[:], func=AF.Square, accum_out=ss6[:, tt:tt + 1]))
                hist[tt]["sq"] = va

            def fin(tt):
                b = tt % 2
                ac = accs[b]
                W(V, s_act, hist[tt]["sd"])
                v(V.reciprocal(out=rs6[:, tt:tt + 1], in_=ss6[:, tt:tt + 1]))
                vo = v(V.scalar_tensor_tensor(out=ot[b][:], in0=ac[:], scalar=rs6[:, tt:tt + 1], in1=fing_row[:], op0=ALU.mult,
                                              op1=ALU.mult))
                hist[tt]["fin"] = vo
                W(SY, s_v, vo)
                hist[tt]["od"] = s_d.dma(SY.dma_start(out=out_d[tt * 128:(tt + 1) * 128, :], in_=ot[b][:]), lane=3 + b)

            def sqrt_(tt):
                W(A, s_act, hist[tt]["sq"])
                hist[tt]["sd"] = s_act.inc(A.activation(out=ss6[:, tt:tt + 1], in_=ss6[:, tt:tt + 1], func=AF.Sqrt,
                                                        scale=1.0 / D, bias=EPS))

            loads(0)
            loads(1)
            loads(2)
            for tt in range(NT + 1):
                if tt < NT:
                    if tt - 2 >= 0:
                        W(V, s_v, hist[tt - 2]["fin"])
                    comb(tt)
                    sqrt_(tt)
                if tt - 1 >= 0:
                    fin(tt - 1)
                    if tt + 2 < NT:
                        loads(tt + 2)
            barrier([s_d, s_v, s_act, s_g])
        W(SY, s_out)
    return nc, dbg_out


def _layout_w(w):
    w5 = w.reshape(32, 4, 2, 128, 1024)
    return np.ascontiguousarray(w5.transpose(1, 0, 3, 2, 4)).reshape(4, 4096, 2048)


_SHARED_CACHE = {}


def make_in_maps(inp, ncores=8):
    f = lambda a: np.ascontiguousarray(a, dtype=np.float32)
    sh = {
        "ada_w": f(inp["ada_w"][0]),
        "ada_b_row": f(inp["ada_b"][0].reshape(1, -1)),
        "ada_b_col": f(inp["ada_b"][0].reshape(48, 128).T),
        "mixg_col": f(inp["mix_norm_g"][0].reshape(8, 128).T),
        "w_in": f(inp["w_in"][0]),
        "convw_col": f(inp["conv_w"][0].T.reshape(4, 128, 4).transpose(1, 0, 2).reshape(128, 16)),
        "lru_cols": f(np.stack([inp["conv_b"][0], inp["rg_b"][0], inp["ig_b"][0], inp["lru_lambda"][0]], axis=-1)
                      .reshape(4, 128, 4).transpose(1, 0, 2).reshape(128, 16)),
        "rg_w": f(inp["rg_w"][0]),
        "ig_w": f(inp["ig_w"][0]),
        "outg_col": f(np.concatenate([inp["attn_out_g"][0], inp["lru_out_g"][0]]).reshape(8, 128).T),
        "w_out": f(inp["w_out"][0]),
        "ffn_g_row": f(inp["ffn_norm_g"][0].reshape(1, -1)),
        "final_g_row": f(inp["final_norm_g"].reshape(1, -1)),
        "router_w": f(inp["router_w"][0]),
        "router_b_row": f(inp["router_b"][0].reshape(1, -1)),
        "wg": _layout_w(f(inp["exp_w_gate"][0])),
        "wu": _layout_w(f(inp["exp_w_up"][0])),
        "wd": _layout_w(f(inp["exp_w_down"][0])),
        "bg": f(inp["exp_b_gate"][0].reshape(32, 8, 128).transpose(0, 2, 1).reshape(4096, 8)),
        "bu": f(inp["exp_b_up"][0].reshape(32, 8, 128).transpose(0, 2, 1).reshape(4096, 8)),
        "bd": f(inp["exp_b_down"][0]),
    }
    maps = []
    for b in range(ncores):
        m = dict(sh)
        m["x"] = f(inp["x"][b])
        m["c_col"] = f(inp["c"][b].reshape(8, 128).T)
        maps.append(m)
    return maps


def kernel(**inputs):
    nc, _ = build_program()
    maps = make_in_maps(inputs, 8)
    res = run_bass_kernel_spmd(nc, maps, core_ids=list(range(8)))
    return np.stack([np.asarray(r["out"], dtype=np.float32) for r in res.results], axis=0)
```

```python
import contextlib
import numpy as np
import concourse.bass as bass
import concourse.mybir as mybir
from concourse.bass_utils import run_bass_kernel_spmd

F32 = mybir.dt.float32
BF16 = mybir.dt.bfloat16
I32 = mybir.dt.int32
ALU = mybir.AluOpType
AF = mybir.ActivationFunctionType
AX = mybir.AxisListType

S = 4096
D = 1024
NT = S // 128
EPS = 1e-6
BLK = 512
QB = BLK // 128
NBLK = 32 + (S * 4 - 32) // BLK
NBP = 80
NSLOT = NBLK * BLK


class Sig:
    def __init__(self, nc, es, name, lanes=1):
        self.sems = [es.enter_context(nc.semaphore(f"{name}_l{i}")) for i in range(lanes)]
        self.cnt = [0] * lanes
        self.lanes = lanes

    @property
    def sem(self):
        return self.sems[0]

    @property
    def n(self):
        return self.cnt[0]

    def inc(self, ins, k=1, lane=0):
        ins.then_inc(self.sems[lane], k)
        self.cnt[lane] += k
        return self.cnt[lane] if self.lanes == 1 else (lane, self.cnt[lane])

    def dma(self, ins, lane=0):
        return self.inc(ins, 16, lane)


def W(eng, sig, val=None):
    if val is None:
        for l in range(sig.lanes):
            if sig.cnt[l] > 0:
                eng.wait_ge(sig.sems[l], sig.cnt[l])
        return
    if isinstance(val, tuple):
        lane, v = val
    else:
        lane, v = 0, val
    if v > 0:
        eng.wait_ge(sig.sems[lane], v)


def build_program(stop=99, dbg=None):
    nc = bass.Bass("TRN2", target_bir_lowering=False)
    dbg_out = {}

    def din(name, shape, dt=F32):
        return nc.dram_tensor(name, list(shape), dt, kind="ExternalInput").ap()

    x_d = din("x", [S, D])
    ccol_d = din("c_col", [128, 8])
    adaw_d = din("ada_w", [D, 6 * D])
    adab_row_d = din("ada_b_row", [1, 6 * D])
    adab_col_d = din("ada_b_col", [128, 48])
    mixg_d = din("mixg_col", [128, 8])
    win_d = din("w_in", [D, 2560])
    convw_d = din("convw_col", [128, 16])
    lruv_d = din("lru_cols", [128, 16])
    rgw_d = din("rg_w", [8, 64, 64])
    igw_d = din("ig_w", [8, 64, 64])
    outg_d = din("outg_col", [128, 8])
    wout_d = din("w_out", [D, D])
    ffng_d = din("ffn_g_row", [1, D])
    fing_d = din("final_g_row", [1, D])
    rw_d = din("router_w", [D, 32])
    rb_d = din("router_b_row", [1, 32])
    wg_d = din("wg", [4, 4096, 2048])
    wu_d = din("wu", [4, 4096, 2048])
    wd_d = din("wd", [4, 4096, 2048])
    bg_d = din("bg", [4096, 8])
    bu_d = din("bu", [4096, 8])
    bd_d = din("bd", [32, D])
    out_d = nc.dram_tensor("out", [S, D], F32, kind="ExternalOutput").ap()
    X1 = nc.dram_tensor("X1s", [S, D], F32, kind="Internal").ap()
    H2 = nc.dram_tensor("H2s", [S, D], BF16, kind="Internal").ap()
    Yd = nc.dram_tensor("Ys", [NSLOT, D], F32, kind="Internal").ap()
    SLOT = nc.dram_tensor("SLOTs", [NSLOT, 16], I32, kind="Internal").ap()

    def dbg_tensor(name, shape, dt=F32):
        t = nc.dram_tensor("dbg_" + name, list(shape), dt, kind="ExternalOutput").ap()
        dbg_out[name] = t
        return t

    V, A, G, T, SY = nc.vector, nc.scalar, nc.gpsimd, nc.tensor, nc.sync

    with contextlib.ExitStack() as top:
        def sb(es, name, shape, dt=F32):
            return es.enter_context(nc.sbuf_tensor(name, list(shape), dt))

        def ps(es, name, shape, dt=F32):
            return es.enter_context(nc.psum_tensor(name, list(shape), dt))

        sig_i = [0]

        def sig(es, name, lanes=1):
            sig_i[0] += 1
            return Sig(nc, es, f"{name}_{sig_i[0]}", lanes)

        s_out = sig(top, "out")

        ident_bf = sb(top, "ident_bf", [128, 128], BF16)
        ident_f = sb(top, "ident_f", [128, 128], F32)
        ones_f = sb(top, "ones_f", [128, 128], F32)
        ones_bf = sb(top, "ones_bf", [128, 128], BF16)
        negones_bf = sb(top, "negones_bf", [128, 128], BF16)
        negU_bf = sb(top, "negU_bf", [128, 128], BF16)
        tri_bf = sb(top, "tri_bf", [128, 128], BF16)
        zeros_bf = sb(top, "zeros_bf", [128, 512], BF16)
        s_c = sig(top, "const")
        s_cv = sig(top, "constv")
        s_cv.inc(V.memset(ones_f[:], 1.0))
        s_cv.inc(V.memset(ones_bf[:], 1.0))
        s_cv.inc(V.memset(negones_bf[:], -1.0))
        s_cv.inc(V.memset(zeros_bf[:], 0.0))
        W(G, s_cv)
        s_c.inc(G.affine_select(out=ident_f[:], in_=ones_f[:], pattern=[[-1, 128]], compare_op=ALU.is_equal,
                                fill=0.0, base=0, channel_multiplier=1))
        s_c.inc(G.affine_select(out=ident_bf[:], in_=ones_bf[:], pattern=[[-1, 128]], compare_op=ALU.is_equal,
                                fill=0.0, base=0, channel_multiplier=1))
        s_c.inc(G.affine_select(out=negU_bf[:], in_=negones_bf[:], pattern=[[-1, 128]], compare_op=ALU.is_ge,
                                fill=0.0, base=0, channel_multiplier=1))
        s_c.inc(G.affine_select(out=tri_bf[:], in_=ones_bf[:], pattern=[[1, 128]], compare_op=ALU.is_gt,
                                fill=0.0, base=0, channel_multiplier=-1))
        for e in (V, A, T, SY):
            W(e, s_c)
        W(G, s_c)

        modrow = sb(top, "modrow", [128, 4 * D])
        modcol = sb(top, "modcol", [128, 16])
        Am = sb(top, "Am", [128, 8])
        fing_row = sb(top, "fing_row", [128, D])
        gm_row = modrow[:, 0:D]
        shf_row = modrow[:, D:2 * D]
        Af_row = modrow[:, 2 * D:3 * D]
        gf_row = modrow[:, 3 * D:4 * D]
        Bm = modcol[:, 0:8]

        ssall = sb(top, "ssall", [128, 32])
        rstdall = sb(top, "rstdall", [128, 32])
        with contextlib.ExitStack() as es:
            xs = [sb(es, f"xs{i}", [128, D]) for i in range(4)]
            junk = sb(es, "junk0", [128, D], BF16)
            s_x = sig(es, "0x", 4)
            s_a = sig(es, "0a")
            s_v0 = sig(es, "0v")
            vxs = {}
            vas = {}
            for tt in range(NT):
                if tt >= 4:
                    W(SY, s_a, vas[tt - 4])
                vxs[tt] = s_x.dma(SY.dma_start(out=xs[tt % 4][:], in_=x_d[tt * 128:(tt + 1) * 128, :]), lane=tt % 4)
                W(A, s_x, vxs[tt])
                vas[tt] = s_a.inc(A.activation(out=junk[:], in_=xs[tt % 4][:], func=AF.Square, accum_out=ssall[:, tt:tt + 1]))
            W(A, s_a)
            va = s_a.inc(A.activation(out=ssall[:], in_=ssall[:], func=AF.Sqrt, scale=1.0 / D, bias=EPS))
            W(V, s_a, va)
            s_v0.inc(V.reciprocal(out=rstdall[:], in_=ssall[:]))
            barrier_early = [s_x, s_a, s_v0]
            for e in (V, A, T, SY, G):
                for s_ in barrier_early:
                    W(e, s_)

        with contextlib.ExitStack() as es:
            ccol = sb(es, "ccol", [128, 8])
            scol = sb(es, "scol", [128, 8])
            scb = sb(es, "scb", [128, 8, 128], BF16)
            scol_b = sb(es, "scol_b", [128, 8], BF16)
            adab_col = sb(es, "adab_col", [128, 48])
            mixg = sb(es, "mixg", [128, 8])
            ffng_row = sb(es, "ffng_row", [128, D])
            adab_row = sb(es, "adab_row", [128, 512])
            aw = [sb(es, f"aw{i}", [128, 8, 512], BF16) for i in range(2)]
            pr = [ps(es, f"p0r{i}", [128, 512]) for i in range(2)]
            pc = ps(es, "p0c", [128, 16])
            s_ld = sig(es, "p0ld")
            s_aw = sig(es, "p0aw", 2)
            s_awfree = sig(es, "p0awf")
            s_a = sig(es, "p0a")
            s_v = sig(es, "p0v")
            s_t = sig(es, "p0t")
            s_b = sig(es, "p0b")
            s_ld.dma(SY.dma_start(out=ccol[:], in_=ccol_d))
            s_ld.dma(SY.dma_start(out=adab_col[:], in_=adab_col_d))
            s_ld.dma(SY.dma_start(out=mixg[:], in_=mixg_d))
            s_ld.dma(SY.dma_start(out=ffng_row[:], in_=ffng_d.broadcast_to([128, D])))
            s_ld.dma(SY.dma_start(out=fing_row[:], in_=fing_d.broadcast_to([128, D])))
            W(A, s_ld)
            s_a.inc(A.activation(out=scol[:], in_=ccol[:], func=AF.Silu))
            W(V, s_a)
            W(V, s_ld)
            for k in range(8):
                s_v.inc(V.tensor_scalar(out=scb[:, k, :], in0=ones_f[:], scalar1=scol[:, k:k + 1], scalar2=None,
                                        op0=ALU.mult))
            s_v.inc(V.tensor_copy(out=scol_b[:], in_=scol[:]))
            aw_view = adaw_d.rearrange("(k p) n -> p k n", p=128)
            W(T, s_v)
            tdone = {}
            vev = {}
            for n in range(12):
                b = n % 2
                if n >= 2:
                    W(G, s_t, tdone[n - 2])
                v_aw = s_aw.dma(G.dma_start(out=aw[b][:], in_=aw_view[:, :, n * 512:(n + 1) * 512]), lane=b)
                W(T, s_aw, v_aw)
                if n < 4:
                    for fcl in range(4):
                        fc = n * 4 + fcl
                        for k in range(8):
                            ins = T.matmul(pc[:, fc:fc + 1], lhsT=aw[b][:, k, fcl * 128:(fcl + 1) * 128],
                                           rhs=scol_b[:, k:k + 1], start=(k == 0), stop=(k == 7))
                    tdone[n] = s_t.inc(ins)
                else:
                    if n - 2 >= 4:
                        W(T, s_v, vev[n - 2])
                    for k in range(8):
                        ins = T.matmul(pr[b][:], lhsT=scb[:, k, :], rhs=aw[b][:, k, :], start=(k == 0), stop=(k == 7))
                    tdone[n] = s_t.inc(ins)
                    if n - 1 >= 4:
                        W(SY, s_v, vev[n - 1])
                    v_b = s_b.dma(SY.dma_start(out=adab_row[:], in_=adab_row_d[:, n * 512:(n + 1) * 512]
                                               .broadcast_to([128, 512])))
                    W(V, s_b, v_b)
                    W(V, s_t, tdone[n])
                    vev[n] = s_v.inc(V.tensor_tensor(out=modrow[:, (n - 4) * 512:(n - 3) * 512], in0=pr[b][:],
                                                     in1=adab_row[:], op=ALU.add))
            W(V, s_t)
            s_v.inc(V.tensor_tensor(out=modcol[:], in0=pc[:], in1=adab_col[:, 0:16], op=ALU.add))
            W(V, s_v)
            s_v.inc(V.scalar_tensor_tensor(out=Am[:], in0=modcol[:, 8:16], scalar=1.0, in1=mixg[:], op0=ALU.add,
                                           op1=ALU.mult))
            s_v.inc(V.scalar_tensor_tensor(out=modrow[:, 2 * D:3 * D], in0=modrow[:, 2 * D:3 * D], scalar=1.0,
                                           in1=ffng_row[:], op0=ALU.add, op1=ALU.mult))
            for e in (A, T, SY, G):
                W(e, s_v)
            W(V, s_v)
            if dbg:
                d1 = dbg_tensor("modrow", [128, 4 * D])
                d2 = dbg_tensor("modcol", [128, 16])
                s_out.dma(SY.dma_start(out=d1, in_=modrow[:]))
                s_out.dma(SY.dma_start(out=d2, in_=modcol[:]))

        if stop <= 0:
            W(SY, s_out)
            return nc, dbg_out

        RA = sb(top, "RA", [128, 49152], BF16)
        RB = sb(top, "RB", [128, 16384], BF16)
        qT = RA[:, 0:16384].rearrange("p (c t) -> p c t", c=4)
        kT = RA[:, 16384:32768].rearrange("p (c t) -> p c t", c=4)
        vtok = RA[:, 32768:49152].rearrange("p (n f) -> p n f", n=32)
        yattnT = RB[:, :].rearrange("p (c t) -> p c t", c=4)
        cnt = {"tile": 0}

        def emit_norm_transpose(es, tiles, hT_of, xn_bufs, pT, xt, sg, hist):
            s_x, s_act, s_v, s_pe = sg
            st = hist
            base = len(st)

            def stage1(jl, tt):
                j = base + jl
                b = j % 2
                if j - 2 in st:
                    W(SY, s_v, st[j - 2]["xn"])
                vx = s_x.dma(SY.dma_start(out=xt[b][:], in_=x_d[tt * 128:(tt + 1) * 128, :]), lane=b)
                W(V, s_x, vx)
                if j - 2 in st:
                    W(V, s_pe, st[j - 2]["tr"])
                v3 = s_v.inc(V.tensor_scalar(out=xn_bufs[b], in0=xt[b][:], scalar1=rstdall[:, tt:tt + 1], scalar2=None,
                                             op0=ALU.mult))
                W(T, s_v, v3)
                if j - 2 in st:
                    W(T, s_v, st[j - 2]["ev"])
                for k in range(8):
                    ins = T.transpose(pT[b][:, k, :], xn_bufs[b][:, k * 128:(k + 1) * 128], ident_bf[:])
                st[j] = {"xn": v3, "tr": s_pe.inc(ins)}

            def stage2(jl):
                j = base + jl
                b = j % 2
                W(V, s_pe, st[j]["tr"])
                dst = hT_of(jl)
                for k in range(8):
                    ins = V.tensor_scalar(out=dst[:, k, :], in0=pT[b][:, k, :], scalar1=Am[:, k:k + 1],
                                          scalar2=Bm[:, k:k + 1], op0=ALU.mult, op1=ALU.add)
                st[j]["ev"] = s_v.inc(ins)

            for jl, tt in enumerate(tiles):
                stage1(jl, tt)
                if jl >= 1:
                    stage2(jl - 1)
            stage2(len(tiles) - 1)
            return st[base + len(tiles) - 1]["ev"]

        with contextlib.ExitStack() as es:
            win = RB[:, 0:12288].rearrange("p (k n) -> p k n", k=8)
            xn_bufs = [RB[:, 12288 + i * 1024:12288 + (i + 1) * 1024] for i in range(2)]
            hT = [sb(es, f"hT{i}", [128, 8, 1024], BF16) for i in range(2)]
            xt = [sb(es, f"xt{i}", [128, D]) for i in range(2)]
            pT = [ps(es, f"pT{i}", [128, 8, 128], BF16) for i in range(2)]
            pj = [ps(es, f"pj{i}", [128, 512]) for i in range(4)]
            s_w = sig(es, "p1w")
            sg = (sig(es, "p1x", 2), sig(es, "p1a"), sig(es, "p1v"), sig(es, "p1t"))
            s_x, s_act, s_v, s_pe = sg
            s_ea = sig(es, "p1ea")
            s_ev = sig(es, "p1ev")
            wv = win_d.rearrange("(k p) n -> p k n", p=128)
            for kh in range(2):
                s_w.dma(G.dma_start(out=win[:, kh * 4:(kh + 1) * 4, :], in_=wv[:, kh * 4:(kh + 1) * 4, 0:1536]))
            W(T, s_w)
            gi = 0
            ev_hist = {}
            hist1 = {}
            hT_last_pe = {}
            vh_q = {}

            def emit_nt(q):
                if q >= 2:
                    W(V, s_pe, hT_last_pe[q - 2])
                hq_ = hT[q % 2]
                vh_q[q] = emit_norm_transpose(es, list(range(q * 8, q * 8 + 8)),
                                              lambda j, hq_=hq_: hq_[:, :, j * 128:(j + 1) * 128], xn_bufs, pT, xt, sg, hist1)

            emit_nt(0)
            for q in range(4):
                hq = hT[q % 2]
                if q + 1 < 4:
                    emit_nt(q + 1)
                W(T, s_v, vh_q[q])
                groups = [("qk", ec, th) for ec in range(8) for th in range(2)] + [("v", tt, 0) for tt in range(8)]
                for (kind, a_, b_) in groups:
                    pb = pj[gi % 4]
                    if gi >= 4:
                        sg_, val_ = ev_hist[gi - 4]
                        W(T, sg_, val_)
                    for k in range(8):
                        if kind == "qk":
                            ins = T.matmul(pb[:], lhsT=win[:, k, a_ * 128:(a_ + 1) * 128],
                                           rhs=hq[:, k, b_ * 512:(b_ + 1) * 512], start=(k == 0), stop=(k == 7))
                        else:
                            ins = T.matmul(pb[:], lhsT=hq[:, k, a_ * 128:(a_ + 1) * 128], rhs=win[:, k, 1024:1536],
                                           start=(k == 0), stop=(k == 7))
                    vt = s_pe.inc(ins)
                    hT_last_pe[q] = vt
                    if kind == "qk":
                        tok0 = q * 1024 + b_ * 512
                        if a_ < 4:
                            dst = qT[:, a_, tok0:tok0 + 512]
                            scale = 0.125
                        else:
                            dst = kT[:, a_ - 4, tok0:tok0 + 512]
                            scale = 1.0
                    else:
                        dst = vtok[:, q * 8 + a_, :]
                        scale = 1.0
                    if gi % 2 == 0:
                        W(A, s_pe, vt)
                        ev_hist[gi] = (s_ea, s_ea.inc(A.activation(out=dst, in_=pb[:], func=AF.Copy, scale=scale)))
                    else:
                        W(V, s_pe, vt)
                        ev_hist[gi] = (s_ev, s_ev.inc(V.tensor_scalar(out=dst, in0=pb[:], scalar1=scale, scalar2=None,
                                                                      op0=ALU.mult)))
                    gi += 1
            for e in (A, V, T, G, SY):
                W(e, s_ea)
                W(e, s_ev)
                W(e, s_pe)
            if dbg and stop == 1:
                dq = dbg_tensor("qkv", [128, 49152], BF16)
                s_out.dma(SY.dma_start(out=dq, in_=RA[:, :]))
                d2 = dbg_tensor("rstd", [128, 32])
                s_out.dma(SY.dma_start(out=d2, in_=rstdall[:, :]))
                d3 = dbg_tensor("hT1", [128, 8192], BF16)
                s_out.dma(SY.dma_start(out=d3, in_=hT[1][:, :, :]))
                d4 = dbg_tensor("Am", [128, 8])
                s_out.dma(SY.dma_start(out=d4, in_=Am[:, :]))
        if stop <= 1:
            W(SY, s_out)
            return nc, dbg_out

        with contextlib.ExitStack() as es:
            e_t = [sb(es, f"e_t{i}", [128, 512]) for i in range(3)]
            S_t = [sb(es, f"S_t{i}", [128, 512], BF16) for i in range(3)]
            w_t = [sb(es, f"w_t{i}", [128, 512], BF16) for i in range(2)]
            R_t = [sb(es, f"R_t{i}", [128, 512], BF16) for i in range(2)]
            mask = [sb(es, f"mask{i}", [128, 512], BF16) for i in range(4)]
            pz = [ps(es, f"pz{i}", [128, 512]) for i in range(2)]
            pw = [ps(es, f"pw{i}", [128, 512]) for i in range(3)]
            po = [ps(es, f"po{i}", [128, 512]) for i in range(2)]
            s_pe = sig(es, "a_pe")
            s_act = sig(es, "a_act")
            s_pool = sig(es, "a_pool")
            s_v = sig(es, "a_v")
            for rel in range(4):
                s_pool.inc(G.affine_select(out=mask[rel][:], in_=zeros_bf[:], pattern=[[1, 512]], compare_op=ALU.is_gt,
                                           fill=-30000.0, base=-128 * rel, channel_multiplier=-1))
            W(T, s_pool)
            pairs = []
            groups = []
            for h in range(8):
                for c in range(8):
                    g = len(groups)
                    kbs = list(range(4 * c + 3, -1, -1))
                    groups.append((h, c, len(pairs), len(pairs) + len(kbs) - 1))
                    for kb in kbs:
                        pairs.append((h, c, kb, g))
            n = len(pairs)
            vA, vB, vC, vD, vE, vF, vG = {}, {}, {}, {}, {}, {}, {}
            gev = {}

            def ops(j):
                h, c, kb, g = pairs[j]
                hp = (h % 2) * 64
                kh = kT[hp:hp + 64, h // 2, kb * 128:(kb + 1) * 128]
                qh = qT[hp:hp + 64, h // 2, c * 512:(c + 1) * 512]
                rel = kb - 4 * c
                first = (j == groups[g][2])
                last = (j == groups[g][3])
                return h, c, kb, g, hp, kh, qh, rel, first, last

            def colsl(j):
                rel = pairs[j][2] - 4 * pairs[j][1]
                return slice(128 * rel, 512) if rel >= 1 else slice(0, 512)

            def zmm(out, j, stop_):
                h, c, kb, g, hp, kh, qh, rel, first, last = ops(j)
                cs = colsl(j)
                if rel >= 0:
                    T.matmul(out[:, cs], lhsT=kh, rhs=qh[:, cs], start=True, stop=False)
                    return T.matmul(out[:, cs], lhsT=ident_bf[:], rhs=mask[rel][:, cs], start=False, stop=stop_)
                return T.matmul(out[:, cs], lhsT=kh, rhs=qh[:, cs], start=True, stop=stop_)

            def st_A(j):
                if j - 2 >= 0:
                    W(T, s_act, vB[j - 2])
                vA[j] = s_pe.inc(zmm(pz[j % 2], j, True))

            def st_B(j):
                W(A, s_pe, vA[j])
                cs = colsl(j)
                vB[j] = s_act.inc(A.activation(out=e_t[j % 3][:, cs], in_=pz[j % 2][:, cs], func=AF.Exp))

            def st_C(j):
                W(A, s_act, vB[j])
                if j - 3 >= 0:
                    W(A, s_pe, vD[j - 3])
                    if (j - 3) in vE:
                        W(A, s_pool, vE[j - 3])
                cs = colsl(j)
                vC[j] = s_act.inc(A.activation(out=S_t[j % 3][:, cs], in_=e_t[j % 3][:, cs], func=AF.Ln, bias=1.0, scale=1.0))

            def st_E(j):
                h, c, kb, g, hp, kh, qh, rel, first, last = ops(j)
                if last:
                    return
                cs = colsl(j)
                W(G, s_act, vC[j])
                if j - 1 >= 0:
                    W(G, s_pe, vD[j - 1])
                Rn = R_t[(j + 1) % 2]
                if cs.start > 0:
                    s_pool.inc(G.memset(Rn[:, 0:cs.start], 0.0))
                if first:
                    vE[j] = s_pool.inc(G.tensor_copy(out=Rn[:, cs], in_=S_t[j % 3][:, cs]))
                else:
                    W(G, s_pool, vE[j - 1])
                    vE[j] = s_pool.inc(G.tensor_tensor(out=Rn[:, cs], in0=R_t[j % 2][:, cs], in1=S_t[j % 3][:, cs],
                                                       op=ALU.add))

            def st_D(j):
                h, c, kb, g, hp, kh, qh, rel, first, last = ops(j)
                W(T, s_act, vC[j])
                if j - 3 >= 0:
                    W(T, s_act, vF[j - 3])
                if not first:
                    W(T, s_pool, vE[j - 1])
                cs = colsl(j)
                zmm(pw[j % 3], j, False)
                ins = T.matmul(pw[j % 3][:, cs], lhsT=negU_bf[:], rhs=S_t[j % 3][:, cs], start=False, stop=first)
                if not first:
                    ins = T.matmul(pw[j % 3][:, cs], lhsT=negones_bf[:], rhs=R_t[j % 2][:, cs], start=False, stop=True)
                vD[j] = s_pe.inc(ins)

            def st_F(j):
                W(A, s_pe, vD[j])
                if j - 2 >= 0:
                    W(A, s_pe, vG[j - 2])
                cs = colsl(j)
                vF[j] = s_act.inc(A.activation(out=w_t[j % 2][:, cs], in_=pw[j % 3][:, cs], func=AF.Exp))

            def st_G(j):
                h, c, kb, g, hp, kh, qh, rel, first, last = ops(j)
                W(T, s_act, vF[j])
                if first and g - 2 >= 0:
                    W(T, s_v, gev[g - 2])
                cs = colsl(j)
                if first:
                    T.matmul(po[g % 2][hp:hp + 64, :], lhsT=zeros_bf[:, 0:64], rhs=mask[0][:], start=True, stop=False)
                ins = T.matmul(po[g % 2][hp:hp + 64, cs], lhsT=vtok[:, kb, h * 64:(h + 1) * 64], rhs=w_t[j % 2][:, cs],
                               start=False, stop=last)
                vG[j] = s_pe.inc(ins)
                if last:
                    W(V, s_pe, vG[j])
                    gev[g] = s_v.inc(V.tensor_copy(out=yattnT[hp:hp + 64, h // 2, c * 512:(c + 1) * 512],
                                                   in_=po[g % 2][hp:hp + 64, :]))

            for i in range(-3, n + 2):
                if 0 <= i + 3 < n:
                    st_A(i + 3)
                if 0 <= i + 2 < n:
                    st_B(i + 2)
                if 0 <= i + 1 < n:
                    st_C(i + 1)
                if 0 <= i < n:
                    st_D(i)
                if 0 <= i + 1 < n:
                    st_E(i + 1)
                if 0 <= i - 1 < n:
                    st_F(i - 1)
                if 0 <= i - 2 < n:
                    st_G(i - 2)
            for e in (A, V, T, G, SY):
                W(e, s_v)
                W(e, s_pe)
                W(e, s_act)
                W(e, s_pool)
            if dbg and stop == 2:
                dq = dbg_tensor("yattn", [128, 16384], BF16)
                s_out.dma(SY.dma_start(out=dq, in_=RB[:, :]))
        if stop <= 2:
            W(SY, s_out)
            return nc, dbg_out

        ENG = (V, A, G, T, SY)

        def barrier(sigs, engines=ENG):
            for e in engines:
                for s_ in sigs:
                    W(e, s_)

        ylruT = RA[:, 0:16384].rearrange("p (c t) -> p c t", c=4)
        with contextlib.ExitStack() as es:
            win = RA[:, 16384:24576].rearrange("p (k n) -> p k n", k=8)
            hT = [RA[:, 24576 + i * 8192:24576 + (i + 1) * 8192].rearrange("p (k t) -> p k t", k=8) for i in range(2)]
            xn_bufs = [RA[:, 40960 + i * 1024:40960 + (i + 1) * 1024] for i in range(2)]
            xt = [sb(es, f"xtb{i}", [128, D]) for i in range(2)]
            HW_ = 512
            xpad = [sb(es, f"xpad{i}", [128, HW_ + 3]) for i in range(2)]
            xc = [sb(es, f"xc{i}", [128, HW_]) for i in range(2)]
            r_t = [sb(es, f"r_t{i}", [128, HW_]) for i in range(2)]
            ig_t = [sb(es, f"ig_t{i}", [128, HW_]) for i in range(2)]
            b_t = [sb(es, f"b_t{i}", [128, HW_]) for i in range(2)]
            h_t = [sb(es, f"h_t{i}", [128, HW_]) for i in range(2)]
            gg = [sb(es, f"gg{i}", [128, HW_]) for i in range(2)]
            g2 = [sb(es, f"g2{i}", [128, HW_]) for i in range(2)]
            xcb = [sb(es, f"xcb{i}", [128, HW_], BF16) for i in range(2)]
            rgbd_b = sb(es, "rgbd_b", [128, 4, 128], BF16)
            igbd_b = sb(es, "igbd_b", [128, 4, 128], BF16)
            rgbd = sb(es, "rgbd", [128, 4, 128])
            igbd = sb(es, "igbd", [128, 4, 128])
            convw = sb(es, "convw", [128, 16])
            lruv = sb(es, "lruv", [128, 16])
            cA = sb(es, "cA", [128, 4])
            tmp4 = sb(es, "tmp4", [128, 4])
            hlast = sb(es, "hlast", [128, 4])
            xtail = sb(es, "xtail", [128, 4, 3])
            pT = [ps(es, f"pTb{i}", [128, 8, 128], BF16) for i in range(2)]
            pxr = [ps(es, f"pxr{i}", [128, HW_]) for i in range(2)]
            pgr = [ps(es, f"pgr{i}", [128, HW_]) for i in range(2)]
            pgt = [ps(es, f"pgt{i}", [128, HW_]) for i in range(2)]
            s_w = sig(es, "lw")
            sg = (sig(es, "lx", 2), sig(es, "la"), sig(es, "lv"), sig(es, "lt"))
            s_x, s_act, s_v, s_pe = sg
            s_g = sig(es, "lg")
            allsig = [s_w, s_x, s_act, s_v, s_pe, s_g]

            def v(ins):
                val = s_v.inc(ins); W(V, s_v, val); return val

            def a(ins):
                val = s_act.inc(ins); W(A, s_act, val); return val

            def g(ins):
                val = s_g.inc(ins); W(G, s_g, val); return val

            wv = win_d.rearrange("(k p) n -> p k n", p=128)
            for kh in range(2):
                s_w.dma(G.dma_start(out=win[:, kh * 4:(kh + 1) * 4, :], in_=wv[:, kh * 4:(kh + 1) * 4, 1536:2560]))
            v(V.memset(rgbd[:], 0.0))
            v(V.memset(igbd[:], 0.0))
            v(V.memset(hlast[:], 0.0))
            v(V.memset(xtail[:], 0.0))
            W(SY, s_v)
            for cc in range(4):
                for hh in range(2):
                    s_w.dma(SY.dma_start(out=rgbd[hh * 64:(hh + 1) * 64, cc, hh * 64:(hh + 1) * 64], in_=rgw_d[2 * cc + hh]))
                    s_w.dma(SY.dma_start(out=igbd[hh * 64:(hh + 1) * 64, cc, hh * 64:(hh + 1) * 64], in_=igw_d[2 * cc + hh]))
            s_w.dma(SY.dma_start(out=convw[:], in_=convw_d))
            s_w.dma(SY.dma_start(out=lruv[:], in_=lruv_d))
            barrier([s_w])
            v(V.tensor_copy(out=rgbd_b[:], in_=rgbd[:]))
            v(V.tensor_copy(out=igbd_b[:], in_=igbd[:]))
            lam = lruv[:, :].rearrange("p (c f) -> p c f", f=4)[:, :, 3]
            a(A.activation(out=tmp4[:], in_=lam, func=AF.Exp, scale=-1.0))
            a(A.activation(out=tmp4[:], in_=tmp4[:], func=AF.Ln, bias=1.0, scale=1.0))
            W(V, s_act)
            v(V.tensor_scalar(out=cA[:], in0=tmp4[:], scalar1=-8.0, scalar2=None, op0=ALU.mult))
            barrier(allsig)
            hist2 = {}
            un = {}

            def S1(u, q, cc, hf, hq):
                p = u % 2
                U = un[u] = {}
                cw = lambda j: convw[:, cc * 4 + j:cc * 4 + j + 1]
                lv = lambda j: lruv[:, cc * 4 + j:cc * 4 + j + 1]
                tsl = slice(hf * HW_, (hf + 1) * HW_)
                if u - 2 in un:
                    P_ = un[u - 2]
                    for e in (T, V, A, G):
                        W(e, s_v, P_["v_end"])
                        W(e, s_act, P_["a_end"])
                        W(e, s_pe, P_["t_end"])
                        W(e, s_g, P_["g_end"])
                for k in range(8):
                    T.matmul(pxr[p][:], lhsT=win[:, k, cc * 128:(cc + 1) * 128], rhs=hq[:, k, tsl], start=(k == 0), stop=(k == 7))
                for k in range(8):
                    ins = T.matmul(pgr[p][:], lhsT=win[:, k, 512 + cc * 128:512 + (cc + 1) * 128], rhs=hq[:, k, tsl],
                                   start=(k == 0), stop=(k == 7))
                vt = s_pe.inc(ins)
                W(V, s_pe, vt)
                W(A, s_pe, vt)
                v(V.tensor_copy(out=xpad[p][:, 0:3], in_=xtail[:, cc, :]))
                v(V.tensor_copy(out=xpad[p][:, 3:HW_ + 3], in_=pxr[p][:]))
                v(V.tensor_copy(out=xtail[:, cc, :], in_=xpad[p][:, HW_:HW_ + 3]))
                v(V.tensor_scalar(out=xc[p][:], in0=xpad[p][:, 0:HW_], scalar1=cw(0), scalar2=lv(0), op0=ALU.mult, op1=ALU.add))
                for j in range(1, 4):
                    vxc = v(V.scalar_tensor_tensor(out=xc[p][:], in0=xpad[p][:, j:j + HW_], scalar=cw(j), in1=xc[p][:],
                                                   op0=ALU.mult, op1=ALU.add))
                va = a(A.activation(out=gg[p][:], in_=pgr[p][:], func=AF.Identity))
                W(G, s_v, vxc)
                vxb = g(G.tensor_copy(out=xcb[p][:], in_=xc[p][:]))
                W(T, s_g, vxb)
                W(G, s_act, va)
                g(G.tensor_tensor(out=g2[p][:], in0=gg[p][:], in1=gg[p][:], op=ALU.mult))
                g(G.tensor_scalar(out=g2[p][:], in0=g2[p][:], scalar1=0.044715, scalar2=1.0, op0=ALU.mult, op1=ALU.add))
                vg = g(G.tensor_tensor(out=g2[p][:], in0=g2[p][:], in1=gg[p][:], op=ALU.mult))
                ins = T.matmul(pgt[p][:], lhsT=rgbd_b[:, cc, :], rhs=xcb[p][:], start=True, stop=True)
                vt = s_pe.inc(ins)
                W(A, s_pe, vt)
                va = a(A.activation(out=r_t[p][:], in_=pgt[p][:], func=AF.Sigmoid, bias=lv(1), scale=1.0))
                W(T, s_act, va)
                ins = T.matmul(pgt[p][:], lhsT=igbd_b[:, cc, :], rhs=xcb[p][:], start=True, stop=True)
                vt = s_pe.inc(ins)
                W(A, s_pe, vt)
                a(A.activation(out=ig_t[p][:], in_=pgt[p][:], func=AF.Sigmoid, bias=lv(2), scale=1.0))
                W(A, s_g, vg)
                vgs = a(A.activation(out=g2[p][:], in_=g2[p][:], func=AF.Sigmoid, scale=1.5957691216))
                W(G, s_act, vgs)
                vg = g(G.tensor_tensor(out=g2[p][:], in0=g2[p][:], in1=gg[p][:], op=ALU.mult))
                va = a(A.activation(out=r_t[p][:], in_=r_t[p][:], func=AF.Exp, scale=cA[:, cc:cc + 1]))
                U["a"] = va
                U["t_end"] = vt
                U["g_end"] = vg
                U["cc"] = cc
                U["q"] = q
                U["hf"] = hf

            def S2a(u):
                p = u % 2
                U = un[u]
                cc, q, hf = U["cc"], U["q"], U["hf"]
                W(V, s_act, U["a"])
                v(V.scalar_tensor_tensor(out=b_t[p][:], in0=r_t[p][:], scalar=-1.0, in1=r_t[p][:], op0=ALU.mult,
                                         op1=ALU.mult))
                vb = v(V.tensor_scalar(out=b_t[p][:], in0=b_t[p][:], scalar1=1.0, scalar2=1e-30, op0=ALU.add,
                                       op1=ALU.max))
                W(A, s_v, vb)
                va = a(A.activation(out=b_t[p][:], in_=b_t[p][:], func=AF.Sqrt))
                U["a_end"] = va

            def S2b(u):
                p = u % 2
                U = un[u]
                cc, q, hf = U["cc"], U["q"], U["hf"]
                W(V, s_act, U["a_end"])
                v(V.tensor_tensor(out=b_t[p][:], in0=b_t[p][:], in1=ig_t[p][:], op=ALU.mult))
                v(V.tensor_tensor(out=b_t[p][:], in0=b_t[p][:], in1=xc[p][:], op=ALU.mult))
                v(V.tensor_tensor_scan(out=h_t[p][:], data0=r_t[p][:], data1=b_t[p][:], initial=hlast[:, cc:cc + 1],
                                       op0=ALU.mult, op1=ALU.add))
                v(V.tensor_copy(out=hlast[:, cc:cc + 1], in_=h_t[p][:, HW_ - 1:HW_]))
                W(V, s_g, U["g_end"])
                t0 = q * 1024 + hf * HW_
                U["v_end"] = v(V.tensor_tensor(out=ylruT[:, cc, t0:t0 + HW_], in0=g2[p][:], in1=h_t[p][:], op=ALU.mult))

            u = 0
            vhq = {}
            qlast = {}

            def emit_nt(q):
                if q >= 2:
                    W(V, s_pe, qlast[q - 2])
                hq_ = hT[q % 2]
                vhq[q] = emit_norm_transpose(es, list(range(q * 8, q * 8 + 8)),
                                             lambda j, hq_=hq_: hq_[:, :, j * 128:(j + 1) * 128], xn_bufs, pT, xt, sg, hist2)

            emit_nt(0)
            for q in range(4):
                hq = hT[q % 2]
                if q + 1 < 4:
                    emit_nt(q + 1)
                W(T, s_v, vhq[q])
                first = u
                for cc in range(4):
                    for hf in range(2):
                        if u - 1 >= first:
                            S2a(u - 1)
                        S1(u, q, cc, hf, hq)
                        if u - 1 >= first:
                            S2b(u - 1)
                        u += 1
                S2a(u - 1)
                S2b(u - 1)
                qlast[q] = un[u - 1]["t_end"]
            barrier(allsig)
            if dbg and stop == 3:
                dq = dbg_tensor("ylru", [128, 16384], BF16)
                s_out.dma(SY.dma_start(out=dq, in_=RA[:, 0:16384]))
                d2 = dbg_tensor("rstd3", [128, 32])
                s_out.dma(SY.dma_start(out=d2, in_=rstdall[:, :]))
                d3 = dbg_tensor("hT23", [128, 16384], BF16)
                s_out.dma(SY.dma_start(out=d3, in_=RA[:, 24576:40960]))
        if stop <= 3:
            W(SY, s_out)
            return nc, dbg_out

        M8all = sb(top, "M8all", [128, 32, 8])
        Lall = sb(top, "Lall", [128, 32, 32])
        rank_all = sb(top, "rank_all", [128, 32, 32])
        cnt_run = sb(top, "cnt_run", [128, 32])
        dest4f = sb(top, "dest4f", [128, 32, 4])
        dest4i = sb(top, "dest4i", [128, 128], I32)
        p4 = sb(top, "p4", [128, 32, 4])
        widx = sb(top, "widx", [128, NBP], I32)
        bidx = sb(top, "bidx", [128, NBP], I32)

        with contextlib.ExitStack() as es:
            wout = RA[:, 16384:24576].rearrange("p (k n) -> p k n", k=8)
            xt = [sb(es, f"x3_{i}", [128, D]) for i in range(2)]
            x1 = [sb(es, f"x1_{i}", [128, D]) for i in range(2)]
            t1 = sb(es, "t1", [128, D])
            h2 = sb(es, "h2", [128, D])
            h2b = [sb(es, f"h2b{i}", [128, D], BF16) for i in range(2)]
            h2T = sb(es, "h2T", [128, 8, 128])
            ysq = sb(es, "ysq", [128, 8, 128], BF16)
            rw = sb(es, "rw", [128, 8, 32])
            rb_row = sb(es, "rb_row", [128, 32])
            outg = sb(es, "outg", [128, 8])
            sd2 = sb(es, "sd2", [128, 2])
            rs2 = sb(es, "rs2", [128, 2])
            ss3 = sb(es, "ss3", [128, 1])
            rs3 = sb(es, "rs3", [128, 1])
            Mf = sb(es, "Mf", [128, 32])
            Mbf = sb(es, "Mbf", [128, 32], BF16)
            PA = [ps(es, f"PA{i}", [128, 512]) for i in range(2)]
            PL = [ps(es, f"PL{i}", [128, 512]) for i in range(2)]
            ptr = ps(es, "ptr", [128, 4, 128])
            psm = ps(es, "psm", [128, 512])
            psl = ps(es, "psl", [128, 512])
            psr = ps(es, "psr", [128, 512])
            s_d = sig(es, "3d", 2)
            s_v = sig(es, "3v")
            s_act = sig(es, "3a")
            s_pe = sig(es, "3t")
            s_st = sig(es, "3st", 4)
            allsig = [s_d, s_v, s_act, s_pe, s_st]

            def v(ins):
                val = s_v.inc(ins); W(V, s_v, val); return val

            def a(ins):
                val = s_act.inc(ins); W(A, s_act, val); return val

            s_d.dma(SY.dma_start(out=rw[:], in_=rw_d.rearrange("(k p) n -> p k n", p=128)))
            s_d.dma(SY.dma_start(out=rb_row[:], in_=rb_d.broadcast_to([128, 32])))
            s_d.dma(SY.dma_start(out=outg[:], in_=outg_d))
            v(V.memset(cnt_run[:], 0.0))
            wov = wout_d.rearrange("(k p) n -> p k n", p=128)
            for k in range(8):
                vd = s_d.dma(SY.dma_start(out=t1[:], in_=wov[:, k, :]))
                W(V, s_d, vd)
                vv = v(V.scalar_tensor_tensor(out=wout[:, k, :], in0=t1[:], scalar=outg[:, k:k + 1], in1=gm_row,
                                              op0=ALU.mult, op1=ALU.mult))
                W(SY, s_v, vv)
            barrier(allsig)
            s_g = sig(es, "3g")
            allsig.append(s_g)
            st3 = {tt: {} for tt in range(NT)}

            def vv(ins):
                return s_v.inc(ins)

            def stA(tt):
                b = tt % 2
                S_ = st3[tt]
                tsl = slice(tt * 128, (tt + 1) * 128)
                if tt - 2 >= 0:
                    W(SY, s_v, st3[tt - 2]["x1"])
                S_["x"] = s_d.dma(SY.dma_start(out=xt[b][:], in_=x_d[tsl, :]), lane=b)
                if tt - 1 >= 0:
                    W(G, s_pe, st3[tt - 1]["stats"])
                s_g.inc(G.tensor_tensor(out=ysq[:, 0:4, :], in0=yattnT[:, :, tsl], in1=yattnT[:, :, tsl], op=ALU.mult))
                vq = s_g.inc(G.tensor_tensor(out=ysq[:, 4:8, :], in0=ylruT[:, :, tsl], in1=ylruT[:, :, tsl], op=ALU.mult))
                W(T, s_g, vq)
                if tt - 1 >= 0:
                    W(T, s_act, st3[tt - 1]["sd2"])
                for gI in range(2):
                    for c in range(4):
                        ins = T.matmul(psm[:, gI:gI + 1], lhsT=ysq[:, gI * 4 + c, :], rhs=ones_bf[:, 0:1], start=(c == 0),
                                       stop=(c == 3))
                S_["stats"] = s_pe.inc(ins)
                if tt - 1 >= 0:
                    W(T, s_v, st3[tt - 1]["comb"])
                for dh in range(2):
                    for c in range(4):
                        T.matmul(PA[dh][:], lhsT=yattnT[:, c, tsl], rhs=wout[:, c, dh * 512:(dh + 1) * 512], start=(c == 0),
                                 stop=(c == 3))
                    for c in range(4):
                        ins = T.matmul(PL[dh][:], lhsT=ylruT[:, c, tsl], rhs=wout[:, 4 + c, dh * 512:(dh + 1) * 512],
                                       start=(c == 0), stop=(c == 3))
                S_["op"] = s_pe.inc(ins)
                W(A, s_pe, S_["stats"])
                if tt - 1 >= 0:
                    W(A, s_v, st3[tt - 1]["rs2"])
                S_["sd2"] = s_act.inc(A.activation(out=sd2[:], in_=psm[:, 0:2], func=AF.Sqrt, scale=1.0 / 512, bias=EPS))

            def stB1(tt):
                b = tt % 2
                S_ = st3[tt]
                tsl = slice(tt * 128, (tt + 1) * 128)
                W(V, s_act, S_["sd2"])
                S_["rs2"] = vv(V.reciprocal(out=rs2[:], in_=sd2[:]))
                W(V, s_v, S_["rs2"])
                W(V, s_pe, S_["op"])
                W(V, s_d, S_["x"])
                if tt - 1 >= 0:
                    W(V, s_act, st3[tt - 1]["sq"])
                for dh in range(2):
                    cs = slice(dh * 512, (dh + 1) * 512)
                    v_ = vv(V.tensor_scalar(out=t1[:, cs], in0=PA[dh][:], scalar1=rs2[:, 0:1], scalar2=None, op0=ALU.mult))
                    W(V, s_v, v_)
                    v_ = vv(V.scalar_tensor_tensor(out=t1[:, cs], in0=PL[dh][:], scalar=rs2[:, 1:2], in1=t1[:, cs], op0=ALU.mult,
                                                   op1=ALU.add))
                S_["comb"] = v_
                W(V, s_v, v_)
                if tt - 2 >= 0:
                    W(V, s_st, st3[tt - 2]["x1st"])
                    W(V, s_v, st3[tt - 2]["h2"])
                S_["x1"] = vv(V.tensor_tensor(out=x1[b][:], in0=t1[:], in1=xt[b][:], op=ALU.add))
                W(SY, s_v, S_["x1"])
                S_["x1st"] = s_st.dma(SY.dma_start(out=X1[tsl, :], in_=x1[b][:]), lane=b)
                W(A, s_v, S_["x1"])
                va = s_act.inc(A.activation(out=t1[:], in_=x1[b][:], func=AF.Square, accum_out=ss3[:]))
                S_["sq"] = va
                W(A, s_act, va)
                if tt - 1 >= 0:
                    W(A, s_v, st3[tt - 1]["rs3"])
                S_["ss3"] = s_act.inc(A.activation(out=ss3[:], in_=ss3[:], func=AF.Sqrt, scale=1.0 / D, bias=EPS))

            def stB2(tt):
                b = tt % 2
                S_ = st3[tt]
                tsl = slice(tt * 128, (tt + 1) * 128)
                W(V, s_act, S_["ss3"])
                S_["rs3"] = vv(V.reciprocal(out=rs3[:], in_=ss3[:]))
                W(V, s_v, S_["rs3"])
                if tt - 1 >= 0:
                    W(V, s_pe, st3[tt - 1]["tr"])
                    W(V, s_act, st3[tt - 1]["h2b"])
                v_ = vv(V.scalar_tensor_tensor(out=h2[:], in0=x1[b][:], scalar=rs3[:, 0:1], in1=Af_row, op0=ALU.mult,
                                               op1=ALU.mult))
                W(V, s_v, v_)
                S_["h2"] = vv(V.tensor_tensor(out=h2[:], in0=h2[:], in1=shf_row, op=ALU.add))
                W(A, s_v, S_["h2"])
                if tt - 2 >= 0:
                    W(A, s_st, st3[tt - 2]["h2st"])
                S_["h2b"] = s_act.inc(A.activation(out=h2b[b][:], in_=h2[:], func=AF.Copy))
                W(SY, s_act, S_["h2b"])
                S_["h2st"] = s_st.dma(SY.dma_start(out=H2[tsl, :], in_=h2b[b][:]), lane=2 + b)
                W(T, s_v, S_["h2"])
                if tt - 1 >= 0:
                    W(T, s_act, st3[tt - 1]["h2T"])
                for half in range(2):
                    if half == 1:
                        W(T, s_act, S_["h2T"])
                    for k in range(4):
                        kk = half * 4 + k
                        ins = T.transpose(ptr[:, k, :], h2[:, kk * 128:(kk + 1) * 128], ident_f[:])
                    S_["tr"] = s_pe.inc(ins)
                    W(A, s_pe, S_["tr"])
                    if tt - 1 >= 0 and half == 0:
                        W(A, s_pe, st3[tt - 1]["rt"])
                    S_["h2T"] = s_act.inc(A.activation(out=h2T[:, half * 4:(half + 1) * 4, :], in_=ptr[:], func=AF.Identity))
                W(T, s_act, S_["h2T"])
                if tt - 1 >= 0:
                    W(T, s_v, st3[tt - 1]["L"])
                for k in range(8):
                    ins = T.matmul(psl[:, 32:64], lhsT=h2T[:, k, :], rhs=rw[:, k, :], start=(k == 0), stop=(k == 7))
                S_["rt"] = s_pe.inc(ins)

            def stC(tt):
                S_ = st3[tt]
                W(V, s_pe, S_["rt"])
                S_["L"] = vv(V.tensor_tensor(out=Lall[:, tt, :], in0=psl[:, 32:64], in1=rb_row[:], op=ALU.add))
                W(V, s_v, S_["L"])
                v_ = vv(V.max(out=M8all[:, tt, :], in_=Lall[:, tt, :]))
                W(V, s_v, v_)
                if tt - 1 >= 0:
                    W(V, s_pe, st3[tt - 1]["rk"])
                v_ = vv(V.tensor_scalar(out=Mf[:], in0=Lall[:, tt, :], scalar1=M8all[:, tt, 3:4], scalar2=None, op0=ALU.is_ge))
                W(V, s_v, v_)
                vm = vv(V.tensor_copy(out=Mbf[:], in_=Mf[:]))
                W(T, s_v, vm)
                if tt - 1 >= 0:
                    W(T, s_v, st3[tt - 1]["cnt"])
                T.matmul(psr[:, 64:96], lhsT=tri_bf[:], rhs=Mbf[:], start=True, stop=True)
                ins = T.matmul(psr[:, 96:128], lhsT=ones_bf[:], rhs=Mbf[:], start=True, stop=True)
                S_["rk"] = s_pe.inc(ins)

            def stC2(tt):
                S_ = st3[tt]
                W(V, s_pe, S_["rk"])
                if tt - 1 >= 0:
                    W(V, s_v, st3[tt - 1]["cnt"])
                v_ = vv(V.tensor_tensor(out=rank_all[:, tt, :], in0=psr[:, 64:96], in1=cnt_run[:], op=ALU.add))
                W(V, s_v, v_)
                S_["cnt"] = vv(V.tensor_tensor(out=cnt_run[:], in0=psr[:, 96:128], in1=cnt_run[:], op=ALU.add))

            stA(0)
            for tt in range(NT):
                stB1(tt)
                if tt + 1 < NT:
                    stA(tt + 1)
                if tt - 1 >= 0:
                    stC(tt - 1)
                stB2(tt)
                if tt - 1 >= 0:
                    stC2(tt - 1)
            stC(NT - 1)
            stC2(NT - 1)
            barrier(allsig)

        with contextlib.ExitStack() as es:
            padded = sb(es, "padded", [128, 32])
            pad_end = sb(es, "pad_end", [128, 32])
            pad_start = sb(es, "pad_start", [128, 32])
            tmp32 = sb(es, "tmp32", [128, 32])
            destE = sb(es, "destE", [128, 32, 32])
            oh = sb(es, "oh", [128, 32, 32])
            E4 = sb(es, "E4", [128, 32, 4])
            den = sb(es, "den", [128, 32])
            tokid = sb(es, "tokid", [128, 32, 16], I32)
            zt = sb(es, "zt", [128, NSLOT * 16 // 128], I32)
            jv_i = sb(es, "jv_i", [128, NBP], I32)
            jv = sb(es, "jv", [128, NBP])
            pid_i = sb(es, "pid_i", [128, NBP], I32)
            pid = sb(es, "pid", [128, NBP])
            cmp = sb(es, "cmp", [128, NBP, 32])
            be = sb(es, "be", [128, NBP])
            s_v = sig(es, "4v")
            s_act = sig(es, "4a")
            s_g = sig(es, "4g")
            s_z = sig(es, "4z")

            def v(ins):
                val = s_v.inc(ins); W(V, s_v, val); return val

            def g(ins):
                val = s_g.inc(ins); W(G, s_g, val); return val

            g(G.iota(out=tokid[:], pattern=[[128, 32], [0, 16]], base=0, channel_multiplier=1))
            g(G.iota(out=jv_i[:], pattern=[[BLK, NBP]], base=0, channel_multiplier=0))
            g(G.iota(out=pid_i[:], pattern=[[0, NBP]], base=0, channel_multiplier=1))
            g(G.memset(zt[:], 0))
            s_z.dma(G.dma_start(out=SLOT.rearrange("(p f) c -> p (f c)", p=128), in_=zt[:]))
            W(V, s_g)
            v(V.tensor_copy(out=jv[:], in_=jv_i[:]))
            v(V.tensor_copy(out=pid[:], in_=pid_i[:]))
            v(V.tensor_tensor(out=destE[:], in0=cnt_run[:, :].unsqueeze(2).to_broadcast([128, 32, 32]),
                              in1=jv[:, 0:32].unsqueeze(1).to_broadcast([128, 32, 32]), op=ALU.is_gt))
            v(V.tensor_reduce(out=tmp32[:], in_=destE[:], axis=AX.X, op=ALU.add))
            v(V.tensor_scalar(out=padded[:], in0=tmp32[:], scalar1=float(BLK), scalar2=None, op0=ALU.mult))
            v(V.tensor_tensor_scan(out=pad_end[:], data0=ones_f[:, 0:32], data1=padded[:], initial=0.0, op0=ALU.mult,
                                   op1=ALU.add))
            v(V.tensor_tensor(out=pad_start[:], in0=pad_end[:], in1=padded[:], op=ALU.subtract))
            v(V.tensor_tensor(out=destE[:], in0=rank_all[:], in1=pad_start[:, :].unsqueeze(1).to_broadcast([128, 32, 32]),
                              op=ALU.add))
            for k in range(4):
                v(V.tensor_tensor(out=oh[:], in0=Lall[:], in1=M8all[:, :, k:k + 1].to_broadcast([128, 32, 32]),
                                  op=ALU.is_equal))
                v(V.tensor_tensor(out=oh[:], in0=oh[:], in1=destE[:], op=ALU.mult))
                v(V.tensor_reduce(out=dest4f[:, :, k], in_=oh[:], axis=AX.X, op=ALU.add))
            v(V.tensor_copy(out=dest4i[:], in_=dest4f[:, :, :].rearrange("p a b -> p (a b)")))
            vv = v(V.tensor_tensor(out=E4[:], in0=M8all[:, :, 0:4], in1=M8all[:, :, 0:1].to_broadcast([128, 32, 4]),
                                   op=ALU.subtract))
            W(A, s_v, vv)
            va = s_act.inc(A.activation(out=E4[:], in_=E4[:], func=AF.Exp))
            W(V, s_act, va)
            v(V.tensor_reduce(out=den[:], in_=E4[:], axis=AX.X, op=ALU.add))
            v(V.reciprocal(out=den[:], in_=den[:]))
            v(V.tensor_tensor(out=p4[:], in0=E4[:], in1=den[:, :].unsqueeze(2).to_broadcast([128, 32, 4]), op=ALU.mult))
            v(V.tensor_tensor(out=cmp[:], in0=pad_end[:, :].unsqueeze(1).to_broadcast([128, NBP, 32]),
                              in1=jv[:, :].unsqueeze(2).to_broadcast([128, NBP, 32]), op=ALU.is_le))
            v(V.tensor_reduce(out=be[:], in_=cmp[:], axis=AX.X, op=ALU.add))
            v(V.tensor_scalar(out=be[:], in0=be[:], scalar1=31.0, scalar2=None, op0=ALU.min))
            v(V.tensor_copy(out=bidx[:], in_=be[:]))
            v(V.scalar_tensor_tensor(out=be[:], in0=be[:], scalar=128.0, in1=pid[:], op0=ALU.mult, op1=ALU.add))
            v(V.tensor_scalar(out=jv[:], in0=jv[:], scalar1=pad_end[:, 31:32], scalar2=None, op0=ALU.is_lt))
            v(V.tensor_scalar(out=pid[:], in0=pid[:], scalar1=0.0, scalar2=None, op0=ALU.is_equal))
            v(V.tensor_tensor(out=jv[:], in0=jv[:], in1=pid[:], op=ALU.max))
            v(V.tensor_scalar(out=be[:], in0=be[:], scalar1=-100000.0, scalar2=None, op0=ALU.add))
            v(V.tensor_tensor(out=be[:], in0=be[:], in1=jv[:], op=ALU.mult))
            v(V.tensor_scalar(out=be[:], in0=be[:], scalar1=100000.0, scalar2=None, op0=ALU.add))
            v(V.tensor_copy(out=widx[:], in_=be[:]))
            W(G, s_v)
            W(G, s_z)
            for tt in range(NT):
                for k in range(4):
                    s_z.dma(G.indirect_dma_start(out=SLOT[:, :],
                                                 out_offset=bass.IndirectOffsetOnAxis(ap=dest4i[:, tt * 4 + k:tt * 4 + k + 1], axis=0),
                                                 in_=tokid[:, tt, :], in_offset=None))
            barrier([s_v, s_act, s_g, s_z])
            if dbg and stop == 4:
                for nm, t_, shp, dt_ in [("Lall", Lall, [128, 1024], F32), ("M8all", M8all, [128, 256], F32),
                                         ("dest4i", dest4i, [128, 128], I32), ("p4", p4, [128, 128], F32),
                                         ("widx", widx, [128, NBP], I32), ("bidx", bidx, [128, NBP], I32),
                                         ("cnt", cnt_run, [128, 32], F32)]:
                    dd = dbg_tensor(nm, shp, dt_)
                    ap_ = t_[:] if len(t_.shape) == 2 else t_[:, :, :].rearrange("p a b -> p (a b)")
                    s_out.dma(SY.dma_start(out=dd, in_=ap_))
                dd = dbg_tensor("slot", [NSLOT, 16], I32)
                s_out.dma(SY.dma_start(out=dd, in_=SLOT))
                dd = dbg_tensor("x1", [S, D])
                s_out.dma(SY.dma_start(out=dd, in_=X1))
                dd = dbg_tensor("h2", [S, D], BF16)
                s_out.dma(SY.dma_start(out=dd, in_=H2))
        if stop <= 4:
            W(SY, s_out)
            return nc, dbg_out

        with contextlib.ExitStack() as es:
            wbuf = [[RA[:, (par * 3 + m) * 8192:(par * 3 + m + 1) * 8192].rearrange("p (k n) -> p k n", k=8)
                     for m in range(3)] for par in range(2)]
            xb = [RB[:, par * 4096:par * 4096 + QB * 1024].rearrange("p (q d) -> p q d", q=QB) for par in range(2)]
            xbT = RB[:, 8192:8192 + 8 * BLK].rearrange("p (k s) -> p k s", k=8)
            actT0 = RB[:, 12288:12288 + 8 * BLK].rearrange("p (k s) -> p k s", k=8)
            actT1 = sb(es, "actT1", [128, 8, BLK], BF16)
            actT = [actT0, actT1]
            bgt = [sb(es, f"bgt{i}", [128, 8]) for i in range(2)]
            but = [sb(es, f"but{i}", [128, 8]) for i in range(2)]
            bdt = [sb(es, f"bdt{i}", [128, D]) for i in range(2)]
            tki = [sb(es, f"tki{i}", [128, QB, 16], I32) for i in range(2)]
            ysb = sb(es, "ysb", [128, QB, D])
            g_t = [sb(es, f"g_t{i}", [128, BLK]) for i in range(2)]
            sg_t = [sb(es, f"sg_t{i}", [128, BLK]) for i in range(2)]
            u_t = [sb(es, f"u_t{i}", [128, BLK]) for i in range(2)]
            ptx = [ps(es, f"ptx{i}", [128, QB, 128], BF16) for i in range(2)]
            pg = [ps(es, f"pg{i}", [128, BLK]) for i in range(2)]
            pu = [ps(es, f"pu{i}", [128, BLK]) for i in range(2)]
            py = [ps(es, f"py{i}", [128, 512]) for i in range(2)]
            s_tk = sig(es, "5tk", 2)
            s_xb = sig(es, "5xb", 2)
            s_wgu = sig(es, "5wgu", 2)
            s_wd = sig(es, "5wd", 2)
            s_pe = sig(es, "5t")
            s_v = sig(es, "5v")
            s_act = sig(es, "5a")
            s_yd = sig(es, "5y")
            blk = {j: {} for j in range(NBLK)}
            wsrc = (wg_d, wu_d, wd_d)
            bc_reg = G.to_reg(4095)

            def wgather(sg_, par, m, j):
                for c4 in range(4):
                    val = sg_.dma(G.indirect_dma_start(
                        out=RA[:, (par * 3 + m) * 8192 + c4 * 2048:(par * 3 + m) * 8192 + (c4 + 1) * 2048], out_offset=None,
                        in_=wsrc[m][0], in_offset=bass.IndirectOffsetOnAxis(ap=widx[:, j:j + 1], axis=0),
                        element_offset=c4 * 4096 * 2048, bounds_check=bc_reg, oob_is_err=False), lane=par)
                return val

            def loads_gu(j):
                par = j % 2
                st = blk[j]
                if j - 2 >= 0:
                    W(G, s_pe, blk[j - 2]["gu_done"])
                    W(G, s_v, blk[j - 2]["sw_done"])
                    W(G, s_act, blk[j - 2]["sw_act"])
                for q in range(QB):
                    vtk = s_tk.dma(G.dma_start(out=tki[par][:, q, :],
                                               in_=SLOT[j * BLK + q * 128:j * BLK + (q + 1) * 128, :]), lane=par)
                W(G, s_tk, vtk)
                for q in range(QB):
                    st["xb"] = s_xb.dma(G.indirect_dma_start(
                        out=xb[par][:, q, :], out_offset=None, in_=H2[:, :],
                        in_offset=bass.IndirectOffsetOnAxis(ap=tki[par][:, q, 0:1], axis=0)), lane=par)
                wgather(s_wgu, par, 0, j)
                wgather(s_wgu, par, 1, j)
                s_wgu.dma(G.indirect_dma_start(out=bgt[par][:], out_offset=None, in_=bg_d[:, :],
                                               in_offset=bass.IndirectOffsetOnAxis(ap=widx[:, j:j + 1], axis=0),
                                               bounds_check=bc_reg, oob_is_err=False), lane=par)
                st["wgu"] = s_wgu.dma(G.indirect_dma_start(out=but[par][:], out_offset=None, in_=bu_d[:, :],
                                                           in_offset=bass.IndirectOffsetOnAxis(ap=widx[:, j:j + 1], axis=0),
                                                           bounds_check=bc_reg, oob_is_err=False), lane=par)

            def loads_d(j):
                par = j % 2
                st = blk[j]
                if j - 2 >= 0:
                    W(G, s_pe, blk[j - 2]["d_done"])
                    W(G, s_v, blk[j - 2]["y_done"])
                wgather(s_wd, par, 2, j)
                st["wd"] = s_wd.dma(G.indirect_dma_start(out=bdt[par][:], out_offset=None, in_=bd_d[:, :],
                                                         in_offset=bass.IndirectOffsetOnAxis(ap=bidx[:, j:j + 1], axis=0)), lane=par)

            cnt5 = {"pgi": 0, "pyi": 0}
            pg_free = {}
            py_free = {}

            def TK(j, k):
                par = j % 2
                st = blk[j]
                if k == 0:
                    W(T, s_xb, st["xb"])
                    st["txe"] = {}
                ev_done = st["txe"]
                pb = ptx[k % 2]
                if k >= 2:
                    W(T, s_v, ev_done[k - 2])
                elif j > 0:
                    W(T, s_v, blk[j - 1]["txe"][6 + k])
                for q in range(QB):
                    ins = T.transpose(pb[:, q, :], xb[par][:, q, k * 128:(k + 1) * 128], ident_bf[:])
                vt = s_pe.inc(ins)
                W(V, s_pe, vt)
                ev_done[k] = s_v.inc(V.tensor_copy(out=xbT[:, k, :], in_=pb[:, :, :].rearrange("p q s -> p (q s)")))

            def GU(j):
                par = j % 2
                st = blk[j]
                ev_done = st["txe"]
                W(T, s_v, ev_done[7])
                W(T, s_wgu, st["wgu"])
                W(V, s_wgu, st["wgu"])
                W(A, s_wgu, st["wgu"])
                if j - 2 >= 0:
                    W(V, s_pe, blk[j - 2]["d_done"])
                at = actT[j % 2]
                for fc in range(8):
                    pgi = cnt5["pgi"]
                    gb = pgi % 2
                    if pgi >= 2:
                        W(T, s_v, pg_free[pgi - 2])
                    for k in range(8):
                        T.matmul(pg[gb][:], lhsT=wbuf[par][0][:, k, fc * 128:(fc + 1) * 128], rhs=xbT[:, k, :],
                                 start=(k == 0), stop=(k == 7))
                    for k in range(8):
                        ins = T.matmul(pu[gb][:], lhsT=wbuf[par][1][:, k, fc * 128:(fc + 1) * 128], rhs=xbT[:, k, :],
                                       start=(k == 0), stop=(k == 7))
                    vt = s_pe.inc(ins)
                    W(V, s_pe, vt)
                    W(A, s_pe, vt)
                    v1 = s_v.inc(V.tensor_scalar(out=g_t[gb][:], in0=pg[gb][:], scalar1=bgt[par][:, fc:fc + 1], scalar2=7.0,
                                                 op0=ALU.add, op1=ALU.min))
                    W(A, s_v, v1)
                    a1 = s_act.inc(A.activation(out=sg_t[gb][:], in_=g_t[gb][:], func=AF.Sigmoid, scale=1.702))
                    a2 = s_act.inc(A.activation(out=u_t[gb][:], in_=pu[gb][:], func=AF.Identity,
                                                bias=but[par][:, fc:fc + 1], scale=1.0))
                    W(V, s_act, a2)
                    v2 = s_v.inc(V.tensor_scalar(out=u_t[gb][:], in0=u_t[gb][:], scalar1=7.0, scalar2=-7.0, op0=ALU.min,
                                                 op1=ALU.max))
                    v3 = s_v.inc(V.tensor_tensor(out=g_t[gb][:], in0=g_t[gb][:], in1=sg_t[gb][:], op=ALU.mult))
                    W(V, s_v, v3)
                    v4 = s_v.inc(V.scalar_tensor_tensor(out=at[:, fc, :], in0=u_t[gb][:], scalar=1.0, in1=g_t[gb][:],
                                                        op0=ALU.add, op1=ALU.mult))
                    pg_free[pgi] = v4
                    cnt5["pgi"] += 1
                st["gu_done"] = vt
                st["sw_done"] = v4
                st["sw_act"] = a2

            def DN(j, jn=None):
                par = j % 2
                st = blk[j]
                at = actT[j % 2]
                W(T, s_v, st["sw_done"])
                W(T, s_wd, st["wd"])
                W(V, s_wd, st["wd"])
                if j - 1 >= 0:
                    W(V, s_yd, blk[j - 1]["yd"])
                for q in range(QB):
                    for dh in range(2):
                        pyi = cnt5["pyi"]
                        yb = pyi % 2
                        if pyi >= 2:
                            W(T, s_v, py_free[pyi - 2])
                        for fc in range(8):
                            ins = T.matmul(py[yb][:], lhsT=at[:, fc, q * 128:(q + 1) * 128],
                                           rhs=wbuf[par][2][:, fc, dh * 512:(dh + 1) * 512], start=(fc == 0), stop=(fc == 7))
                        vt = s_pe.inc(ins)
                        W(V, s_pe, vt)
                        py_free[pyi] = s_v.inc(V.tensor_tensor(out=ysb[:, q, dh * 512:(dh + 1) * 512], in0=py[yb][:],
                                                               in1=bdt[par][:, dh * 512:(dh + 1) * 512], op=ALU.add))
                        cnt5["pyi"] += 1
                        if jn is not None:
                            TK(jn, q * 2 + dh)
                if jn is not None:
                    for k in range(2 * QB, 8):
                        TK(jn, k)
                st["d_done"] = vt
                st["y_done"] = py_free[cnt5["pyi"] - 1]
                W(SY, s_v, st["y_done"])
                st["yd"] = s_yd.dma(SY.dma_start(out=Yd[j * BLK:(j + 1) * BLK, :].rearrange("(q p) d -> p q d", p=128),
                                                 in_=ysb[:]))

            loads_gu(0)
            loads_d(0)
            loads_gu(1)
            loads_d(1)
            for k in range(8):
                TK(0, k)
            for j in range(NBLK + 1):
                if j < NBLK:
                    GU(j)
                    if j + 2 < NBLK:
                        loads_gu(j + 2)
                if j >= 1:
                    DN(j - 1, j + 1 if j + 1 < NBLK else None)
                    if j + 1 < NBLK:
                        loads_d(j + 1)
                elif NBLK > 1:
                    for k in range(8):
                        TK(1, k)
            barrier([s_pe, s_v, s_act, s_yd, s_wgu, s_wd, s_xb, s_tk])

        if stop <= 5:
            if dbg:
                dd = dbg_tensor("Y", [NSLOT, D])
                s_out.dma(SY.dma_start(out=dd, in_=Yd))
            W(SY, s_out)
            return nc, dbg_out

        with contextlib.ExitStack() as es:
            RAf = RA[:, :].bitcast(F32)

            class _V:
                def __init__(self, ap): self.ap = ap
                def __getitem__(self, idx): return self.ap

            def fv(i):
                return _V(RAf[:, i * D:(i + 1) * D])
            yg = [[fv(i * 4 + k) for k in range(4)] for i in range(3)]
            x1t = [fv(12 + i) for i in range(3)]
            ot = [fv(17 + i) for i in range(2)]
            ss6 = sb(es, "ss6", [128, 32])
            rs6 = sb(es, "rs6", [128, 32])
            s_g = sig(es, "6g", 3)
            s_d = sig(es, "6d", 5)
            s_v = sig(es, "6v")
            s_act = sig(es, "6a")
            hist = {}

            def v(ins):
                val = s_v.inc(ins); W(V, s_v, val); return val

            def a(ins):
                val = s_act.inc(ins); W(A, s_act, val); return val

            def loads(tt):
                b = tt % 3
                if tt - 3 >= 0:
                    W(G, s_v, hist[tt - 3]["acc"])
                    W(SY, s_v, hist[tt - 3]["acc"])
                for k in range(4):
                    vg = s_g.dma(G.indirect_dma_start(out=yg[b][k][:], out_offset=None, in_=Yd[:, :],
                                                      in_offset=bass.IndirectOffsetOnAxis(
                                                          ap=dest4i[:, tt * 4 + k:tt * 4 + k + 1], axis=0)), lane=b)
                vd = s_d.dma(SY.dma_start(out=x1t[b][:], in_=X1[tt * 128:(tt + 1) * 128, :]), lane=b)
                hist[tt] = {"g": vg, "d": vd}

            accs = [fv(15), fv(16)]

            def comb(tt):
                b = tt % 2
                b3 = tt % 3
                ac = accs[b]
                W(V, s_g, hist[tt]["g"])
                W(V, s_d, hist[tt]["d"])
                v(V.tensor_scalar(out=ac[:], in0=yg[b3][0][:], scalar1=p4[:, tt, 0:1], scalar2=None, op0=ALU.mult))
                for k in range(1, 4):
                    v(V.scalar_tensor_tensor(out=ac[:], in0=yg[b3][k][:], scalar=p4[:, tt, k:k + 1], in1=ac[:],
                                             op0=ALU.mult, op1=ALU.add))
                v(V.tensor_tensor(out=ac[:], in0=ac[:], in1=gf_row, op=ALU.mult))
                vacc = v(V.tensor_tensor(out=ac[:], in0=ac[:], in1=x1t[b3][:], op=ALU.add))
                hist[tt]["acc"] = vacc
                W(A, s_v, vacc)
                if tt - 2 >= 0:
                    W(A, s_d, hist[tt - 2]["od"])
                va = s_act.inc(A.activation(out=ot[b][:], in_=ac[:], func=AF.Square, accum_out=ss6[:, tt:tt + 1]))
                hist[tt]["sq"] = va

            def fin(tt):
                b = tt % 2
                ac = accs[b]
                W(V, s_act, hist[tt]["sd"])
                v(V.reciprocal(out=rs6[:, tt:tt + 1], in_=ss6[:, tt:tt + 1]))
                vo = v(V.scalar_tensor_tensor(out=ot[b][:], in0=ac[:], scalar=rs6[:, tt:tt + 1], in1=fing_row[:], op0=ALU.mult,
                                              op1=ALU.mult))
                hist[tt]["fin"] = vo
                W(SY, s_v, vo)
                hist[tt]["od"] = s_d.dma(SY.dma_start(out=out_d[tt * 128:(tt + 1) * 128, :], in_=ot[b][:]), lane=3 + b)

            def sqrt_(tt):
                W(A, s_act, hist[tt]["sq"])
                hist[tt]["sd"] = s_act.inc(A.activation(out=ss6[:, tt:tt + 1], in_=ss6[:, tt:tt + 1], func=AF.Sqrt,
                                                        scale=1.0 / D, bias=EPS))

            loads(0)
            loads(1)
            loads(2)
            for tt in range(NT + 1):
                if tt < NT:
                    if tt - 2 >= 0:
                        W(V, s_v, hist[tt - 2]["fin"])
                    comb(tt)
                    sqrt_(tt)
                if tt - 1 >= 0:
                    fin(tt - 1)
                    if tt + 2 < NT:
                        loads(tt + 2)
            barrier([s_d, s_v, s_act, s_g])
        W(SY, s_out)
    return nc, dbg_out


def _layout_w(w):
    w5 = w.reshape(32, 4, 2, 128, 1024)
    return np.ascontiguousarray(w5.transpose(1, 0, 3, 2, 4)).reshape(4, 4096, 2048)


_SHARED_CACHE = {}


def make_in_maps(inp, ncores=8):
    f = lambda a: np.ascontiguousarray(a, dtype=np.float32)
    sh = {
        "ada_w": f(inp["ada_w"][0]),
        "ada_b_row": f(inp["ada_b"][0].reshape(1, -1)),
        "ada_b_col": f(inp["ada_b"][0].reshape(48, 128).T),
        "mixg_col": f(inp["mix_norm_g"][0].reshape(8, 128).T),
        "w_in": f(inp["w_in"][0]),
        "convw_col": f(inp["conv_w"][0].T.reshape(4, 128, 4).transpose(1, 0, 2).reshape(128, 16)),
        "lru_cols": f(np.stack([inp["conv_b"][0], inp["rg_b"][0], inp["ig_b"][0], inp["lru_lambda"][0]], axis=-1)
                      .reshape(4, 128, 4).transpose(1, 0, 2).reshape(128, 16)),
        "rg_w": f(inp["rg_w"][0]),
        "ig_w": f(inp["ig_w"][0]),
        "outg_col": f(np.concatenate([inp["attn_out_g"][0], inp["lru_out_g"][0]]).reshape(8, 128).T),
        "w_out": f(inp["w_out"][0]),
        "ffn_g_row": f(inp["ffn_norm_g"][0].reshape(1, -1)),
        "final_g_row": f(inp["final_norm_g"].reshape(1, -1)),
        "router_w": f(inp["router_w"][0]),
        "router_b_row": f(inp["router_b"][0].reshape(1, -1)),
        "wg": _layout_w(f(inp["exp_w_gate"][0])),
        "wu": _layout_w(f(inp["exp_w_up"][0])),
        "wd": _layout_w(f(inp["exp_w_down"][0])),
        "bg": f(inp["exp_b_gate"][0].reshape(32, 8, 128).transpose(0, 2, 1).reshape(4096, 8)),
        "bu": f(inp["exp_b_up"][0].reshape(32, 8, 128).transpose(0, 2, 1).reshape(4096, 8)),
        "bd": f(inp["exp_b_down"][0]),
    }
    maps = []
    for b in range(ncores):
        m = dict(sh)
        m["x"] = f(inp["x"][b])
        m["c_col"] = f(inp["c"][b].reshape(8, 128).T)
        maps.append(m)
    return maps


def kernel(**inputs):
    nc, _ = build_program()
    maps = make_in_maps(inputs, 8)
    res = run_bass_kernel_spmd(nc, maps, core_ids=list(range(8)))
    return np.stack([np.asarray(r["out"], dtype=np.float32) for r in res.results], axis=0)
```

```python
import contextlib
import numpy as np
import concourse.bass as bass
import concourse.mybir as mybir
from concourse.bass_utils import run_bass_kernel_spmd

F32 = mybir.dt.float32
BF16 = mybir.dt.bfloat16
I32 = mybir.dt.int32
ALU = mybir.AluOpType
AF = mybir.ActivationFunctionType
AX = mybir.AxisListType

S = 4096
D = 1024
NT = S // 128
EPS = 1e-6
BLK = 512
QB = BLK // 128
NBLK = 32 + (S * 4 - 32) // BLK
NBP = 80
NSLOT = NBLK * BLK


class Sig:
    def __init__(self, nc, es, name, lanes=1):
        self.sems = [es.enter_context(nc.semaphore(f"{name}_l{i}")) for i in range(lanes)]
        self.cnt = [0] * lanes
        self.lanes = lanes

    @property
    def sem(self):
        return self.sems[0]

    @property
    def n(self):
        return self.cnt[0]

    def inc(self, ins, k=1, lane=0):
        ins.then_inc(self.sems[lane], k)
        self.cnt[lane] += k
        return self.cnt[lane] if self.lanes == 1 else (lane, self.cnt[lane])

    def dma(self, ins, lane=0):
        return self.inc(ins, 16, lane)


def W(eng, sig, val=None):
    if val is None:
        for l in range(sig.lanes):
            if sig.cnt[l] > 0:
                eng.wait_ge(sig.sems[l], sig.cnt[l])
        return
    if isinstance(val, tuple):
        lane, v = val
    else:
        lane, v = 0, val
    if v > 0:
        eng.wait_ge(sig.sems[lane], v)


def build_program(stop=99, dbg=None):
    nc = bass.Bass("TRN2", target_bir_lowering=False)
    dbg_out = {}

    def din(name, shape, dt=F32):
        return nc.dram_tensor(name, list(shape), dt, kind="ExternalInput").ap()

    x_d = din("x", [S, D])
    ccol_d = din("c_col", [128, 8])
    adaw_d = din("ada_w", [D, 6 * D])
    adab_row_d = din("ada_b_row", [1, 6 * D])
    adab_col_d = din("ada_b_col", [128, 48])
    mixg_d = din("mixg_col", [128, 8])
    win_d = din("w_in", [D, 2560])
    convw_d = din("convw_col", [128, 16])
    lruv_d = din("lru_cols", [128, 16])
    rgw_d = din("rg_w", [8, 64, 64])
    igw_d = din("ig_w", [8, 64, 64])
    outg_d = din("outg_col", [128, 8])
    wout_d = din("w_out", [D, D])
    ffng_d = din("ffn_g_row", [1, D])
    fing_d = din("final_g_row", [1, D])
    rw_d = din("router_w", [D, 32])
    rb_d = din("router_b_row", [1, 32])
    wg_d = din("wg", [4, 4096, 2048])
    wu_d = din("wu", [4, 4096, 2048])
    wd_d = din("wd", [4, 4096, 2048])
    bg_d = din("bg", [4096, 8])
    bu_d = din("bu", [4096, 8])
    bd_d = din("bd", [32, D])
    out_d = nc.dram_tensor("out", [S, D], F32, kind="ExternalOutput").ap()
    X1 = nc.dram_tensor("X1s", [S, D], F32, kind="Internal").ap()
    H2 = nc.dram_tensor("H2s", [S, D], BF16, kind="Internal").ap()
    Yd = nc.dram_tensor("Ys", [NSLOT, D], F32, kind="Internal").ap()
    SLOT = nc.dram_tensor("SLOTs", [NSLOT, 16], I32, kind="Internal").ap()

    def dbg_tensor(name, shape, dt=F32):
        t = nc.dram_tensor("dbg_" + name, list(shape), dt, kind="ExternalOutput").ap()
        dbg_out[name] = t
        return t

    V, A, G, T, SY = nc.vector, nc.scalar, nc.gpsimd, nc.tensor, nc.sync

    with contextlib.ExitStack() as top:
        def sb(es, name, shape, dt=F32):
            return es.enter_context(nc.sbuf_tensor(name, list(shape), dt))

        def ps(es, name, shape, dt=F32):
            return es.enter_context(nc.psum_tensor(name, list(shape), dt))

        sig_i = [0]

        def sig(es, name, lanes=1):
            sig_i[0] += 1
            return Sig(nc, es, f"{name}_{sig_i[0]}", lanes)

        s_out = sig(top, "out")

        ident_bf = sb(top, "ident_bf", [128, 128], BF16)
        ident_f = sb(top, "ident_f", [128, 128], F32)
        ones_f = sb(top, "ones_f", [128, 128], F32)
        ones_bf = sb(top, "ones_bf", [128, 128], BF16)
        negones_bf = sb(top, "negones_bf", [128, 128], BF16)
        negU_bf = sb(top, "negU_bf", [128, 128], BF16)
        tri_bf = sb(top, "tri_bf", [128, 128], BF16)
        zeros_bf = sb(top, "zeros_bf", [128, 512], BF16)
        s_c = sig(top, "const")
        s_cv = sig(top, "constv")
        s_cv.inc(V.memset(ones_f[:], 1.0))
        s_cv.inc(V.memset(ones_bf[:], 1.0))
        s_cv.inc(V.memset(negones_bf[:], -1.0))
        s_cv.inc(V.memset(zeros_bf[:], 0.0))
        W(G, s_cv)
        s_c.inc(G.affine_select(out=ident_f[:], in_=ones_f[:], pattern=[[-1, 128]], compare_op=ALU.is_equal,
                                fill=0.0, base=0, channel_multiplier=1))
        s_c.inc(G.affine_select(out=ident_bf[:], in_=ones_bf[:], pattern=[[-1, 128]], compare_op=ALU.is_equal,
                                fill=0.0, base=0, channel_multiplier=1))
        s_c.inc(G.affine_select(out=negU_bf[:], in_=negones_bf[:], pattern=[[-1, 128]], compare_op=ALU.is_ge,
                                fill=0.0, base=0, channel_multiplier=1))
        s_c.inc(G.affine_select(out=tri_bf[:], in_=ones_bf[:], pattern=[[1, 128]], compare_op=ALU.is_gt,
                                fill=0.0, base=0, channel_multiplier=-1))
        for e in (V, A, T, SY):
            W(e, s_c)
        W(G, s_c)

        modrow = sb(top, "modrow", [128, 4 * D])
        modcol = sb(top, "modcol", [128, 16])
        Am = sb(top, "Am", [128, 8])
        fing_row = sb(top, "fing_row", [128, D])
        gm_row = modrow[:, 0:D]
        shf_row = modrow[:, D:2 * D]
        Af_row = modrow[:, 2 * D:3 * D]
        gf_row = modrow[:, 3 * D:4 * D]
        Bm = modcol[:, 0:8]

        ssall = sb(top, "ssall", [128, 32])
        rstdall = sb(top, "rstdall", [128, 32])
        with contextlib.ExitStack() as es:
            xs = [sb(es, f"xs{i}", [128, D]) for i in range(4)]
            junk = sb(es, "junk0", [128, D], BF16)
            s_x = sig(es, "0x", 4)
            s_a = sig(es, "0a")
            s_v0 = sig(es, "0v")
            vxs = {}
            vas = {}
            for tt in range(NT):
                if tt >= 4:
                    W(SY, s_a, vas[tt - 4])
                vxs[tt] = s_x.dma(SY.dma_start(out=xs[tt % 4][:], in_=x_d[tt * 128:(tt + 1) * 128, :]), lane=tt % 4)
                W(A, s_x, vxs[tt])
                vas[tt] = s_a.inc(A.activation(out=junk[:], in_=xs[tt % 4][:], func=AF.Square, accum_out=ssall[:, tt:tt + 1]))
            W(A, s_a)
            va = s_a.inc(A.activation(out=ssall[:], in_=ssall[:], func=AF.Sqrt, scale=1.0 / D, bias=EPS))
            W(V, s_a, va)
            s_v0.inc(V.reciprocal(out=rstdall[:], in_=ssall[:]))
            barrier_early = [s_x, s_a, s_v0]
            for e in (V, A, T, SY, G):
                for s_ in barrier_early:
                    W(e, s_)

        with contextlib.ExitStack() as es:
            ccol = sb(es, "ccol", [128, 8])
            scol = sb(es, "scol", [128, 8])
            scb = sb(es, "scb", [128, 8, 128], BF16)
            scol_b = sb(es, "scol_b", [128, 8], BF16)
            adab_col = sb(es, "adab_col", [128, 48])
            mixg = sb(es, "mixg", [128, 8])
            ffng_row = sb(es, "ffng_row", [128, D])
            adab_row = sb(es, "adab_row", [128, 512])
            aw = [sb(es, f"aw{i}", [128, 8, 512], BF16) for i in range(2)]
            pr = [ps(es, f"p0r{i}", [128, 512]) for i in range(2)]
            pc = ps(es, "p0c", [128, 16])
            s_ld = sig(es, "p0ld")
            s_aw = sig(es, "p0aw", 2)
            s_awfree = sig(es, "p0awf")
            s_a = sig(es, "p0a")
            s_v = sig(es, "p0v")
            s_t = sig(es, "p0t")
            s_b = sig(es, "p0b")
            s_ld.dma(SY.dma_start(out=ccol[:], in_=ccol_d))
            s_ld.dma(SY.dma_start(out=adab_col[:], in_=adab_col_d))
            s_ld.dma(SY.dma_start(out=mixg[:], in_=mixg_d))
            s_ld.dma(SY.dma_start(out=ffng_row[:], in_=ffng_d.broadcast_to([128, D])))
            s_ld.dma(SY.dma_start(out=fing_row[:], in_=fing_d.broadcast_to([128, D])))
            W(A, s_ld)
            s_a.inc(A.activation(out=scol[:], in_=ccol[:], func=AF.Silu))
            W(V, s_a)
            W(V, s_ld)
            for k in range(8):
                s_v.inc(V.tensor_scalar(out=scb[:, k, :], in0=ones_f[:], scalar1=scol[:, k:k + 1], scalar2=None,
                                        op0=ALU.mult))
            s_v.inc(V.tensor_copy(out=scol_b[:], in_=scol[:]))
            aw_view = adaw_d.rearrange("(k p) n -> p k n", p=128)
            W(T, s_v)
            tdone = {}
            vev = {}
            for n in range(12):
                b = n % 2
                if n >= 2:
                    W(G, s_t, tdone[n - 2])
                v_aw = s_aw.dma(G.dma_start(out=aw[b][:], in_=aw_view[:, :, n * 512:(n + 1) * 512]), lane=b)
                W(T, s_aw, v_aw)
                if n < 4:
                    for fcl in range(4):
                        fc = n * 4 + fcl
                        for k in range(8):
                            ins = T.matmul(pc[:, fc:fc + 1], lhsT=aw[b][:, k, fcl * 128:(fcl + 1) * 128],
                                           rhs=scol_b[:, k:k + 1], start=(k == 0), stop=(k == 7))
                    tdone[n] = s_t.inc(ins)
                else:
                    if n - 2 >= 4:
                        W(T, s_v, vev[n - 2])
                    for k in range(8):
                        ins = T.matmul(pr[b][:], lhsT=scb[:, k, :], rhs=aw[b][:, k, :], start=(k == 0), stop=(k == 7))
                    tdone[n] = s_t.inc(ins)
                    if n - 1 >= 4:
                        W(SY, s_v, vev[n - 1])
                    v_b = s_b.dma(SY.dma_start(out=adab_row[:], in_=adab_row_d[:, n * 512:(n + 1) * 512]
                                               .broadcast_to([128, 512])))
                    W(V, s_b, v_b)
                    W(V, s_t, tdone[n])
                    vev[n] = s_v.inc(V.tensor_tensor(out=modrow[:, (n - 4) * 512:(n - 3) * 512], in0=pr[b][:],
                                                     in1=adab_row[:], op=ALU.add))
            W(V, s_t)
            s_v.inc(V.tensor_tensor(out=modcol[:], in0=pc[:], in1=adab_col[:, 0:16], op=ALU.add))
            W(V, s_v)
            s_v.inc(V.scalar_tensor_tensor(out=Am[:], in0=modcol[:, 8:16], scalar=1.0, in1=mixg[:], op0=ALU.add,
                                           op1=ALU.mult))
            s_v.inc(V.scalar_tensor_tensor(out=modrow[:, 2 * D:3 * D], in0=modrow[:, 2 * D:3 * D], scalar=1.0,
                                           in1=ffng_row[:], op0=ALU.add, op1=ALU.mult))
            for e in (A, T, SY, G):
                W(e, s_v)
            W(V, s_v)
            if dbg:
                d1 = dbg_tensor("modrow", [128, 4 * D])
                d2 = dbg_tensor("modcol", [128, 16])
                s_out.dma(SY.dma_start(out=d1, in_=modrow[:]))
                s_out.dma(SY.dma_start(out=d2, in_=modcol[:]))

        if stop <= 0:
            W(SY, s_out)
            return nc, dbg_out

        RA = sb(top, "RA", [128, 49152], BF16)
        RB = sb(top, "RB", [128, 16384], BF16)
        qT = RA[:, 0:16384].rearrange("p (c t) -> p c t", c=4)
        kT = RA[:, 16384:32768].rearrange("p (c t) -> p c t", c=4)
        vtok = RA[:, 32768:49152].rearrange("p (n f) -> p n f", n=32)
        yattnT = RB[:, :].rearrange("p (c t) -> p c t", c=4)
        cnt = {"tile": 0}

        def emit_norm_transpose(es, tiles, hT_of, xn_bufs, pT, xt, sg, hist):
            s_x, s_act, s_v, s_pe = sg
            st = hist
            base = len(st)

            def stage1(jl, tt):
                j = base + jl
                b = j % 2
                if j - 2 in st:
                    W(SY, s_v, st[j - 2]["xn"])
                vx = s_x.dma(SY.dma_start(out=xt[b][:], in_=x_d[tt * 128:(tt + 1) * 128, :]), lane=b)
                W(V, s_x, vx)
                if j - 2 in st:
                    W(V, s_pe, st[j - 2]["tr"])
                v3 = s_v.inc(V.tensor_scalar(out=xn_bufs[b], in0=xt[b][:], scalar1=rstdall[:, tt:tt + 1], scalar2=None,
                                             op0=ALU.mult))
                W(T, s_v, v3)
                if j - 2 in st:
                    W(T, s_v, st[j - 2]["ev"])
                for k in range(8):
                    ins = T.transpose(pT[b][:, k, :], xn_bufs[b][:, k * 128:(k + 1) * 128], ident_bf[:])
                st[j] = {"xn": v3, "tr": s_pe.inc(ins)}

            def stage2(jl):
                j = base + jl
                b = j % 2
                W(V, s_pe, st[j]["tr"])
                dst = hT_of(jl)
                for k in range(8):
                    ins = V.tensor_scalar(out=dst[:, k, :], in0=pT[b][:, k, :], scalar1=Am[:, k:k + 1],
                                          scalar2=Bm[:, k:k + 1], op0=ALU.mult, op1=ALU.add)
                st[j]["ev"] = s_v.inc(ins)

            for jl, tt in enumerate(tiles):
                stage1(jl, tt)
                if jl >= 1:
                    stage2(jl - 1)
            stage2(len(tiles) - 1)
            return st[base + len(tiles) - 1]["ev"]

        with contextlib.ExitStack() as es:
            win = RB[:, 0:12288].rearrange("p (k n) -> p k n", k=8)
            xn_bufs = [RB[:, 12288 + i * 1024:12288 + (i + 1) * 1024] for i in range(2)]
            hT = [sb(es, f"hT{i}", [128, 8, 1024], BF16) for i in range(2)]
            xt = [sb(es, f"xt{i}", [128, D]) for i in range(2)]
            pT = [ps(es, f"pT{i}", [128, 8, 128], BF16) for i in range(2)]
            pj = [ps(es, f"pj{i}", [128, 512]) for i in range(4)]
            s_w = sig(es, "p1w")
            sg = (sig(es, "p1x", 2), sig(es, "p1a"), sig(es, "p1v"), sig(es, "p1t"))
            s_x, s_act, s_v, s_pe = sg
            s_ea = sig(es, "p1ea")
            s_ev = sig(es, "p1ev")
            wv = win_d.rearrange("(k p) n -> p k n", p=128)
            for kh in range(2):
                s_w.dma(G.dma_start(out=win[:, kh * 4:(kh + 1) * 4, :], in_=wv[:, kh * 4:(kh + 1) * 4, 0:1536]))
            W(T, s_w)
            gi = 0
            ev_hist = {}
            hist1 = {}
            hT_last_pe = {}
            for q in range(4):
                hq = hT[q % 2]
                if q >= 2:
                    W(V, s_pe, hT_last_pe[q - 2])
                v_h = emit_norm_transpose(es, list(range(q * 8, q * 8 + 8)),
                                          lambda j, hq=hq: hq[:, :, j * 128:(j + 1) * 128], xn_bufs, pT, xt, sg, hist1)
                W(T, s_v, v_h)
                groups = [("qk", ec, th) for ec in range(8) for th in range(2)] + [("v", tt, 0) for tt in range(8)]
                for (kind, a_, b_) in groups:
                    pb = pj[gi % 4]
                    if gi >= 4:
                        sg_, val_ = ev_hist[gi - 4]
                        W(T, sg_, val_)
                    for k in range(8):
                        if kind == "qk":
                            ins = T.matmul(pb[:], lhsT=win[:, k, a_ * 128:(a_ + 1) * 128],
                                           rhs=hq[:, k, b_ * 512:(b_ + 1) * 512], start=(k == 0), stop=(k == 7))
                        else:
                            ins = T.matmul(pb[:], lhsT=hq[:, k, a_ * 128:(a_ + 1) * 128], rhs=win[:, k, 1024:1536],
                                           start=(k == 0), stop=(k == 7))
                    vt = s_pe.inc(ins)
                    hT_last_pe[q] = vt
                    if kind == "qk":
                        tok0 = q * 1024 + b_ * 512
                        if a_ < 4:
                            dst = qT[:, a_, tok0:tok0 + 512]
                            scale = 0.125
                        else:
                            dst = kT[:, a_ - 4, tok0:tok0 + 512]
                            scale = 1.0
                    else:
                        dst = vtok[:, q * 8 + a_, :]
                        scale = 1.0
                    if gi % 2 == 0:
                        W(A, s_pe, vt)
                        ev_hist[gi] = (s_ea, s_ea.inc(A.activation(out=dst, in_=pb[:], func=AF.Copy, scale=scale)))
                    else:
                        W(V, s_pe, vt)
                        ev_hist[gi] = (s_ev, s_ev.inc(V.tensor_scalar(out=dst, in0=pb[:], scalar1=scale, scalar2=None,
                                                                      op0=ALU.mult)))
                    gi += 1
            for e in (A, V, T, G, SY):
                W(e, s_ea)
                W(e, s_ev)
                W(e, s_pe)
            if dbg and stop == 1:
                dq = dbg_tensor("qkv", [128, 49152], BF16)
                s_out.dma(SY.dma_start(out=dq, in_=RA[:, :]))
                d2 = dbg_tensor("rstd", [128, 32])
                s_out.dma(SY.dma_start(out=d2, in_=rstdall[:, :]))
                d3 = dbg_tensor("hT1", [128, 8192], BF16)
                s_out.dma(SY.dma_start(out=d3, in_=hT[1][:, :, :]))
                d4 = dbg_tensor("Am", [128, 8])
                s_out.dma(SY.dma_start(out=d4, in_=Am[:, :]))
        if stop <= 1:
            W(SY, s_out)
            return nc, dbg_out

        with contextlib.ExitStack() as es:
            e_t = [sb(es, f"e_t{i}", [128, 512]) for i in range(3)]
            S_t = [sb(es, f"S_t{i}", [128, 512], BF16) for i in range(3)]
            w_t = [sb(es, f"w_t{i}", [128, 512], BF16) for i in range(2)]
            R_t = [sb(es, f"R_t{i}", [128, 512], BF16) for i in range(2)]
            mask = [sb(es, f"mask{i}", [128, 512], BF16) for i in range(4)]
            pz = [ps(es, f"pz{i}", [128, 512]) for i in range(2)]
            pw = [ps(es, f"pw{i}", [128, 512]) for i in range(3)]
            po = [ps(es, f"po{i}", [128, 512]) for i in range(2)]
            s_pe = sig(es, "a_pe")
            s_act = sig(es, "a_act")
            s_pool = sig(es, "a_pool")
            s_v = sig(es, "a_v")
            for rel in range(4):
                s_pool.inc(G.affine_select(out=mask[rel][:], in_=zeros_bf[:], pattern=[[1, 512]], compare_op=ALU.is_gt,
                                           fill=-30000.0, base=-128 * rel, channel_multiplier=-1))
            W(T, s_pool)
            pairs = []
            groups = []
            for h in range(8):
                for c in range(8):
                    g = len(groups)
                    kbs = list(range(4 * c + 3, -1, -1))
                    groups.append((h, c, len(pairs), len(pairs) + len(kbs) - 1))
                    for kb in kbs:
                        pairs.append((h, c, kb, g))
            n = len(pairs)
            vA, vB, vC, vD, vE, vF, vG = {}, {}, {}, {}, {}, {}, {}
            gev = {}

            def ops(j):
                h, c, kb, g = pairs[j]
                hp = (h % 2) * 64
                kh = kT[hp:hp + 64, h // 2, kb * 128:(kb + 1) * 128]
                qh = qT[hp:hp + 64, h // 2, c * 512:(c + 1) * 512]
                rel = kb - 4 * c
                first = (j == groups[g][2])
                last = (j == groups[g][3])
                return h, c, kb, g, hp, kh, qh, rel, first, last

            def colsl(j):
                rel = pairs[j][2] - 4 * pairs[j][1]
                return slice(128 * rel, 512) if rel >= 1 else slice(0, 512)

            def zmm(out, j, stop_):
                h, c, kb, g, hp, kh, qh, rel, first, last = ops(j)
                cs = colsl(j)
                if rel >= 0:
                    T.matmul(out[:, cs], lhsT=kh, rhs=qh[:, cs], start=True, stop=False)
                    return T.matmul(out[:, cs], lhsT=ident_bf[:], rhs=mask[rel][:, cs], start=False, stop=stop_)
                return T.matmul(out[:, cs], lhsT=kh, rhs=qh[:, cs], start=True, stop=stop_)

            def st_A(j):
                if j - 2 >= 0:
                    W(T, s_act, vB[j - 2])
                vA[j] = s_pe.inc(zmm(pz[j % 2], j, True))

            def st_B(j):
                W(A, s_pe, vA[j])
                cs = colsl(j)
                vB[j] = s_act.inc(A.activation(out=e_t[j % 3][:, cs], in_=pz[j % 2][:, cs], func=AF.Exp))

            def st_C(j):
                W(A, s_act, vB[j])
                if j - 3 >= 0:
                    W(A, s_pe, vD[j - 3])
                    if (j - 3) in vE:
                        W(A, s_pool, vE[j - 3])
                cs = colsl(j)
                vC[j] = s_act.inc(A.activation(out=S_t[j % 3][:, cs], in_=e_t[j % 3][:, cs], func=AF.Ln, bias=1.0, scale=1.0))

            def st_E(j):
                h, c, kb, g, hp, kh, qh, rel, first, last = ops(j)
                if last:
                    return
                cs = colsl(j)
                W(G, s_act, vC[j])
                if j - 1 >= 0:
                    W(G, s_pe, vD[j - 1])
                Rn = R_t[(j + 1) % 2]
                if cs.start > 0:
                    s_pool.inc(G.memset(Rn[:, 0:cs.start], 0.0))
                if first:
                    vE[j] = s_pool.inc(G.tensor_copy(out=Rn[:, cs], in_=S_t[j % 3][:, cs]))
                else:
                    W(G, s_pool, vE[j - 1])
                    vE[j] = s_pool.inc(G.tensor_tensor(out=Rn[:, cs], in0=R_t[j % 2][:, cs], in1=S_t[j % 3][:, cs],
                                                       op=ALU.add))

            def st_D(j):
                h, c, kb, g, hp, kh, qh, rel, first, last = ops(j)
                W(T, s_act, vC[j])
                if j - 3 >= 0:
                    W(T, s_act, vF[j - 3])
                if not first:
                    W(T, s_pool, vE[j - 1])
                cs = colsl(j)
                zmm(pw[j % 3], j, False)
                ins = T.matmul(pw[j % 3][:, cs], lhsT=negU_bf[:], rhs=S_t[j % 3][:, cs], start=False, stop=first)
                if not first:
                    ins = T.matmul(pw[j % 3][:, cs], lhsT=negones_bf[:], rhs=R_t[j % 2][:, cs], start=False, stop=True)
                vD[j] = s_pe.inc(ins)

            def st_F(j):
                W(A, s_pe, vD[j])
                if j - 2 >= 0:
                    W(A, s_pe, vG[j - 2])
                cs = colsl(j)
                vF[j] = s_act.inc(A.activation(out=w_t[j % 2][:, cs], in_=pw[j % 3][:, cs], func=AF.Exp))

            def st_G(j):
                h, c, kb, g, hp, kh, qh, rel, first, last = ops(j)
                W(T, s_act, vF[j])
                if first and g - 2 >= 0:
                    W(T, s_v, gev[g - 2])
                cs = colsl(j)
                if first:
                    T.matmul(po[g % 2][hp:hp + 64, :], lhsT=zeros_bf[:, 0:64], rhs=mask[0][:], start=True, stop=False)
                ins = T.matmul(po[g % 2][hp:hp + 64, cs], lhsT=vtok[:, kb, h * 64:(h + 1) * 64], rhs=w_t[j % 2][:, cs],
                               start=False, stop=last)
                vG[j] = s_pe.inc(ins)
                if last:
                    W(V, s_pe, vG[j])
                    gev[g] = s_v.inc(V.tensor_copy(out=yattnT[hp:hp + 64, h // 2, c * 512:(c + 1) * 512],
                                                   in_=po[g % 2][hp:hp + 64, :]))

            for i in range(-3, n + 2):
                if 0 <= i + 3 < n:
                    st_A(i + 3)
                if 0 <= i + 2 < n:
                    st_B(i + 2)
                if 0 <= i + 1 < n:
                    st_C(i + 1)
                if 0 <= i < n:
                    st_D(i)
                if 0 <= i + 1 < n:
                    st_E(i + 1)
                if 0 <= i - 1 < n:
                    st_F(i - 1)
                if 0 <= i - 2 < n:
                    st_G(i - 2)
            for e in (A, V, T, G, SY):
                W(e, s_v)
                W(e, s_pe)
                W(e, s_act)
                W(e, s_pool)
            if dbg and stop == 2:
                dq = dbg_tensor("yattn", [128, 16384], BF16)
                s_out.dma(SY.dma_start(out=dq, in_=RB[:, :]))
        if stop <= 2:
            W(SY, s_out)
            return nc, dbg_out

        ENG = (V, A, G, T, SY)

        def barrier(sigs, engines=ENG):
            for e in engines:
                for s_ in sigs:
                    W(e, s_)

        ylruT = RA[:, 0:16384].rearrange("p (c t) -> p c t", c=4)
        with contextlib.ExitStack() as es:
            win = RA[:, 16384:24576].rearrange("p (k n) -> p k n", k=8)
            hT = [RA[:, 24576 + i * 8192:24576 + (i + 1) * 8192].rearrange("p (k t) -> p k t", k=8) for i in range(2)]
            xn_bufs = [RA[:, 40960 + i * 1024:40960 + (i + 1) * 1024] for i in range(2)]
            xt = [sb(es, f"xtb{i}", [128, D]) for i in range(2)]
            HW_ = 512
            xpad = [sb(es, f"xpad{i}", [128, HW_ + 3]) for i in range(2)]
            xc = [sb(es, f"xc{i}", [128, HW_]) for i in range(2)]
            r_t = [sb(es, f"r_t{i}", [128, HW_]) for i in range(2)]
            ig_t = [sb(es, f"ig_t{i}", [128, HW_]) for i in range(2)]
            b_t = [sb(es, f"b_t{i}", [128, HW_]) for i in range(2)]
            h_t = [sb(es, f"h_t{i}", [128, HW_]) for i in range(2)]
            gg = [sb(es, f"gg{i}", [128, HW_]) for i in range(2)]
            g2 = [sb(es, f"g2{i}", [128, HW_]) for i in range(2)]
            xcb = [sb(es, f"xcb{i}", [128, HW_], BF16) for i in range(2)]
            rgbd_b = sb(es, "rgbd_b", [128, 4, 128], BF16)
            igbd_b = sb(es, "igbd_b", [128, 4, 128], BF16)
            rgbd = sb(es, "rgbd", [128, 4, 128])
            igbd = sb(es, "igbd", [128, 4, 128])
            convw = sb(es, "convw", [128, 16])
            lruv = sb(es, "lruv", [128, 16])
            cA = sb(es, "cA", [128, 4])
            tmp4 = sb(es, "tmp4", [128, 4])
            hlast = sb(es, "hlast", [128, 4])
            xtail = sb(es, "xtail", [128, 4, 3])
            pT = [ps(es, f"pTb{i}", [128, 8, 128], BF16) for i in range(2)]
            pxr = [ps(es, f"pxr{i}", [128, HW_]) for i in range(2)]
            pgr = [ps(es, f"pgr{i}", [128, HW_]) for i in range(2)]
            pgt = [ps(es, f"pgt{i}", [128, HW_]) for i in range(2)]
            s_w = sig(es, "lw")
            sg = (sig(es, "lx", 2), sig(es, "la"), sig(es, "lv"), sig(es, "lt"))
            s_x, s_act, s_v, s_pe = sg
            s_g = sig(es, "lg")
            allsig = [s_w, s_x, s_act, s_v, s_pe, s_g]

            def v(ins):
                val = s_v.inc(ins); W(V, s_v, val); return val

            def a(ins):
                val = s_act.inc(ins); W(A, s_act, val); return val

            def g(ins):
                val = s_g.inc(ins); W(G, s_g, val); return val

            wv = win_d.rearrange("(k p) n -> p k n", p=128)
            for kh in range(2):
                s_w.dma(G.dma_start(out=win[:, kh * 4:(kh + 1) * 4, :], in_=wv[:, kh * 4:(kh + 1) * 4, 1536:2560]))
            v(V.memset(rgbd[:], 0.0))
            v(V.memset(igbd[:], 0.0))
            v(V.memset(hlast[:], 0.0))
            v(V.memset(xtail[:], 0.0))
            W(SY, s_v)
            for cc in range(4):
                for hh in range(2):
                    s_w.dma(SY.dma_start(out=rgbd[hh * 64:(hh + 1) * 64, cc, hh * 64:(hh + 1) * 64], in_=rgw_d[2 * cc + hh]))
                    s_w.dma(SY.dma_start(out=igbd[hh * 64:(hh + 1) * 64, cc, hh * 64:(hh + 1) * 64], in_=igw_d[2 * cc + hh]))
            s_w.dma(SY.dma_start(out=convw[:], in_=convw_d))
            s_w.dma(SY.dma_start(out=lruv[:], in_=lruv_d))
            barrier([s_w])
            v(V.tensor_copy(out=rgbd_b[:], in_=rgbd[:]))
            v(V.tensor_copy(out=igbd_b[:], in_=igbd[:]))
            lam = lruv[:, :].rearrange("p (c f) -> p c f", f=4)[:, :, 3]
            a(A.activation(out=tmp4[:], in_=lam, func=AF.Exp, scale=-1.0))
            a(A.activation(out=tmp4[:], in_=tmp4[:], func=AF.Ln, bias=1.0, scale=1.0))
            W(V, s_act)
            v(V.tensor_scalar(out=cA[:], in0=tmp4[:], scalar1=-8.0, scalar2=None, op0=ALU.mult))
            barrier(allsig)
            hist2 = {}
            un = {}

            def S1(u, q, cc, hf, hq):
                p = u % 2
                U = un[u] = {}
                cw = lambda j: convw[:, cc * 4 + j:cc * 4 + j + 1]
                lv = lambda j: lruv[:, cc * 4 + j:cc * 4 + j + 1]
                tsl = slice(hf * HW_, (hf + 1) * HW_)
                if u - 2 in un:
                    P_ = un[u - 2]
                    for e in (T, V, A, G):
                        W(e, s_v, P_["v_end"])
                        W(e, s_act, P_["a_end"])
                        W(e, s_pe, P_["t_end"])
                        W(e, s_g, P_["g_end"])
                for k in range(8):
                    T.matmul(pxr[p][:], lhsT=win[:, k, cc * 128:(cc + 1) * 128], rhs=hq[:, k, tsl], start=(k == 0), stop=(k == 7))
                for k in range(8):
                    ins = T.matmul(pgr[p][:], lhsT=win[:, k, 512 + cc * 128:512 + (cc + 1) * 128], rhs=hq[:, k, tsl],
                                   start=(k == 0), stop=(k == 7))
                vt = s_pe.inc(ins)
                W(V, s_pe, vt)
                W(A, s_pe, vt)
                v(V.tensor_copy(out=xpad[p][:, 0:3], in_=xtail[:, cc, :]))
                v(V.tensor_copy(out=xpad[p][:, 3:HW_ + 3], in_=pxr[p][:]))
                v(V.tensor_copy(out=xtail[:, cc, :], in_=xpad[p][:, HW_:HW_ + 3]))
                v(V.tensor_scalar(out=xc[p][:], in0=xpad[p][:, 0:HW_], scalar1=cw(0), scalar2=lv(0), op0=ALU.mult, op1=ALU.add))
                for j in range(1, 4):
                    vxc = v(V.scalar_tensor_tensor(out=xc[p][:], in0=xpad[p][:, j:j + HW_], scalar=cw(j), in1=xc[p][:],
                                                   op0=ALU.mult, op1=ALU.add))
                va = a(A.activation(out=gg[p][:], in_=pgr[p][:], func=AF.Identity))
                W(G, s_v, vxc)
                vxb = g(G.tensor_copy(out=xcb[p][:], in_=xc[p][:]))
                W(T, s_g, vxb)
                W(G, s_act, va)
                g(G.tensor_tensor(out=g2[p][:], in0=gg[p][:], in1=gg[p][:], op=ALU.mult))
                g(G.tensor_scalar(out=g2[p][:], in0=g2[p][:], scalar1=0.044715, scalar2=1.0, op0=ALU.mult, op1=ALU.add))
                vg = g(G.tensor_tensor(out=g2[p][:], in0=g2[p][:], in1=gg[p][:], op=ALU.mult))
                ins = T.matmul(pgt[p][:], lhsT=rgbd_b[:, cc, :], rhs=xcb[p][:], start=True, stop=True)
                vt = s_pe.inc(ins)
                W(A, s_pe, vt)
                va = a(A.activation(out=r_t[p][:], in_=pgt[p][:], func=AF.Sigmoid, bias=lv(1), scale=1.0))
                W(T, s_act, va)
                ins = T.matmul(pgt[p][:], lhsT=igbd_b[:, cc, :], rhs=xcb[p][:], start=True, stop=True)
                vt = s_pe.inc(ins)
                W(A, s_pe, vt)
                a(A.activation(out=ig_t[p][:], in_=pgt[p][:], func=AF.Sigmoid, bias=lv(2), scale=1.0))
                W(A, s_g, vg)
                vgs = a(A.activation(out=g2[p][:], in_=g2[p][:], func=AF.Sigmoid, scale=1.5957691216))
                W(G, s_act, vgs)
                vg = g(G.tensor_tensor(out=g2[p][:], in0=g2[p][:], in1=gg[p][:], op=ALU.mult))
                va = a(A.activation(out=r_t[p][:], in_=r_t[p][:], func=AF.Exp, scale=cA[:, cc:cc + 1]))
                U["a"] = va
                U["t_end"] = vt
                U["g_end"] = vg
                U["cc"] = cc
                U["q"] = q
                U["hf"] = hf

            def S2a(u):
                p = u % 2
                U = un[u]
                cc, q, hf = U["cc"], U["q"], U["hf"]
                W(V, s_act, U["a"])
                v(V.scalar_tensor_tensor(out=b_t[p][:], in0=r_t[p][:], scalar=-1.0, in1=r_t[p][:], op0=ALU.mult,
                                         op1=ALU.mult))
                vb = v(V.tensor_scalar(out=b_t[p][:], in0=b_t[p][:], scalar1=1.0, scalar2=1e-30, op0=ALU.add,
                                       op1=ALU.max))
                W(A, s_v, vb)
                va = a(A.activation(out=b_t[p][:], in_=b_t[p][:], func=AF.Sqrt))
                U["a_end"] = va

            def S2b(u):
                p = u % 2
                U = un[u]
                cc, q, hf = U["cc"], U["q"], U["hf"]
                W(V, s_act, U["a_end"])
                v(V.tensor_tensor(out=b_t[p][:], in0=b_t[p][:], in1=ig_t[p][:], op=ALU.mult))
                v(V.tensor_tensor(out=b_t[p][:], in0=b_t[p][:], in1=xc[p][:], op=ALU.mult))
                v(V.tensor_tensor_scan(out=h_t[p][:], data0=r_t[p][:], data1=b_t[p][:], initial=hlast[:, cc:cc + 1],
                                       op0=ALU.mult, op1=ALU.add))
                v(V.tensor_copy(out=hlast[:, cc:cc + 1], in_=h_t[p][:, HW_ - 1:HW_]))
                W(V, s_g, U["g_end"])
                t0 = q * 1024 + hf * HW_
                U["v_end"] = v(V.tensor_tensor(out=ylruT[:, cc, t0:t0 + HW_], in0=g2[p][:], in1=h_t[p][:], op=ALU.mult))

            u = 0
            vhq = {}
            qlast = {}

            def emit_nt(q):
                if q >= 2:
                    W(V, s_pe, qlast[q - 2])
                hq_ = hT[q % 2]
                vhq[q] = emit_norm_transpose(es, list(range(q * 8, q * 8 + 8)),
                                             lambda j, hq_=hq_: hq_[:, :, j * 128:(j + 1) * 128], xn_bufs, pT, xt, sg, hist2)

            emit_nt(0)
            for q in range(4):
                hq = hT[q % 2]
                if q + 1 < 4:
                    emit_nt(q + 1)
                W(T, s_v, vhq[q])
                first = u
                for cc in range(4):
                    for hf in range(2):
                        if u - 1 >= first:
                            S2a(u - 1)
                        S1(u, q, cc, hf, hq)
                        if u - 1 >= first:
                            S2b(u - 1)
                        u += 1
                S2a(u - 1)
                S2b(u - 1)
                qlast[q] = un[u - 1]["t_end"]
            barrier(allsig)
            if dbg and stop == 3:
                dq = dbg_tensor("ylru", [128, 16384], BF16)
                s_out.dma(SY.dma_start(out=dq, in_=RA[:, 0:16384]))
                d2 = dbg_tensor("rstd3", [128, 32])
                s_out.dma(SY.dma_start(out=d2, in_=rstdall[:, :]))
                d3 = dbg_tensor("hT23", [128, 16384], BF16)
                s_out.dma(SY.dma_start(out=d3, in_=RA[:, 24576:40960]))
        if stop <= 3:
            W(SY, s_out)
            return nc, dbg_out

        M8all = sb(top, "M8all", [128, 32, 8])
        Lall = sb(top, "Lall", [128, 32, 32])
        rank_all = sb(top, "rank_all", [128, 32, 32])
        cnt_run = sb(top, "cnt_run", [128, 32])
        dest4f = sb(top, "dest4f", [128, 32, 4])
        dest4i = sb(top, "dest4i", [128, 128], I32)
        p4 = sb(top, "p4", [128, 32, 4])
        widx = sb(top, "widx", [128, NBP], I32)
        bidx = sb(top, "bidx", [128, NBP], I32)

        with contextlib.ExitStack() as es:
            wout = RA[:, 16384:24576].rearrange("p (k n) -> p k n", k=8)
            xt = [sb(es, f"x3_{i}", [128, D]) for i in range(2)]
            x1 = [sb(es, f"x1_{i}", [128, D]) for i in range(2)]
            t1 = sb(es, "t1", [128, D])
            h2 = sb(es, "h2", [128, D])
            h2b = [sb(es, f"h2b{i}", [128, D], BF16) for i in range(2)]
            h2T = sb(es, "h2T", [128, 8, 128])
            ysq = sb(es, "ysq", [128, 8, 128], BF16)
            rw = sb(es, "rw", [128, 8, 32])
            rb_row = sb(es, "rb_row", [128, 32])
            outg = sb(es, "outg", [128, 8])
            sd2 = sb(es, "sd2", [128, 2])
            rs2 = sb(es, "rs2", [128, 2])
            ss3 = sb(es, "ss3", [128, 1])
            rs3 = sb(es, "rs3", [128, 1])
            Mf = sb(es, "Mf", [128, 32])
            Mbf = sb(es, "Mbf", [128, 32], BF16)
            PA = [ps(es, f"PA{i}", [128, 512]) for i in range(2)]
            PL = [ps(es, f"PL{i}", [128, 512]) for i in range(2)]
            ptr = ps(es, "ptr", [128, 4, 128])
            psm = ps(es, "psm", [128, 512])
            psl = ps(es, "psl", [128, 512])
            psr = ps(es, "psr", [128, 512])
            s_d = sig(es, "3d", 2)
            s_v = sig(es, "3v")
            s_act = sig(es, "3a")
            s_pe = sig(es, "3t")
            s_st = sig(es, "3st", 4)
            allsig = [s_d, s_v, s_act, s_pe, s_st]

            def v(ins):
                val = s_v.inc(ins); W(V, s_v, val); return val

            def a(ins):
                val = s_act.inc(ins); W(A, s_act, val); return val

            s_d.dma(SY.dma_start(out=rw[:], in_=rw_d.rearrange("(k p) n -> p k n", p=128)))
            s_d.dma(SY.dma_start(out=rb_row[:], in_=rb_d.broadcast_to([128, 32])))
            s_d.dma(SY.dma_start(out=outg[:], in_=outg_d))
            v(V.memset(cnt_run[:], 0.0))
            wov = wout_d.rearrange("(k p) n -> p k n", p=128)
            for k in range(8):
                vd = s_d.dma(SY.dma_start(out=t1[:], in_=wov[:, k, :]))
                W(V, s_d, vd)
                vv = v(V.scalar_tensor_tensor(out=wout[:, k, :], in0=t1[:], scalar=outg[:, k:k + 1], in1=gm_row,
                                              op0=ALU.mult, op1=ALU.mult))
                W(SY, s_v, vv)
            barrier(allsig)
            s_g = sig(es, "3g")
            allsig.append(s_g)
            st3 = {tt: {} for tt in range(NT)}

            def vv(ins):
                return s_v.inc(ins)

            def stA(tt):
                b = tt % 2
                S_ = st3[tt]
                tsl = slice(tt * 128, (tt + 1) * 128)
                if tt - 2 >= 0:
                    W(SY, s_v, st3[tt - 2]["x1"])
                S_["x"] = s_d.dma(SY.dma_start(out=xt[b][:], in_=x_d[tsl, :]), lane=b)
                if tt - 1 >= 0:
                    W(G, s_pe, st3[tt - 1]["stats"])
                s_g.inc(G.tensor_tensor(out=ysq[:, 0:4, :], in0=yattnT[:, :, tsl], in1=yattnT[:, :, tsl], op=ALU.mult))
                vq = s_g.inc(G.tensor_tensor(out=ysq[:, 4:8, :], in0=ylruT[:, :, tsl], in1=ylruT[:, :, tsl], op=ALU.mult))
                W(T, s_g, vq)
                if tt - 1 >= 0:
                    W(T, s_act, st3[tt - 1]["sd2"])
                for gI in range(2):
                    for c in range(4):
                        ins = T.matmul(psm[:, gI:gI + 1], lhsT=ysq[:, gI * 4 + c, :], rhs=ones_bf[:, 0:1], start=(c == 0),
                                       stop=(c == 3))
                S_["stats"] = s_pe.inc(ins)
                if tt - 1 >= 0:
                    W(T, s_v, st3[tt - 1]["comb"])
                for dh in range(2):
                    for c in range(4):
                        T.matmul(PA[dh][:], lhsT=yattnT[:, c, tsl], rhs=wout[:, c, dh * 512:(dh + 1) * 512], start=(c == 0),
                                 stop=(c == 3))
                    for c in range(4):
                        ins = T.matmul(PL[dh][:], lhsT=ylruT[:, c, tsl], rhs=wout[:, 4 + c, dh * 512:(dh + 1) * 512],
                                       start=(c == 0), stop=(c == 3))
                S_["op"] = s_pe.inc(ins)
                W(A, s_pe, S_["stats"])
                if tt - 1 >= 0:
                    W(A, s_v, st3[tt - 1]["rs2"])
                S_["sd2"] = s_act.inc(A.activation(out=sd2[:], in_=psm[:, 0:2], func=AF.Sqrt, scale=1.0 / 512, bias=EPS))

            def stB1(tt):
                b = tt % 2
                S_ = st3[tt]
                tsl = slice(tt * 128, (tt + 1) * 128)
                W(V, s_act, S_["sd2"])
                S_["rs2"] = vv(V.reciprocal(out=rs2[:], in_=sd2[:]))
                W(V, s_v, S_["rs2"])
                W(V, s_pe, S_["op"])
                W(V, s_d, S_["x"])
                if tt - 1 >= 0:
                    W(V, s_act, st3[tt - 1]["sq"])
                for dh in range(2):
                    cs = slice(dh * 512, (dh + 1) * 512)
                    v_ = vv(V.tensor_scalar(out=t1[:, cs], in0=PA[dh][:], scalar1=rs2[:, 0:1], scalar2=None, op0=ALU.mult))
                    W(V, s_v, v_)
                    v_ = vv(V.scalar_tensor_tensor(out=t1[:, cs], in0=PL[dh][:], scalar=rs2[:, 1:2], in1=t1[:, cs], op0=ALU.mult,
                                                   op1=ALU.add))
                S_["comb"] = v_
                W(V, s_v, v_)
                if tt - 2 >= 0:
                    W(V, s_st, st3[tt - 2]["x1st"])
                    W(V, s_v, st3[tt - 2]["h2"])
                S_["x1"] = vv(V.tensor_tensor(out=x1[b][:], in0=t1[:], in1=xt[b][:], op=ALU.add))
                W(SY, s_v, S_["x1"])
                S_["x1st"] = s_st.dma(SY.dma_start(out=X1[tsl, :], in_=x1[b][:]), lane=b)
                W(A, s_v, S_["x1"])
                va = s_act.inc(A.activation(out=t1[:], in_=x1[b][:], func=AF.Square, accum_out=ss3[:]))
                S_["sq"] = va
                W(A, s_act, va)
                if tt - 1 >= 0:
                    W(A, s_v, st3[tt - 1]["rs3"])
                S_["ss3"] = s_act.inc(A.activation(out=ss3[:], in_=ss3[:], func=AF.Sqrt, scale=1.0 / D, bias=EPS))

            def stB2(tt):
                b = tt % 2
                S_ = st3[tt]
                tsl = slice(tt * 128, (tt + 1) * 128)
                W(V, s_act, S_["ss3"])
                S_["rs3"] = vv(V.reciprocal(out=rs3[:], in_=ss3[:]))
                W(V, s_v, S_["rs3"])
                if tt - 1 >= 0:
                    W(V, s_pe, st3[tt - 1]["tr"])
                    W(V, s_act, st3[tt - 1]["h2b"])
                v_ = vv(V.scalar_tensor_tensor(out=h2[:], in0=x1[b][:], scalar=rs3[:, 0:1], in1=Af_row, op0=ALU.mult,
                                               op1=ALU.mult))
                W(V, s_v, v_)
                S_["h2"] = vv(V.tensor_tensor(out=h2[:], in0=h2[:], in1=shf_row, op=ALU.add))
                W(A, s_v, S_["h2"])
                if tt - 2 >= 0:
                    W(A, s_st, st3[tt - 2]["h2st"])
                S_["h2b"] = s_act.inc(A.activation(out=h2b[b][:], in_=h2[:], func=AF.Copy))
                W(SY, s_act, S_["h2b"])
                S_["h2st"] = s_st.dma(SY.dma_start(out=H2[tsl, :], in_=h2b[b][:]), lane=2 + b)
                W(T, s_v, S_["h2"])
                if tt - 1 >= 0:
                    W(T, s_act, st3[tt - 1]["h2T"])
                for half in range(2):
                    if half == 1:
                        W(T, s_act, S_["h2T"])
                    for k in range(4):
                        kk = half * 4 + k
                        ins = T.transpose(ptr[:, k, :], h2[:, kk * 128:(kk + 1) * 128], ident_f[:])
                    S_["tr"] = s_pe.inc(ins)
                    W(A, s_pe, S_["tr"])
                    if tt - 1 >= 0 and half == 0:
                        W(A, s_pe, st3[tt - 1]["rt"])
                    S_["h2T"] = s_act.inc(A.activation(out=h2T[:, half * 4:(half + 1) * 4, :], in_=ptr[:], func=AF.Identity))
                W(T, s_act, S_["h2T"])
                if tt - 1 >= 0:
                    W(T, s_v, st3[tt - 1]["L"])
                for k in range(8):
                    ins = T.matmul(psl[:, 32:64], lhsT=h2T[:, k, :], rhs=rw[:, k, :], start=(k == 0), stop=(k == 7))
                S_["rt"] = s_pe.inc(ins)

            def stC(tt):
                S_ = st3[tt]
                W(V, s_pe, S_["rt"])
                S_["L"] = vv(V.tensor_tensor(out=Lall[:, tt, :], in0=psl[:, 32:64], in1=rb_row[:], op=ALU.add))
                W(V, s_v, S_["L"])
                v_ = vv(V.max(out=M8all[:, tt, :], in_=Lall[:, tt, :]))
                W(V, s_v, v_)
                if tt - 1 >= 0:
                    W(V, s_pe, st3[tt - 1]["rk"])
                v_ = vv(V.tensor_scalar(out=Mf[:], in0=Lall[:, tt, :], scalar1=M8all[:, tt, 3:4], scalar2=None, op0=ALU.is_ge))
                W(V, s_v, v_)
                vm = vv(V.tensor_copy(out=Mbf[:], in_=Mf[:]))
                W(T, s_v, vm)
                if tt - 1 >= 0:
                    W(T, s_v, st3[tt - 1]["cnt"])
                T.matmul(psr[:, 64:96], lhsT=tri_bf[:], rhs=Mbf[:], start=True, stop=True)
                ins = T.matmul(psr[:, 96:128], lhsT=ones_bf[:], rhs=Mbf[:], start=True, stop=True)
                S_["rk"] = s_pe.inc(ins)

            def stC2(tt):
                S_ = st3[tt]
                W(V, s_pe, S_["rk"])
                if tt - 1 >= 0:
                    W(V, s_v, st3[tt - 1]["cnt"])
                v_ = vv(V.tensor_tensor(out=rank_all[:, tt, :], in0=psr[:, 64:96], in1=cnt_run[:], op=ALU.add))
                W(V, s_v, v_)
                S_["cnt"] = vv(V.tensor_tensor(out=cnt_run[:], in0=psr[:, 96:128], in1=cnt_run[:], op=ALU.add))

            stA(0)
            for tt in range(NT):
                stB1(tt)
                if tt + 1 < NT:
                    stA(tt + 1)
                if tt - 1 >= 0:
                    stC(tt - 1)
                stB2(tt)
                if tt - 1 >= 0:
                    stC2(tt - 1)
            stC(NT - 1)
            stC2(NT - 1)
            barrier(allsig)

        with contextlib.ExitStack() as es:
            padded = sb(es, "padded", [128, 32])
            pad_end = sb(es, "pad_end", [128, 32])
            pad_start = sb(es, "pad_start", [128, 32])
            tmp32 = sb(es, "tmp32", [128, 32])
            destE = sb(es, "destE", [128, 32, 32])
            oh = sb(es, "oh", [128, 32, 32])
            E4 = sb(es, "E4", [128, 32, 4])
            den = sb(es, "den", [128, 32])
            tokid = sb(es, "tokid", [128, 32, 16], I32)
            zt = sb(es, "zt", [128, NSLOT * 16 // 128], I32)
            jv_i = sb(es, "jv_i", [128, NBP], I32)
            jv = sb(es, "jv", [128, NBP])
            pid_i = sb(es, "pid_i", [128, NBP], I32)
            pid = sb(es, "pid", [128, NBP])
            cmp = sb(es, "cmp", [128, NBP, 32])
            be = sb(es, "be", [128, NBP])
            s_v = sig(es, "4v")
            s_act = sig(es, "4a")
            s_g = sig(es, "4g")
            s_z = sig(es, "4z")

            def v(ins):
                val = s_v.inc(ins); W(V, s_v, val); return val

            def g(ins):
                val = s_g.inc(ins); W(G, s_g, val); return val

            g(G.iota(out=tokid[:], pattern=[[128, 32], [0, 16]], base=0, channel_multiplier=1))
            g(G.iota(out=jv_i[:], pattern=[[BLK, NBP]], base=0, channel_multiplier=0))
            g(G.iota(out=pid_i[:], pattern=[[0, NBP]], base=0, channel_multiplier=1))
            g(G.memset(zt[:], 0))
            s_z.dma(G.dma_start(out=SLOT.rearrange("(p f) c -> p (f c)", p=128), in_=zt[:]))
            W(V, s_g)
            v(V.tensor_copy(out=jv[:], in_=jv_i[:]))
            v(V.tensor_copy(out=pid[:], in_=pid_i[:]))
            v(V.tensor_tensor(out=destE[:], in0=cnt_run[:, :].unsqueeze(2).to_broadcast([128, 32, 32]),
                              in1=jv[:, 0:32].unsqueeze(1).to_broadcast([128, 32, 32]), op=ALU.is_gt))
            v(V.tensor_reduce(out=tmp32[:], in_=destE[:], axis=AX.X, op=ALU.add))
            v(V.tensor_scalar(out=padded[:], in0=tmp32[:], scalar1=float(BLK), scalar2=None, op0=ALU.mult))
            v(V.tensor_tensor_scan(out=pad_end[:], data0=ones_f[:, 0:32], data1=padded[:], initial=0.0, op0=ALU.mult,
                                   op1=ALU.add))
            v(V.tensor_tensor(out=pad_start[:], in0=pad_end[:], in1=padded[:], op=ALU.subtract))
            v(V.tensor_tensor(out=destE[:], in0=rank_all[:], in1=pad_start[:, :].unsqueeze(1).to_broadcast([128, 32, 32]),
                              op=ALU.add))
            for k in range(4):
                v(V.tensor_tensor(out=oh[:], in0=Lall[:], in1=M8all[:, :, k:k + 1].to_broadcast([128, 32, 32]),
                                  op=ALU.is_equal))
                v(V.tensor_tensor(out=oh[:], in0=oh[:], in1=destE[:], op=ALU.mult))
                v(V.tensor_reduce(out=dest4f[:, :, k], in_=oh[:], axis=AX.X, op=ALU.add))
            v(V.tensor_copy(out=dest4i[:], in_=dest4f[:, :, :].rearrange("p a b -> p (a b)")))
            vv = v(V.tensor_tensor(out=E4[:], in0=M8all[:, :, 0:4], in1=M8all[:, :, 0:1].to_broadcast([128, 32, 4]),
                                   op=ALU.subtract))
            W(A, s_v, vv)
            va = s_act.inc(A.activation(out=E4[:], in_=E4[:], func=AF.Exp))
            W(V, s_act, va)
            v(V.tensor_reduce(out=den[:], in_=E4[:], axis=AX.X, op=ALU.add))
            v(V.reciprocal(out=den[:], in_=den[:]))
            v(V.tensor_tensor(out=p4[:], in0=E4[:], in1=den[:, :].unsqueeze(2).to_broadcast([128, 32, 4]), op=ALU.mult))
            v(V.tensor_tensor(out=cmp[:], in0=pad_end[:, :].unsqueeze(1).to_broadcast([128, NBP, 32]),
                              in1=jv[:, :].unsqueeze(2).to_broadcast([128, NBP, 32]), op=ALU.is_le))
            v(V.tensor_reduce(out=be[:], in_=cmp[:], axis=AX.X, op=ALU.add))
            v(V.tensor_scalar(out=be[:], in0=be[:], scalar1=31.0, scalar2=None, op0=ALU.min))
            v(V.tensor_copy(out=bidx[:], in_=be[:]))
            v(V.scalar_tensor_tensor(out=be[:], in0=be[:], scalar=128.0, in1=pid[:], op0=ALU.mult, op1=ALU.add))
            v(V.tensor_scalar(out=jv[:], in0=jv[:], scalar1=pad_end[:, 31:32], scalar2=None, op0=ALU.is_lt))
            v(V.tensor_scalar(out=pid[:], in0=pid[:], scalar1=0.0, scalar2=None, op0=ALU.is_equal))
            v(V.tensor_tensor(out=jv[:], in0=jv[:], in1=pid[:], op=ALU.max))
            v(V.tensor_scalar(out=be[:], in0=be[:], scalar1=-100000.0, scalar2=None, op0=ALU.add))
            v(V.tensor_tensor(out=be[:], in0=be[:], in1=jv[:], op=ALU.mult))
            v(V.tensor_scalar(out=be[:], in0=be[:], scalar1=100000.0, scalar2=None, op0=ALU.add))
            v(V.tensor_copy(out=widx[:], in_=be[:]))
            W(G, s_v)
            W(G, s_z)
            for tt in range(NT):
                for k in range(4):
                    s_z.dma(G.indirect_dma_start(out=SLOT[:, :],
                                                 out_offset=bass.IndirectOffsetOnAxis(ap=dest4i[:, tt * 4 + k:tt * 4 + k + 1], axis=0),
                                                 in_=tokid[:, tt, :], in_offset=None))
            barrier([s_v, s_act, s_g, s_z])
            if dbg and stop == 4:
                for nm, t_, shp, dt_ in [("Lall", Lall, [128, 1024], F32), ("M8all", M8all, [128, 256], F32),
                                         ("dest4i", dest4i, [128, 128], I32), ("p4", p4, [128, 128], F32),
                                         ("widx", widx, [128, NBP], I32), ("bidx", bidx, [128, NBP], I32),
                                         ("cnt", cnt_run, [128, 32], F32)]:
                    dd = dbg_tensor(nm, shp, dt_)
                    ap_ = t_[:] if len(t_.shape) == 2 else t_[:, :, :].rearrange("p a b -> p (a b)")
                    s_out.dma(SY.dma_start(out=dd, in_=ap_))
                dd = dbg_tensor("slot", [NSLOT, 16], I32)
                s_out.dma(SY.dma_start(out=dd, in_=SLOT))
                dd = dbg_tensor("x1", [S, D])
                s_out.dma(SY.dma_start(out=dd, in_=X1))
                dd = dbg_tensor("h2", [S, D], BF16)
                s_out.dma(SY.dma_start(out=dd, in_=H2))
        if stop <= 4:
            W(SY, s_out)
            return nc, dbg_out

        with contextlib.ExitStack() as es:
            wbuf = [[RA[:, (par * 3 + m) * 8192:(par * 3 + m + 1) * 8192].rearrange("p (k n) -> p k n", k=8)
                     for m in range(3)] for par in range(2)]
            xb = [RB[:, par * 4096:par * 4096 + QB * 1024].rearrange("p (q d) -> p q d", q=QB) for par in range(2)]
            xbT = RB[:, 8192:8192 + 8 * BLK].rearrange("p (k s) -> p k s", k=8)
            actT0 = RB[:, 12288:12288 + 8 * BLK].rearrange("p (k s) -> p k s", k=8)
            actT1 = sb(es, "actT1", [128, 8, BLK], BF16)
            actT = [actT0, actT1]
            bgt = [sb(es, f"bgt{i}", [128, 8]) for i in range(2)]
            but = [sb(es, f"but{i}", [128, 8]) for i in range(2)]
            bdt = [sb(es, f"bdt{i}", [128, D]) for i in range(2)]
            tki = [sb(es, f"tki{i}", [128, QB, 16], I32) for i in range(2)]
            ysb = sb(es, "ysb", [128, QB, D])
            g_t = [sb(es, f"g_t{i}", [128, BLK]) for i in range(2)]
            sg_t = [sb(es, f"sg_t{i}", [128, BLK]) for i in range(2)]
            u_t = [sb(es, f"u_t{i}", [128, BLK]) for i in range(2)]
            ptx = [ps(es, f"ptx{i}", [128, QB, 128], BF16) for i in range(2)]
            pg = [ps(es, f"pg{i}", [128, BLK]) for i in range(2)]
            pu = [ps(es, f"pu{i}", [128, BLK]) for i in range(2)]
            py = [ps(es, f"py{i}", [128, 512]) for i in range(2)]
            s_tk = sig(es, "5tk", 2)
            s_xb = sig(es, "5xb", 2)
            s_wgu = sig(es, "5wgu", 2)
            s_wd = sig(es, "5wd", 2)
            s_pe = sig(es, "5t")
            s_v = sig(es, "5v")
            s_act = sig(es, "5a")
            s_yd = sig(es, "5y")
            blk = {j: {} for j in range(NBLK)}
            wsrc = (wg_d, wu_d, wd_d)
            bc_reg = G.to_reg(4095)

            def wgather(sg_, par, m, j):
                for c4 in range(4):
                    val = sg_.dma(G.indirect_dma_start(
                        out=RA[:, (par * 3 + m) * 8192 + c4 * 2048:(par * 3 + m) * 8192 + (c4 + 1) * 2048], out_offset=None,
                        in_=wsrc[m][0], in_offset=bass.IndirectOffsetOnAxis(ap=widx[:, j:j + 1], axis=0),
                        element_offset=c4 * 4096 * 2048, bounds_check=bc_reg, oob_is_err=False), lane=par)
                return val

            def loads_gu(j):
                par = j % 2
                st = blk[j]
                if j - 2 >= 0:
                    W(G, s_pe, blk[j - 2]["gu_done"])
                    W(G, s_v, blk[j - 2]["sw_done"])
                    W(G, s_act, blk[j - 2]["sw_act"])
                for q in range(QB):
                    vtk = s_tk.dma(G.dma_start(out=tki[par][:, q, :],
                                               in_=SLOT[j * BLK + q * 128:j * BLK + (q + 1) * 128, :]), lane=par)
                W(G, s_tk, vtk)
                for q in range(QB):
                    st["xb"] = s_xb.dma(G.indirect_dma_start(
                        out=xb[par][:, q, :], out_offset=None, in_=H2[:, :],
                        in_offset=bass.IndirectOffsetOnAxis(ap=tki[par][:, q, 0:1], axis=0)), lane=par)
                wgather(s_wgu, par, 0, j)
                wgather(s_wgu, par, 1, j)
                s_wgu.dma(G.indirect_dma_start(out=bgt[par][:], out_offset=None, in_=bg_d[:, :],
                                               in_offset=bass.IndirectOffsetOnAxis(ap=widx[:, j:j + 1], axis=0),
                                               bounds_check=bc_reg, oob_is_err=False), lane=par)
                st["wgu"] = s_wgu.dma(G.indirect_dma_start(out=but[par][:], out_offset=None, in_=bu_d[:, :],
                                                           in_offset=bass.IndirectOffsetOnAxis(ap=widx[:, j:j + 1], axis=0),
                                                           bounds_check=bc_reg, oob_is_err=False), lane=par)

            def loads_d(j):
                par = j % 2
                st = blk[j]
                if j - 2 >= 0:
                    W(G, s_pe, blk[j - 2]["d_done"])
                    W(G, s_v, blk[j - 2]["y_done"])
                wgather(s_wd, par, 2, j)
                st["wd"] = s_wd.dma(G.indirect_dma_start(out=bdt[par][:], out_offset=None, in_=bd_d[:, :],
                                                         in_offset=bass.IndirectOffsetOnAxis(ap=bidx[:, j:j + 1], axis=0)), lane=par)

            cnt5 = {"pgi": 0, "pyi": 0}
            pg_free = {}
            py_free = {}

            def TK(j, k):
                par = j % 2
                st = blk[j]
                if k == 0:
                    W(T, s_xb, st["xb"])
                    st["txe"] = {}
                ev_done = st["txe"]
                pb = ptx[k % 2]
                if k >= 2:
                    W(T, s_v, ev_done[k - 2])
                elif j > 0:
                    W(T, s_v, blk[j - 1]["txe"][6 + k])
                for q in range(QB):
                    ins = T.transpose(pb[:, q, :], xb[par][:, q, k * 128:(k + 1) * 128], ident_bf[:])
                vt = s_pe.inc(ins)
                W(V, s_pe, vt)
                ev_done[k] = s_v.inc(V.tensor_copy(out=xbT[:, k, :], in_=pb[:, :, :].rearrange("p q s -> p (q s)")))

            def GU(j):
                par = j % 2
                st = blk[j]
                ev_done = st["txe"]
                W(T, s_v, ev_done[7])
                W(T, s_wgu, st["wgu"])
                W(V, s_wgu, st["wgu"])
                W(A, s_wgu, st["wgu"])
                if j - 2 >= 0:
                    W(V, s_pe, blk[j - 2]["d_done"])
                at = actT[j % 2]
                for fc in range(8):
                    pgi = cnt5["pgi"]
                    gb = pgi % 2
                    if pgi >= 2:
                        W(T, s_v, pg_free[pgi - 2])
                    for k in range(8):
                        T.matmul(pg[gb][:], lhsT=wbuf[par][0][:, k, fc * 128:(fc + 1) * 128], rhs=xbT[:, k, :],
                                 start=(k == 0), stop=(k == 7))
                    for k in range(8):
                        ins = T.matmul(pu[gb][:], lhsT=wbuf[par][1][:, k, fc * 128:(fc + 1) * 128], rhs=xbT[:, k, :],
                                       start=(k == 0), stop=(k == 7))
                    vt = s_pe.inc(ins)
                    W(V, s_pe, vt)
                    W(A, s_pe, vt)
                    v1 = s_v.inc(V.tensor_scalar(out=g_t[gb][:], in0=pg[gb][:], scalar1=bgt[par][:, fc:fc + 1], scalar2=7.0,
                                                 op0=ALU.add, op1=ALU.min))
                    W(A, s_v, v1)
                    a1 = s_act.inc(A.activation(out=sg_t[gb][:], in_=g_t[gb][:], func=AF.Sigmoid, scale=1.702))
                    a2 = s_act.inc(A.activation(out=u_t[gb][:], in_=pu[gb][:], func=AF.Identity,
                                                bias=but[par][:, fc:fc + 1], scale=1.0))
                    W(V, s_act, a2)
                    v2 = s_v.inc(V.tensor_scalar(out=u_t[gb][:], in0=u_t[gb][:], scalar1=7.0, scalar2=-7.0, op0=ALU.min,
                                                 op1=ALU.max))
                    v3 = s_v.inc(V.tensor_tensor(out=g_t[gb][:], in0=g_t[gb][:], in1=sg_t[gb][:], op=ALU.mult))
                    W(V, s_v, v3)
                    v4 = s_v.inc(V.scalar_tensor_tensor(out=at[:, fc, :], in0=u_t[gb][:], scalar=1.0, in1=g_t[gb][:],
                                                        op0=ALU.add, op1=ALU.mult))
                    pg_free[pgi] = v4
                    cnt5["pgi"] += 1
                st["gu_done"] = vt
                st["sw_done"] = v4
                st["sw_act"] = a2

            def DN(j, jn=None):
                par = j % 2
                st = blk[j]
                at = actT[j % 2]
                W(T, s_v, st["sw_done"])
                W(T, s_wd, st["wd"])
                W(V, s_wd, st["wd"])
                if j - 1 >= 0:
                    W(V, s_yd, blk[j - 1]["yd"])
                for q in range(QB):
                    for dh in range(2):
                        pyi = cnt5["pyi"]
                        yb = pyi % 2
                        if pyi >= 2:
                            W(T, s_v, py_free[pyi - 2])
                        for fc in range(8):
                            ins = T.matmul(py[yb][:], lhsT=at[:, fc, q * 128:(q + 1) * 128],
                                           rhs=wbuf[par][2][:, fc, dh * 512:(dh + 1) * 512], start=(fc == 0), stop=(fc == 7))
                        vt = s_pe.inc(ins)
                        W(V, s_pe, vt)
                        py_free[pyi] = s_v.inc(V.tensor_tensor(out=ysb[:, q, dh * 512:(dh + 1) * 512], in0=py[yb][:],
                                                               in1=bdt[par][:, dh * 512:(dh + 1) * 512], op=ALU.add))
                        cnt5["pyi"] += 1
                        if jn is not None:
                            TK(jn, q * 2 + dh)
                if jn is not None:
                    for k in range(2 * QB, 8):
                        TK(jn, k)
                st["d_done"] = vt
                st["y_done"] = py_free[cnt5["pyi"] - 1]
                W(SY, s_v, st["y_done"])
                st["yd"] = s_yd.dma(SY.dma_start(out=Yd[j * BLK:(j + 1) * BLK, :].rearrange("(q p) d -> p q d", p=128),
                                                 in_=ysb[:]))

            loads_gu(0)
            loads_d(0)
            loads_gu(1)
            loads_d(1)
            for k in range(8):
                TK(0, k)
            for j in range(NBLK + 1):
                if j < NBLK:
                    GU(j)
                    if j + 2 < NBLK:
                        loads_gu(j + 2)
                if j >= 1:
                    DN(j - 1, j + 1 if j + 1 < NBLK else None)
                    if j + 1 < NBLK:
                        loads_d(j + 1)
                elif NBLK > 1:
                    for k in range(8):
                        TK(1, k)
            barrier([s_pe, s_v, s_act, s_yd, s_wgu, s_wd, s_xb, s_tk])

        if stop <= 5:
            if dbg:
                dd = dbg_tensor("Y", [NSLOT, D])
                s_out.dma(SY.dma_start(out=dd, in_=Yd))
            W(SY, s_out)
            return nc, dbg_out

        with contextlib.ExitStack() as es:
            RAf = RA[:, :].bitcast(F32)

            class _V:
                def __init__(self, ap): self.ap = ap
                def __getitem__(self, idx): return self.ap

            def fv(i):
                return _V(RAf[:, i * D:(i + 1) * D])
            yg = [[fv(i * 4 + k) for k in range(4)] for i in range(3)]
            x1t = [fv(12 + i) for i in range(3)]
            ot = [fv(17 + i) for i in range(2)]
            ss6 = sb(es, "ss6", [128, 32])
            rs6 = sb(es, "rs6", [128, 32])
            s_g = sig(es, "6g", 3)
            s_d = sig(es, "6d", 5)
            s_v = sig(es, "6v")
            s_act = sig(es, "6a")
            hist = {}

            def v(ins):
                val = s_v.inc(ins); W(V, s_v, val); return val

            def a(ins):
                val = s_act.inc(ins); W(A, s_act, val); return val

            def loads(tt):
                b = tt % 3
                if tt - 3 >= 0:
                    W(G, s_v, hist[tt - 3]["acc"])
                    W(SY, s_v, hist[tt - 3]["acc"])
                for k in range(4):
                    vg = s_g.dma(G.indirect_dma_start(out=yg[b][k][:], out_offset=None, in_=Yd[:, :],
                                                      in_offset=bass.IndirectOffsetOnAxis(
                                                          ap=dest4i[:, tt * 4 + k:tt * 4 + k + 1], axis=0)), lane=b)
                vd = s_d.dma(SY.dma_start(out=x1t[b][:], in_=X1[tt * 128:(tt + 1) * 128, :]), lane=b)
                hist[tt] = {"g": vg, "d": vd}

            accs = [fv(15), fv(16)]

            def comb(tt):
                b = tt % 2
                b3 = tt % 3
                ac = accs[b]
                W(V, s_g, hist[tt]["g"])
                W(V, s_d, hist[tt]["d"])
                v(V.tensor_scalar(out=ac[:], in0=yg[b3][0][:], scalar1=p4[:, tt, 0:1], scalar2=None, op0=ALU.mult))
                for k in range(1, 4):
                    v(V.scalar_tensor_tensor(out=ac[:], in0=yg[b3][k][:], scalar=p4[:, tt, k:k + 1], in1=ac[:],
                                             op0=ALU.mult, op1=ALU.add))
                v(V.tensor_tensor(out=ac[:], in0=ac[:], in1=gf_row, op=ALU.mult))
                vacc = v(V.tensor_tensor(out=ac[:], in0=ac[:], in1=x1t[b3][:], op=ALU.add))
                hist[tt]["acc"] = vacc
                W(A, s_v, vacc)
                if tt - 2 >= 0:
                    W(A, s_d, hist[tt - 2]["od"])
                va = s_act.inc(A.activation(out=ot[b][:], in_=ac[:], func=AF.Square, accum_out=ss6[:, tt:tt + 1]))
                hist[tt]["sq"] = va

            def fin(tt):
                b = tt % 2
                ac = accs[b]
                W(V, s_act, hist[tt]["sd"])
                v(V.reciprocal(out=rs6[:, tt:tt + 1], in_=ss6[:, tt:tt + 1]))
                vo = v(V.scalar_tensor_tensor(out=ot[b][:], in0=ac[:], scalar=rs6[:, tt:tt + 1], in1=fing_row[:], op0=ALU.mult,
                                              op1=ALU.mult))
                hist[tt]["fin"] = vo
                W(SY, s_v, vo)
                hist[tt]["od"] = s_d.dma(SY.dma_start(out=out_d[tt * 128:(tt + 1) * 128, :], in_=ot[b][:]), lane=3 + b)

            def sqrt_(tt):
                W(A, s_act, hist[tt]["sq"])
                hist[tt]["sd"] = s_act.inc(A.activation(out=ss6[:, tt:tt + 1], in_=ss6[:, tt:tt + 1], func=AF.Sqrt,
                                                        scale=1.0 / D, bias=EPS))

            loads(0)
            loads(1)
            loads(2)
            for tt in range(NT + 1):
                if tt < NT:
                    if tt - 2 >= 0:
                        W(V, s_v, hist[tt - 2]["fin"])
                    comb(tt)
                    sqrt_(tt)
                if tt - 1 >= 0:
                    fin(tt - 1)
                    if tt + 2 < NT:
                        loads(tt + 2)
            barrier([s_d, s_v, s_act, s_g])
        W(SY, s_out)
    return nc, dbg_out


def _layout_w(w):
    w5 = w.reshape(32, 4, 2, 128, 1024)
    return np.ascontiguousarray(w5.transpose(1, 0, 3, 2, 4)).reshape(4, 4096, 2048)


_SHARED_CACHE = {}


def make_in_maps(inp, ncores=8):
    f = lambda a: np.ascontiguousarray(a, dtype=np.float32)
    sh = {
        "ada_w": f(inp["ada_w"][0]),
        "ada_b_row": f(inp["ada_b"][0].reshape(1, -1)),
        "ada_b_col": f(inp["ada_b"][0].reshape(48, 128).T),
        "mixg_col": f(inp["mix_norm_g"][0].reshape(8, 128).T),
        "w_in": f(inp["w_in"][0]),
        "convw_col": f(inp["conv_w"][0].T.reshape(4, 128, 4).transpose(1, 0, 2).reshape(128, 16)),
        "lru_cols": f(np.stack([inp["conv_b"][0], inp["rg_b"][0], inp["ig_b"][0], inp["lru_lambda"][0]], axis=-1)
                      .reshape(4, 128, 4).transpose(1, 0, 2).reshape(128, 16)),
        "rg_w": f(inp["rg_w"][0]),
        "ig_w": f(inp["ig_w"][0]),
        "outg_col": f(np.concatenate([inp["attn_out_g"][0], inp["lru_out_g"][0]]).reshape(8, 128).T),
        "w_out": f(inp["w_out"][0]),
        "ffn_g_row": f(inp["ffn_norm_g"][0].reshape(1, -1)),
        "final_g_row": f(inp["final_norm_g"].reshape(1, -1)),
        "router_w": f(inp["router_w"][0]),
        "router_b_row": f(inp["router_b"][0].reshape(1, -1)),
        "wg": _layout_w(f(inp["exp_w_gate"][0])),
        "wu": _layout_w(f(inp["exp_w_up"][0])),
        "wd": _layout_w(f(inp["exp_w_down"][0])),
        "bg": f(inp["exp_b_gate"][0].reshape(32, 8, 128).transpose(0, 2, 1).reshape(4096, 8)),
        "bu": f(inp["exp_b_up"][0].reshape(32, 8, 128).transpose(0, 2, 1).reshape(4096, 8)),
        "bd": f(inp["exp_b_down"][0]),
    }
    maps = []
    for b in range(ncores):
        m = dict(sh)
        m["x"] = f(inp["x"][b])
        m["c_col"] = f(inp["c"][b].reshape(8, 128).T)
        maps.append(m)
    return maps


def kernel(**inputs):
    nc, _ = build_program()
    maps = make_in_maps(inputs, 8)
    res = run_bass_kernel_spmd(nc, maps, core_ids=list(range(8)))
    return np.stack([np.asarray(r["out"], dtype=np.float32) for r in res.results], axis=0)
```
